# Optimizing a Trainium2 kernel written in Bass

```python
import jax, jax.numpy as jnp
from jax import lax
import numpy as np

D_MODEL = 1024
BATCH = 16
SEQ = 2048
DEPTH = 2

CHUNK = 64
N_A_LAYERS = DEPTH // 2
N_B_LAYERS = DEPTH - N_A_LAYERS
EPS = 1e-6

MA_HEADS = 8
MA_QK = 64
MA_V = D_MODEL // MA_HEADS
MA_CONV = 4
MA_QK_COLS = 2 * MA_HEADS * MA_QK
MA_PROJ = MA_QK_COLS + 2 * D_MODEL + 2 * MA_HEADS

SB_HEADS = 16
SB_DIM = D_MODEL // SB_HEADS
SB_BLOCK = 128

PEER_HEADS = 8
PEER_NKEYS = 128
PEER_EXPERTS = PEER_NKEYS * PEER_NKEYS
PEER_QDIM = 256
PEER_TOPK = 16
PEER_BLOCK = 128

kernel_name = 'hybrid_mlstm_stickbreaking_peer'


def rmsnorm(x, g):
    xf = x.astype(jnp.float32)
    y = xf * lax.rsqrt(jnp.mean(xf * xf, axis=-1, keepdims=True) + EPS)
    return (y * g.astype(jnp.float32)).astype(x.dtype)


def ada_mod(c, w, b):
    return jnp.einsum('bd,de->be', jax.nn.silu(c), w) + b


def modulate(h, shift, scale):
    return h * (1.0 + scale[:, None, :]) + shift[:, None, :]


def causal_conv(x, w):
    width, ch = w.shape
    return lax.conv_general_dilated(x, w[:, None, :], window_strides=(1,), padding=[(width - 1, 0)],
                                    dimension_numbers=('NWC', 'WIO', 'NWC'), feature_group_count=ch)


def mlstm_mixer(h, w_in, conv_w, b_if, hnorm_g, w_out):
    bsz, seq, _ = h.shape
    nc = seq // CHUNK
    proj = jnp.einsum('bsd,de->bse', h, w_in)
    qk, v, o, gates = jnp.split(proj, [MA_QK_COLS, MA_QK_COLS + D_MODEL, MA_QK_COLS + 2 * D_MODEL], axis=-1)
    qk = jax.nn.silu(causal_conv(qk, conv_w))
    q, k = jnp.split(qk, 2, axis=-1)
    gates = (gates + b_if).astype(jnp.float32)
    i_pre, f_pre = jnp.split(gates, 2, axis=-1)

    def to_chunks(t, d):
        return t.reshape(bsz, nc, CHUNK, MA_HEADS, d).transpose(0, 3, 1, 2, 4)

    q = to_chunks(q, MA_QK).astype(jnp.float32) * (MA_QK ** -0.5)
    k = to_chunks(k, MA_QK).astype(jnp.float32)
    v = to_chunks(v, MA_V).astype(jnp.float32)
    i_pre = i_pre.reshape(bsz, nc, CHUNK, MA_HEADS).transpose(0, 3, 1, 2)
    log_f = jax.nn.log_sigmoid(f_pre).reshape(bsz, nc, CHUNK, MA_HEADS).transpose(0, 3, 1, 2)
    b = jnp.cumsum(log_f, axis=-1)
    b_last = b[..., -1]

    w_state = b_last[..., None] - b + i_pre
    m_loc = jnp.max(w_state, axis=-1)
    e_state = jnp.exp(w_state - m_loc[..., None])
    c_loc = jnp.einsum('bhcl,bhclv,bhclk->bhcvk', e_state, v, k)
    n_loc = jnp.einsum('bhcl,bhclk->bhck', e_state, k)

    def step(carry, inp):
        c_st, n_st, m_st = carry
        cl, nl, ml, bl = inp
        m_new = jnp.maximum(bl + m_st, ml)
        a = jnp.exp(bl + m_st - m_new)
        r = jnp.exp(ml - m_new)
        c_new = a[..., None, None] * c_st + r[..., None, None] * cl
        n_new = a[..., None] * n_st + r[..., None] * nl
        return (c_new, n_new, m_new), (c_st, n_st, m_st)

    init = (jnp.zeros((bsz, MA_HEADS, MA_V, MA_QK), jnp.float32),
            jnp.zeros((bsz, MA_HEADS, MA_QK), jnp.float32),
            jnp.zeros((bsz, MA_HEADS), jnp.float32))
    xs = (c_loc.transpose(2, 0, 1, 3, 4), n_loc.transpose(2, 0, 1, 3),
          m_loc.transpose(2, 0, 1), b_last.transpose(2, 0, 1))
    _, (c_prev, n_prev, m_prev) = lax.scan(step, init, xs)
    c_prev = c_prev.transpose(1, 2, 0, 3, 4)
    n_prev = n_prev.transpose(1, 2, 0, 3)
    m_prev = m_prev.transpose(1, 2, 0)

    causal = jnp.tril(jnp.ones((CHUNK, CHUNK), dtype=bool))
    log_d = jnp.where(causal, b[..., :, None] - b[..., None, :] + i_pre[..., None, :], -jnp.inf)
    inter_log = b + m_prev[..., None]
    m_t = jnp.maximum(inter_log, jnp.max(log_d, axis=-1))
    inter_w = jnp.exp(inter_log - m_t)
    s_qk = jnp.einsum('bhcld,bhcsd->bhcls', q, k) * jnp.exp(log_d - m_t[..., None])
    num = jnp.einsum('bhcls,bhcsv->bhclv', s_qk, v) + inter_w[..., None] * jnp.einsum('bhcvk,bhclk->bhclv', c_prev, q)
    den = jnp.sum(s_qk, axis=-1) + inter_w * jnp.einsum('bhck,bhclk->bhcl', n_prev, q)
    hh = num / jnp.maximum(jnp.abs(den), jnp.exp(-m_t))[..., None]
    hh = hh.transpose(0, 2, 3, 1, 4).reshape(bsz, seq, MA_HEADS, MA_V)
    hh = rmsnorm(hh, hnorm_g) * jax.nn.sigmoid(o.astype(jnp.float32)).reshape(bsz, seq, MA_HEADS, MA_V)
    return jnp.einsum('bse,ed->bsd', hh.reshape(bsz, seq, D_MODEL).astype(h.dtype), w_out)


def shared_kv(x, c, kv_ada_w, kv_ada_b, kv_norm_g, kv_w, k_norm_g):
    bsz, seq, _ = x.shape
    shift, scale = jnp.split(ada_mod(c, kv_ada_w, kv_ada_b), 2, axis=-1)
    h = modulate(rmsnorm(x, kv_norm_g), shift, scale)
    kv = jnp.einsum('bsd,de->bse', h, kv_w)
    k, v = jnp.split(kv, 2, axis=-1)
    k = rmsnorm(k.reshape(bsz, seq, SB_HEADS, SB_DIM), k_norm_g).transpose(0, 2, 1, 3)
    v = v.reshape(bsz, seq, SB_HEADS, SB_DIM).transpose(0, 2, 1, 3)
    return k, v


def stick_breaking_mixer(h, k, v, w_q, q_norm_g, w_out):
    bsz, seq, _ = h.shape
    q = jnp.einsum('bsd,de->bse', h, w_q).reshape(bsz, seq, SB_HEADS, SB_DIM)
    q = (rmsnorm(q, q_norm_g) * (SB_DIM ** -0.5)).transpose(0, 2, 1, 3)
    outs = []
    for blk in range(seq // SB_BLOCK):
        t0 = blk * SB_BLOCK
        end = t0 + SB_BLOCK
        z = jnp.einsum('bhtd,bhsd->bhts', q[:, :, t0:end], k[:, :, :end]).astype(jnp.float32)
        strict = jnp.arange(end)[None, :] < (t0 + jnp.arange(SB_BLOCK))[:, None]
        log1m = jnp.where(strict, -jax.nn.softplus(z), 0.0)
        between = lax.cumsum(log1m, axis=3, reverse=True) - log1m
        a = jnp.where(strict, jnp.exp(jax.nn.log_sigmoid(z) + between), 0.0)
        outs.append(jnp.einsum('bhts,bhsd->bhtd', a, v[:, :, :end].astype(jnp.float32)))
    o = jnp.concatenate(outs, axis=2).transpose(0, 2, 1, 3).reshape(bsz, seq, D_MODEL)
    return jnp.einsum('bse,ed->bsd', o.astype(h.dtype), w_out)


def peer_ffn(h, w_q, sub_keys, peer_u, peer_v):
    bsz, seq, d = h.shape
    xt = h.reshape(bsz * seq // PEER_BLOCK, PEER_BLOCK, d)

    def block(xb):
        q = jnp.einsum('td,de->te', xb, w_q).reshape(PEER_BLOCK, PEER_HEADS, 2, PEER_QDIM // 2)
        s = jnp.einsum('thpk,pnk->thpn', q, sub_keys).astype(jnp.float32)
        s_top, i_top = lax.top_k(s, PEER_TOPK)
        cand = (s_top[:, :, 0, :, None] + s_top[:, :, 1, None, :]).reshape(PEER_BLOCK, PEER_HEADS, PEER_TOPK * PEER_TOPK)
        cand_idx = (i_top[:, :, 0, :, None] * PEER_NKEYS + i_top[:, :, 1, None, :]).reshape(PEER_BLOCK, PEER_HEADS, PEER_TOPK * PEER_TOPK)
        g_top, pos = lax.top_k(cand, PEER_TOPK)
        expert = jnp.take_along_axis(cand_idx, pos, axis=-1)
        g = jax.nn.softmax(g_top, axis=-1)
        u = jnp.take(peer_u, expert, axis=0)
        act = jax.nn.gelu(jnp.einsum('thkd,td->thk', u, xb).astype(jnp.float32), approximate=False)
        vv = jnp.take(peer_v, expert, axis=0)
        return jnp.einsum('thk,thkd->td', (g * act).astype(xb.dtype), vv)

    return lax.map(block, xt).reshape(bsz, seq, d)


def setup_inputs(seed: int = 0) -> dict:
    key = jax.random.key(seed)
    ks = jax.random.split(key, 24)
    f32 = jnp.float32
    d = D_MODEL

    def nrm(k, shape, scale):
        return jax.random.normal(k, shape, f32) * scale

    def gain(k, shape):
        return 1.0 + 0.02 * jax.random.normal(k, shape, f32)

    if_base = jnp.concatenate([jnp.zeros((MA_HEADS,), f32), jnp.linspace(3.0, 6.0, MA_HEADS, dtype=f32)])
    return {
        'x': nrm(ks[0], (BATCH, SEQ, d), 1.0),
        'c': nrm(ks[1], (BATCH, d), 1.0),
        'ada_w': nrm(ks[2], (DEPTH, d, 6 * d), 0.5 * d ** -0.5),
        'ada_b': nrm(ks[3], (DEPTH, 6 * d), 0.01),
        'norm_mix_g': gain(ks[4], (DEPTH, d)),
        'norm_ffn_g': gain(ks[5], (DEPTH, d)),
        'ma_w_in': nrm(ks[6], (N_A_LAYERS, d, MA_PROJ), d ** -0.5),
        'ma_conv_w': nrm(ks[7], (N_A_LAYERS, MA_CONV, MA_QK_COLS), MA_CONV ** -0.5),
        'ma_b_if': if_base + nrm(ks[8], (N_A_LAYERS, 2 * MA_HEADS), 0.1),
        'ma_hnorm_g': gain(ks[9], (N_A_LAYERS, MA_HEADS, MA_V)),
        'ma_w_out': nrm(ks[10], (N_A_LAYERS, d, d), d ** -0.5),
        'kv_ada_w': nrm(ks[11], (d, 2 * d), 0.5 * d ** -0.5),
        'kv_ada_b': nrm(ks[12], (2 * d,), 0.01),
        'kv_norm_g': gain(ks[13], (d,)),
        'kv_w': nrm(ks[14], (d, 2 * d), d ** -0.5),
        'k_norm_g': gain(ks[15], (SB_DIM,)),
        'sb_w_q': nrm(ks[16], (N_B_LAYERS, d, d), d ** -0.5),
        'sb_q_norm_g': gain(ks[17], (N_B_LAYERS, SB_DIM)),
        'sb_w_out': nrm(ks[18], (N_B_LAYERS, d, d), d ** -0.5),
        'peer_w_q': nrm(ks[19], (DEPTH, d, PEER_HEADS * PEER_QDIM), d ** -0.5),
        'peer_sub_keys': nrm(ks[20], (DEPTH, 2, PEER_NKEYS, PEER_QDIM // 2), (PEER_QDIM // 2) ** -0.5),
        'peer_u': nrm(ks[21], (DEPTH, PEER_EXPERTS, d), d ** -0.5),
        'peer_v': nrm(ks[22], (DEPTH, PEER_EXPERTS, d), PEER_HEADS ** -0.5),
    }


def reference(x, c, ada_w, ada_b, norm_mix_g, norm_ffn_g, ma_w_in, ma_conv_w, ma_b_if, ma_hnorm_g, ma_w_out,
              kv_ada_w, kv_ada_b, kv_norm_g, kv_w, k_norm_g, sb_w_q, sb_q_norm_g, sb_w_out,
              peer_w_q, peer_sub_keys, peer_u, peer_v):
    k_sh = None
    v_sh = None
    for layer in range(DEPTH):
        sh1, sc1, g1, sh2, sc2, g2 = jnp.split(ada_mod(c, ada_w[layer], ada_b[layer]), 6, axis=-1)
        h = modulate(rmsnorm(x, norm_mix_g[layer]), sh1, sc1)
        if layer < N_A_LAYERS:
            y = mlstm_mixer(h, ma_w_in[layer], ma_conv_w[layer], ma_b_if[layer], ma_hnorm_g[layer], ma_w_out[layer])
        else:
            if layer == N_A_LAYERS:
                k_sh, v_sh = shared_kv(x, c, kv_ada_w, kv_ada_b, kv_norm_g, kv_w, k_norm_g)
            j = layer - N_A_LAYERS
            y = stick_breaking_mixer(h, k_sh, v_sh, sb_w_q[j], sb_q_norm_g[j], sb_w_out[j])
        x = x + g1[:, None, :] * y
        h = modulate(rmsnorm(x, norm_ffn_g[layer]), sh2, sc2)
        x = x + g2[:, None, :] * peer_ffn(h, peer_w_q[layer], peer_sub_keys[layer], peer_u[layer], peer_v[layer])
    return x
```

```python
import numpy as np
from contextlib import ExitStack
import concourse.bass as bass
import concourse.mybir as mybir
from concourse.bass_utils import run_bass_kernel_spmd

F32 = mybir.dt.float32
BF16 = mybir.dt.bfloat16
U32 = mybir.dt.uint32
I32 = mybir.dt.int32
AF = mybir.ActivationFunctionType
ALU = mybir.AluOpType
AX = mybir.AxisListType


class Buf:
    __slots__ = ("name", "w", "r", "t")

    def __init__(self, name, t=None):
        self.name = name
        self.w = None
        self.r = {}
        self.t = t

    def __getitem__(self, idx):
        return self.t[idx]


class Sched:
    NDMA = 12

    def __init__(self, nc, es):
        self.nc = nc
        self.es = es
        self.engs = {"pe": nc.tensor, "act": nc.scalar, "dve": nc.vector, "pool": nc.gpsimd, "sp": nc.sync}
        self.sem = {}
        self.cnt = {}
        for k in ("pe", "act", "dve", "pool"):
            self.sem[k] = es.enter_context(nc.semaphore("s_" + k))
            self.cnt[k] = 0
        self.dq = {}
        for q in ("sp", "act", "pool"):
            sems = [es.enter_context(nc.semaphore(f"d_{q}{i}")) for i in range(self.NDMA)]
            self.dq[q] = {"sems": sems, "n": 0}
            for i in range(self.NDMA):
                self.sem[("dma", q, i)] = sems[i]
                self.cnt[("dma", q, i)] = 0
        self.waited = {e: {} for e in self.engs}
        self.nins = 0

    def sbuf(self, name, shape, dt):
        self.nins += 0
        self._uid = getattr(self, "_uid", 0) + 1
        name = f"sb{self._uid}_{name}"
        t = self.es.enter_context(self.nc.sbuf_tensor(name, list(shape), dt))
        return Buf(name, t)

    def psum(self, name, shape, dt=F32):
        self._uid = getattr(self, "_uid", 0) + 1
        name = f"ps{self._uid}_{name}"
        t = self.es.enter_context(self.nc.psum_tensor(name, list(shape), dt))
        return Buf(name, t)

    def view(self, name):
        return Buf(name)

    def scope(self):
        return _Scope(self)

    def _wait(self, engine, key, c):
        if c <= 0:
            return
        w = self.waited[engine]
        if w.get(key, 0) >= c:
            return
        self.engs[engine].wait_ge(self.sem[key], c)
        w[key] = c

    def _deps(self, engine, reads, writes):
        deps = {}
        for b in reads:
            if b.w is not None:
                k, c = b.w
                deps[k] = max(deps.get(k, 0), c)
        for b in writes:
            if b.w is not None:
                k, c = b.w
                deps[k] = max(deps.get(k, 0), c)
            for k, c in b.r.items():
                deps[k] = max(deps.get(k, 0), c)
        return deps

    def op(self, engine, fn, reads=(), writes=()):
        deps = self._deps(engine, reads, writes)
        for k, c in deps.items():
            if k == engine and engine == "pe":
                continue
            self._wait(engine, k, c)
        ins = fn(self.engs[engine])
        self.cnt[engine] += 1
        ins.then_inc(self.sem[engine], 1)
        me = (engine, self.cnt[engine])
        for b in reads:
            b.r[engine] = self.cnt[engine]
        for b in writes:
            b.w = me
            b.r = {}
        self.nins += 1
        return ins

    def dma(self, q, fn, reads=(), writes=()):
        d = self.dq[q]
        i = d["n"] % self.NDMA
        d["n"] += 1
        key = ("dma", q, i)
        deps = self._deps(q, reads, writes)
        deps[key] = max(deps.get(key, 0), self.cnt[key])
        for k, c in deps.items():
            self._wait(q, k, c)
        ins = fn(self.engs[q])
        self.cnt[key] += 16
        ins.then_inc(self.sem[key], 16)
        me = (key, self.cnt[key])
        for b in reads:
            b.r[key] = self.cnt[key]
        for b in writes:
            b.w = me
            b.r = {}
        self.nins += 1
        return ins

    def finish(self, bufs):
        for b in bufs:
            if b.w is not None:
                k, c = b.w
                for e in ("sp", "act", "pool", "dve", "pe"):
                    self._wait(e, k, c)

    def barrier(self):
        for e in self.engs:
            for k, c in self.cnt.items():
                if c > 0 and k != e:
                    self._wait(e, k, c)


class _Scope:
    def __init__(self, S):
        self.S = S

    def __enter__(self):
        self.old = self.S.es
        self.es2 = ExitStack()
        self.es2.__enter__()
        self.S.es = self.es2
        return self

    def __exit__(self, *a):
        self.S.barrier()
        self.S.es = self.old
        return self.es2.__exit__(*a)


EPS = 1e-6
D = 1024


def load_bcast(S, q, dst, row_ap):
    S.dma(q, lambda e: e.dma_start(out=dst[:], in_=row_ap.partition_broadcast(128)), writes=[dst])


def emit_norm_mod(S, xs, gmod, shift, hb, st, junk):
    S.op("act", lambda e: e.activation(out=junk[:], in_=xs[:], func=AF.Square, accum_out=st[:, 0:1]),
         reads=[xs], writes=[junk, st])
    S.op("act", lambda e: e.activation(out=st[:, 1:2], in_=st[:, 0:1], func=AF.Sqrt, scale=1.0 / D, bias=EPS),
         reads=[st], writes=[st])
    S.op("dve", lambda e: e.reciprocal(out=st[:, 2:3], in_=st[:, 1:2]), reads=[st], writes=[st])
    S.op("dve", lambda e: e.scalar_tensor_tensor(out=junk[:], in0=xs[:], scalar=st[:, 2:3], in1=gmod[:],
                                                 op0=ALU.mult, op1=ALU.mult), reads=[xs, st, gmod], writes=[junk])
    S.op("pool", lambda e: e.tensor_add(out=hb[:], in0=junk[:], in1=shift[:]), reads=[junk, shift], writes=[hb])


def emit_transpose8(S, hb, pT, hT, identb):
    for kc in range(8):
        S.op("pe", lambda e: e.transpose(out=pT[:, kc, :], in_=hb[:, kc * 128:(kc + 1) * 128], identity=identb[:]),
             reads=[hb, identb], writes=[pT])
    S.op("act", lambda e: e.copy(out=hT[:], in_=pT[:]), reads=[pT], writes=[hT])


def emit_peer(S, nc, x_in, x_out, wq_d, sk_d, puv_d, gffn_row, mod_d, mod_base, ntiles, tiles_per_batch,
              identb, iota16, idx_base=0):
    wq = S.sbuf("wq", [128, 8, 2048], BF16)
    wq_v = wq_d.rearrange("(k p) n -> p k n", p=128)
    for kc in range(8):
        S.dma("pool", lambda e: e.dma_start(out=wq[:, kc, :], in_=wq_v[:, kc, :]), writes=[wq])
    skn = S.sbuf("skn", [128, 2, 128], BF16)
    S.dma("pool", lambda e: e.dma_start(out=skn[:], in_=sk_d.rearrange("p n k -> n p k")), writes=[skn])
    skT = S.sbuf("skT", [128, 2, 128], BF16)
    pT = S.psum("pT", [128, 8, 128], BF16)
    for p in range(2):
        S.op("pe", lambda e: e.transpose(out=pT[:, p, :], in_=skn[:, p, :], identity=identb[:]),
             reads=[skn, identb], writes=[pT])
    S.op("act", lambda e: e.copy(out=skT[:], in_=pT[:, 0:2, :]), reads=[pT], writes=[skT])

    grow = S.sbuf("grow", [128, D], F32)
    load_bcast(S, "sp", grow, gffn_row)
    gmod = S.sbuf("gmod", [128, D], F32)
    shift = S.sbuf("shift", [128, D], F32)
    nbatch = (ntiles + tiles_per_batch - 1) // tiles_per_batch
    gates = [S.sbuf(f"gate{i}", [128, D], F32) for i in range(nbatch)]
    for i in range(nbatch):
        load_bcast(S, "sp", gates[i], mod_d[i, mod_base + 2:mod_base + 3, :])

    NB = 2
    xs = [S.sbuf(f"xs{i}", [128, D], F32) for i in range(NB)]
    hb = [S.sbuf(f"hb{i}", [128, D], BF16) for i in range(NB)]
    hT = [S.sbuf(f"hT{i}", [128, 8, 128], BF16) for i in range(NB)]
    junk = S.sbuf("junk", [128, D], F32)
    junkb = S.sbuf("junkb", [128, D], BF16)
    st = [S.sbuf(f"st{i}", [128, 4], F32) for i in range(NB)]
    qT = S.sbuf("qT", [128, 16, 128], BF16)
    pq = [S.psum(f"pq{i}", [128, 4, 128], F32) for i in range(1)]
    ps = [S.psum(f"ps{i}", [128, 4, 128], F32) for i in range(1)]
    pTU = [pT, S.psum("pTU1", [128, 8, 128], BF16)]
    pMs = [S.psum(f"pM{i}", [128, 128], F32) for i in range(2)]
    UT = [S.sbuf(f"UT{i}", [128, 8, 128], BF16) for i in range(2)]
    junkM = S.sbuf("junkM", [128, 128], F32)
    identf = S.sbuf("identf", [128, 128], F32)
    S.op("dve", lambda e: e.tensor_copy(out=identf[:], in_=identb[:]), reads=[identb], writes=[identf])
    pout = [S.psum(f"po{i}", [128, 512], F32) for i in range(2)]
    s_sb = S.sbuf("s_sb", [128, 16, 128], F32)
    s2 = S.sbuf("s2", [128, 16, 128], F32)
    top = S.sbuf("top", [128, 16, 16], F32)
    tix = S.sbuf("tix", [128, 16, 16], U32)
    tixf = S.sbuf("tixf", [128, 16, 16], F32)
    cand = S.sbuf("cand", [128, 8, 256], F32)
    cand2 = S.sbuf("cand2", [128, 8, 256], F32)
    pos = S.sbuf("pos", [128, 8, 16], U32)
    posf = S.sbuf("posf", [128, 8, 16], F32)
    ai = S.sbuf("ai", [128, 8, 16], I32)
    af = S.sbuf("af", [128, 8, 16], F32)
    bf_ = S.sbuf("bf_", [128, 8, 16], F32)
    ia = S.sbuf("ia", [128, 8, 16], F32)
    ja = S.sbuf("ja", [128, 8, 16], F32)
    oh = S.sbuf("oh", [128, 2048], F32)
    g = S.sbuf("g", [128, 8, 16], F32)
    ge = S.sbuf("ge", [128, 8, 16], F32)
    gsum = S.sbuf("gsum", [128, 8], F32)
    ef = S.sbuf("ef", [128, 128], F32)
    eidx = [S.sbuf(f"eidx{i}", [128, 128], I32) for i in range(NB)]
    gs = [S.sbuf(f"gs{i}", [128, 128], F32) for i in range(NB)]
    act = S.sbuf("act", [128, 128], F32)
    wv = S.sbuf("wv", [128, 128], F32)
    NG = 14
    ug = [S.sbuf(f"ug{i}", [128, 2 * D], BF16) for i in range(NG)]
    actg = [S.sbuf(f"actg{i}", [128, 4], F32) for i in range(4)]
    wvg = [S.sbuf(f"wvg{i}", [128, 4], F32) for i in range(4)]
    ND = 8
    dg = [S.sbuf(f"dg{i}", [128, 128], BF16) for i in range(ND)]
    yo = [S.sbuf(f"yo{i}", [128, D], F32) for i in range(NB)]
    gi = 0
    di = 0
    ti = 0

    def stage_I(t):
        b = t // tiles_per_batch
        i2 = t % NB
        if t % tiles_per_batch == 0:
            load_bcast(S, "sp", shift, mod_d[b, mod_base + 0:mod_base + 1, :])
            load_bcast(S, "sp", gmod, mod_d[b, mod_base + 1:mod_base + 2, :])
            S.op("dve", lambda e: e.scalar_tensor_tensor(out=gmod[:], in0=gmod[:], scalar=1.0, in1=grow[:],
                                                         op0=ALU.add, op1=ALU.mult), reads=[gmod, grow], writes=[gmod])
        X, HB, HT, ST = xs[i2], hb[i2], hT[i2], st[i2]
        S.dma("sp", lambda e: e.dma_start(out=X[:], in_=x_in[t * 128:(t + 1) * 128, :]), writes=[X])
        yield
        emit_norm_mod(S, X, gmod, shift, HB, ST, junk)
        yield
        emit_transpose8(S, HB, pT, HT, identb)
        yield
        for gq in range(4):
            P = pq[0]
            for j in range(4):
                hp = gq * 4 + j
                for kc in range(8):
                    S.op("pe", lambda e: e.matmul(P[:, j, :], lhsT=wq[:, kc, hp * 128:(hp + 1) * 128], rhs=HT[:, kc, :],
                                                  start=(kc == 0), stop=(kc == 7)), reads=[wq, HT], writes=[P])
            S.op("act", lambda e: e.copy(out=qT[:, gq * 4:(gq + 1) * 4, :], in_=P[:]), reads=[P], writes=[qT])
            yield
        for gq in range(4):
            P = ps[0]
            for j in range(4):
                hp = gq * 4 + j
                S.op("pe", lambda e: e.matmul(P[:, j, :], lhsT=qT[:, hp, :], rhs=skT[:, hp % 2, :], start=True, stop=True),
                     reads=[qT, skT], writes=[P])
            S.op("act", lambda e: e.copy(out=s_sb[:, gq * 4:(gq + 1) * 4, :], in_=P[:]), reads=[P], writes=[s_sb])
            yield
        for hp in range(16):
            S.op("dve", lambda e: e.max(out=top[:, hp, 0:8], in_=s_sb[:, hp, :]), reads=[s_sb], writes=[top])
            S.op("dve", lambda e: e.max_index(out=tix[:, hp, 0:8], in_max=top[:, hp, 0:8], in_values=s_sb[:, hp, :]),
                 reads=[s_sb, top], writes=[tix])
            S.op("dve", lambda e: e.match_replace(out=s2[:, hp, :], in_to_replace=top[:, hp, 0:8], in_values=s_sb[:, hp, :],
                                                  imm_value=-1e30), reads=[s_sb, top], writes=[s2])
            S.op("dve", lambda e: e.max(out=top[:, hp, 8:16], in_=s2[:, hp, :]), reads=[s2], writes=[top])
            S.op("dve", lambda e: e.max_index(out=tix[:, hp, 8:16], in_max=top[:, hp, 8:16], in_values=s2[:, hp, :]),
                 reads=[s2, top], writes=[tix])
            yield
        S.op("dve", lambda e: e.tensor_copy(out=tixf[:], in_=tix[:]), reads=[tix], writes=[tixf])
        top4 = top[:].rearrange("q (h p) a -> q h p a", p=2)
        tix4 = tixf[:].rearrange("q (h p) a -> q h p a", p=2)
        c4 = cand[:].rearrange("q h (a b) -> q h a b", b=16)
        S.op("dve", lambda e: e.tensor_tensor(out=c4, in0=top4[:, :, 0, :].unsqueeze(3).to_broadcast([128, 8, 16, 16]),
                                              in1=top4[:, :, 1, :].unsqueeze(2).to_broadcast([128, 8, 16, 16]), op=ALU.add),
             reads=[top], writes=[cand])
        for h in range(8):
            S.op("dve", lambda e: e.max(out=g[:, h, 0:8], in_=cand[:, h, :]), reads=[cand], writes=[g])
            S.op("dve", lambda e: e.match_replace(out=cand2[:, h, :], in_to_replace=g[:, h, 0:8], in_values=cand[:, h, :],
                                                  imm_value=-1e30), reads=[cand, g], writes=[cand2])
            S.op("dve", lambda e: e.max(out=g[:, h, 8:16], in_=cand2[:, h, :]), reads=[cand2], writes=[g])
            yield
        for h in range(8):
            S.op("dve", lambda e: e.max_index(out=pos[:, h, 0:8], in_max=g[:, h, 0:8], in_values=cand[:, h, :]),
                 reads=[cand, g], writes=[pos])
            S.op("dve", lambda e: e.max_index(out=pos[:, h, 8:16], in_max=g[:, h, 8:16], in_values=cand2[:, h, :]),
                 reads=[cand2, g], writes=[pos])
            yield
        S.op("dve", lambda e: e.tensor_copy(out=posf[:], in_=pos[:]), reads=[pos], writes=[posf])
        S.op("dve", lambda e: e.tensor_scalar(out=ai[:], in0=posf[:], scalar1=0.0625, scalar2=-0.46875, op0=ALU.mult, op1=ALU.add),
             reads=[posf], writes=[ai])
        S.op("dve", lambda e: e.tensor_copy(out=af[:], in_=ai[:]), reads=[ai], writes=[af])
        S.op("dve", lambda e: e.scalar_tensor_tensor(out=bf_[:], in0=af[:], scalar=-16.0, in1=posf[:], op0=ALU.mult, op1=ALU.add),
             reads=[af, posf], writes=[bf_])
        yield
        oh4 = oh[:].rearrange("q (h k a) -> q h k a", k=16, a=16)
        io4 = iota16[:].unsqueeze(1).unsqueeze(1).to_broadcast([128, 8, 16, 16])
        for (src, pidx, dst) in ((af, 0, ia), (bf_, 1, ja)):
            S.op("dve", lambda e: e.tensor_tensor(out=oh4, in0=src[:].unsqueeze(3).to_broadcast([128, 8, 16, 16]), in1=io4, op=ALU.is_equal),
                 reads=[src, iota16], writes=[oh])
            S.op("dve", lambda e: e.tensor_tensor(out=oh4, in0=oh4, in1=tix4[:, :, pidx, :].unsqueeze(2).to_broadcast([128, 8, 16, 16]), op=ALU.mult),
                 reads=[oh, tixf], writes=[oh])
            S.op("dve", lambda e: e.reduce_sum(out=dst[:], in_=oh4, axis=AX.X), reads=[oh], writes=[dst])
            yield
        S.op("dve", lambda e: e.scalar_tensor_tensor(out=ef[:].rearrange("q (h k) -> q h k", k=16), in0=ia[:], scalar=128.0, in1=ja[:],
                                                     op0=ALU.mult, op1=ALU.add), reads=[ia, ja], writes=[ef])
        EI, GS = eidx[i2], gs[i2]
        S.op("dve", lambda e: e.tensor_scalar(out=ef[:], in0=ef[:], scalar1=16383.0, scalar2=0.0, op0=ALU.min, op1=ALU.max),
             reads=[ef], writes=[ef])
        if idx_base:
            S.op("dve", lambda e: e.tensor_scalar_add(out=ef[:], in0=ef[:], scalar1=float(idx_base)), reads=[ef], writes=[ef])
        S.op("dve", lambda e: e.tensor_copy(out=EI[:], in_=ef[:]), reads=[ef], writes=[EI])
        S.op("dve", lambda e: e.tensor_tensor(out=ge[:], in0=g[:], in1=g[:, :, 0:1].to_broadcast([128, 8, 16]), op=ALU.subtract),
             reads=[g], writes=[ge])
        S.op("act", lambda e: e.activation(out=ge[:], in_=ge[:], func=AF.Exp), reads=[ge], writes=[ge])
        S.op("dve", lambda e: e.reduce_sum(out=gsum[:], in_=ge[:], axis=AX.X), reads=[ge], writes=[gsum])
        S.op("dve", lambda e: e.reciprocal(out=gsum[:], in_=gsum[:]), reads=[gsum], writes=[gsum])
        S.op("dve", lambda e: e.tensor_tensor(out=GS[:].rearrange("q (h k) -> q h k", k=16), in0=ge[:],
                                              in1=gsum[:].unsqueeze(2).to_broadcast([128, 8, 16]), op=ALU.mult),
             reads=[ge, gsum], writes=[GS])
    def stage_UV(t, nxt):
        nonlocal gi, di, ti
        i2 = t % NB
        X, HB, EI, GS = xs[i2], hb[i2], eidx[i2], gs[i2]
        gate = gates[t // tiles_per_batch]
        HT = hT[i2]
        UVs = {}
        DGs = {}

        def front(s):
            nonlocal gi, ti
            UV = ug[gi % NG]
            gi += 1
            UVs[s] = UV
            S.dma("pool", lambda e: e.indirect_dma_start(out=UV[:], out_offset=None, in_=puv_d,
                                                         in_offset=bass.IndirectOffsetOnAxis(ap=EI[:, s:s + 1], axis=0)),
                  reads=[EI], writes=[UV])
            PT, UTb = pTU[ti % 2], UT[ti % 2]
            ti += 1
            for kc in range(8):
                S.op("pe", lambda e: e.transpose(out=PT[:, kc, :], in_=UV[:, kc * 128:(kc + 1) * 128], identity=identb[:]),
                     reads=[UV, identb], writes=[PT])
            S.op("act", lambda e: e.copy(out=UTb[:], in_=PT[:]), reads=[PT], writes=[UTb])
            return UTb

        def dot(s, UTb):
            j = s % 4
            AG = actg[(s // 4) % 4]
            R = pMs[s % 2]
            for kc in range(8):
                S.op("pe", lambda e: e.matmul(R[:], lhsT=UTb[:, kc, :], rhs=HT[:, kc, :], start=(kc == 0), stop=(kc == 7)),
                     reads=[UTb, HT], writes=[R])
            S.op("dve", lambda e: e.scalar_tensor_tensor(out=junkM[:], in0=R[:], scalar=1.0, in1=identf[:], op0=ALU.mult,
                                                         op1=ALU.mult, accum_out=AG[:, j:j + 1]),
                 reads=[R, identf], writes=[junkM, AG])

        def act_chain(grp):
            nonlocal di
            AG, WG = actg[grp % 4], wvg[grp % 4]
            S.op("act", lambda e: e.activation(out=WG[:], in_=AG[:], func=AF.Gelu), reads=[AG], writes=[WG])
            for j in range(4):
                S.op("act", lambda e: e.mul(out=WG[:, j:j + 1], in_=WG[:, j:j + 1], mul=GS[:, grp * 4 + j:grp * 4 + j + 1]),
                     reads=[WG, GS], writes=[WG])
            for j in range(4):
                DG = dg[di % ND]
                di += 1
                DGs[grp * 4 + j] = DG
                S.op("act", lambda e: e.mul(out=DG[:], in_=identb[:], mul=WG[:, j:j + 1]), reads=[identb, WG], writes=[DG])

        def vmm(grp):
            for j in range(4):
                s = grp * 4 + j
                DG, UV = DGs.pop(s), UVs.pop(s)
                for hh in range(2):
                    S.op("pe", lambda e: e.matmul(pout[hh][:], lhsT=DG[:], rhs=UV[:, D + hh * 512:D + (hh + 1) * 512],
                                                  start=(s == 0), stop=(s == 127)), reads=[DG, UV], writes=[pout[hh]])

        pend = {}
        for s in range(128 + 3):
            if s < 128:
                pend[s] = front(s)
            if 0 <= s - 1 < 128:
                dot(s - 1, pend.pop(s - 1))
                if (s - 1) % 4 == 3:
                    act_chain((s - 1) // 4)
            if 0 <= s - 3 < 128 and (s - 3) % 4 == 3:
                vmm((s - 3) // 4)
            if s % 4 == 3:
                for _ in range(6):
                    next(nxt, None)
        Y = yo[i2]
        for hh in range(2):
            S.op("dve", lambda e: e.tensor_mul(out=Y[:, hh * 512:(hh + 1) * 512], in0=pout[hh][:],
                                               in1=gate[:, hh * 512:(hh + 1) * 512]), reads=[pout[hh], gate], writes=[Y])
        S.op("pool", lambda e: e.tensor_add(out=Y[:], in0=Y[:], in1=X[:]), reads=[Y, X], writes=[Y])
        S.dma("sp", lambda e: e.dma_start(out=x_out[t * 128:(t + 1) * 128, :], in_=Y[:]), reads=[Y], writes=[])
        for _ in nxt:
            pass

    g0 = stage_I(0)
    for _ in g0:
        pass
    for t in range(ntiles):
        nxt = stage_I(t + 1) if t + 1 < ntiles else iter(())
        stage_UV(t, nxt)


def emit_convert(S, nc, pairs, rows):
    NBUF = 4
    bufs = [S.sbuf(f"cv{i}", [128, 4, D], BF16) for i in range(NBUF)]
    i = 0
    for src, dst in pairs:
        sv = src.rearrange("(c p r) d -> c p r d", p=128, r=4)
        dv = dst.rearrange("(c p r) d -> c p r d", p=128, r=4)
        for c in range(rows // 512):
            B = bufs[i % NBUF]
            S.dma("pool", lambda e: e.dma_start(out=B[:], in_=sv[c]), writes=[B])
            S.dma(("sp", "act")[i % 2], lambda e: e.dma_start(out=dv[c], in_=B[:]), reads=[B], writes=[])
            i += 1


def emit_convert_inplace(S, nc, tabs, rows):
    NBUF = 4
    bufs = [S.sbuf(f"cv{i}", [128, 4, D], BF16) for i in range(NBUF)]
    views = []
    i = 0
    for tab in tabs:
        sv = tab.rearrange("(c p r) d -> c p r d", p=128, r=4)
        v16 = tab.bitcast(BF16).rearrange("n (two d) -> (n two) d", two=2)
        dv = v16[0:rows, :].rearrange("(c p r) d -> c p r d", p=128, r=4)
        views.append(v16)
        for c in range(rows // 512):
            B = bufs[i % NBUF]
            S.dma("pool", lambda e: e.dma_start(out=B[:], in_=sv[c]), writes=[B])
            S.dma(("sp", "act")[i % 2], lambda e: e.dma_start(out=dv[c], in_=B[:]), reads=[B], writes=[])
            i += 1
    return views


def emit_convert_uv(S, nc, tab_u, tab_v, rows):
    NBUF = 3
    bufs = [S.sbuf(f"cuv{i}", [128, 4, 2 * D], BF16) for i in range(NBUF)]
    su = tab_u.rearrange("(c p r) d -> c p r d", p=128, r=4)
    sv = tab_v.rearrange("(c p r) d -> c p r d", p=128, r=4)
    uv = tab_u.bitcast(BF16)
    dv = uv.rearrange("(c p r) d -> c p r d", p=128, r=4)
    for c in range(rows // 512):
        B = bufs[c % NBUF]
        S.dma("pool", lambda e: e.dma_start(out=B[:, :, 0:D], in_=su[c]), writes=[B])
        S.dma("pool", lambda e: e.dma_start(out=B[:, :, D:2 * D], in_=sv[c]), writes=[B])
        S.dma(("sp", "act")[c % 2], lambda e: e.dma_start(out=dv[c], in_=B[:]), reads=[B], writes=[])
    return uv


def gen_convert_uv(S, nc, tab_u, tab_v, rows):
    NBUF = 3
    bufs = [S.sbuf(f"cuv{i}", [128, 4, 2 * D], BF16) for i in range(NBUF)]
    su = tab_u.rearrange("(c p r) d -> c p r d", p=128, r=4)
    sv = tab_v.rearrange("(c p r) d -> c p r d", p=128, r=4)
    uv = tab_u.bitcast(BF16)
    dv = uv.rearrange("(c p r) d -> c p r d", p=128, r=4)
    for c in range(rows // 512):
        B = bufs[c % NBUF]
        S.dma("pool", lambda e: e.dma_start(out=B[:, :, 0:D], in_=su[c]), writes=[B])
        S.dma("pool", lambda e: e.dma_start(out=B[:, :, D:2 * D], in_=sv[c]), writes=[B])
        S.dma("sp", lambda e: e.dma_start(out=dv[c], in_=B[:]), reads=[B], writes=[])
        yield


import math


def emit_ada(S, nc, cT_d, ada_w_d, ada_b_d, kvw_d, kvb_d, mod_d):
    cT = S.sbuf("cT", [128, 8, 2], F32)
    S.dma("sp", lambda e: e.dma_start(out=cT[:], in_=cT_d), writes=[cT])
    cs = S.sbuf("cs", [128, 8, 2], F32)
    S.op("act", lambda e: e.activation(out=cs[:], in_=cT[:], func=AF.Silu), reads=[cT], writes=[cs])
    wch = [S.sbuf(f"wch{i}", [128, 8, 512], F32) for i in range(2)]
    bch = [S.sbuf(f"bch{i}", [2, 512], F32) for i in range(2)]
    och = [S.sbuf(f"och{i}", [2, 512], F32) for i in range(2)]
    pa = [S.psum(f"pada{i}", [128, 512], F32) for i in range(2)]
    jobs = []
    for l in range(2):
        for ch in range(12):
            jobs.append((ada_w_d[l], ada_b_d[l:l + 1, :], ch, 6 * l + ch // 2, ch % 2))
    for ch in range(4):
        jobs.append((kvw_d, kvb_d, ch, 12 + ch // 2, ch % 2))
    for i, (w_d, b_d, ch, row, half) in enumerate(jobs):
        W, B, O, P = wch[i % 2], bch[i % 2], och[i % 2], pa[i % 2]
        wv = w_d.rearrange("(k p) n -> p k n", p=128)
        q = ("sp", "act")[i % 2]
        S.dma(q, lambda e: e.dma_start(out=W[:], in_=wv[:, :, ch * 512:(ch + 1) * 512]), writes=[W])
        S.dma(q, lambda e: e.dma_start(out=B[:], in_=b_d[:, ch * 512:(ch + 1) * 512].partition_broadcast(2)), writes=[B])
        for kc in range(8):
            S.op("pe", lambda e: e.matmul(P[0:2, :], lhsT=cs[:, kc, :], rhs=W[:, kc, :], start=(kc == 0), stop=(kc == 7)),
                 reads=[cs, W], writes=[P])
        S.op("dve", lambda e: e.tensor_add(out=O[:], in0=P[0:2, :], in1=B[:]), reads=[P, B], writes=[O])
        S.dma(q, lambda e: e.dma_start(out=mod_d[:, row, half * 512:(half + 1) * 512], in_=O[:]), reads=[O], writes=[])


def emit_mlstm(S, nc, x_in, x_out, win_d, cwT_d, bif_d, hg_d, wout_d, gmix_row, mod_d, ntiles, tiles_per_seq,
               identb, utri, ones, side=None):
    win = S.sbuf("win", [128, 8, 3088], BF16)
    win_v = win_d.rearrange("(k p) n -> p k n", p=128)
    for kc in range(8):
        S.dma("pool", lambda e: e.dma_start(out=win[:, kc, 0:2048], in_=win_v[:, kc, 0:2048]), writes=[win])
        S.dma("pool", lambda e: e.dma_start(out=win[:, kc, 2048:3088], in_=win_v[:, kc, 2048:3088]), writes=[win])
    wout = S.sbuf("wout", [128, 8, 1024], BF16)
    wout_v = wout_d.rearrange("(k p) n -> p k n", p=128)
    for kc in range(8):
        S.dma("pool", lambda e: e.dma_start(out=wout[:, kc, :], in_=wout_v[:, kc, :]), writes=[wout])
    cwT = S.sbuf("cwT", [128, 8, 4], F32)
    S.dma("sp", lambda e: e.dma_start(out=cwT[:], in_=cwT_d), writes=[cwT])
    bif = S.sbuf("bif", [128, 16], F32)
    load_bcast(S, "sp", bif, bif_d)
    hg = S.sbuf("hg", [128, D], F32)
    load_bcast(S, "sp", hg, hg_d)
    grow = S.sbuf("grow", [128, D], F32)
    load_bcast(S, "sp", grow, gmix_row)
    gmod = S.sbuf("gmod", [128, D], F32)
    shift = S.sbuf("shift", [128, D], F32)
    gate = S.sbuf("gate", [128, D], F32)

    NB = 2
    xs = [S.sbuf(f"xs{i}", [128, D], F32) for i in range(NB)]
    hb = S.sbuf("hb", [128, D], BF16)
    hT = S.sbuf("hT", [128, 8, 128], BF16)
    junk = S.sbuf("junk", [128, D], F32)
    st = S.sbuf("st", [128, 4], F32)
    cb = S.sbuf("cb", [128, 8, 131], F32)
    cacc = S.sbuf("cacc", [128, 8, 128], F32)
    ctmp = S.sbuf("ctmp", [128, 8, 128], F32)
    qkT = S.sbuf("qkT", [128, 8, 128], BF16)
    ktok = S.sbuf("ktok", [128, 4, 128], BF16)
    qm = S.sbuf("qm", [128, 8, 128], BF16)
    S.op("pool", lambda e: e.memset(qm[:], 0.0), writes=[qm])
    gts = S.sbuf("gts", [128, 16], F32)
    spf = S.sbuf("spf", [128, 8], F32)
    wexp = S.sbuf("wexp", [128, 8], F32)
    eb = S.sbuf("eb", [128, 8], F32)
    ebL = S.sbuf("ebL", [128, 8], F32)
    vaug = S.sbuf("vaug", [128, 8, 129], BF16)
    sig = S.sbuf("sig", [128, D], F32)
    ATs = S.sbuf("ATs", [128, 8, 128], BF16)
    Cst = S.sbuf("Cst", [128, 8, 129], F32)
    Cbf = S.sbuf("Cbf", [128, 8, 129], BF16)
    den = S.sbuf("den", [128, 8], F32)
    hh = S.sbuf("hh", [128, 8, 128], F32)
    sq = S.sbuf("sq", [128, 8, 128], F32)
    hss = S.sbuf("hss", [128, 8], F32)
    hob = S.sbuf("hob", [128, D], BF16)
    hoT = S.sbuf("hoT", [128, 8, 128], BF16)
    yo = [S.sbuf(f"yo{i}", [128, D], F32) for i in range(NB)]

    pT = S.psum("pT", [128, 8, 128], BF16)
    pg = [S.psum(f"pg{i}", [128, 512], F32) for i in range(2)]
    pAT = [S.psum(f"pAT{i}", [128, 4, 128], F32) for i in range(2)]
    pOS = [S.psum(f"pOS{i}", [128, 3, 160], F32) for i in range(3)]
    pgi = 0

    def PO(h):
        return pOS[h // 3], h % 3

    LN8 = math.log(0.125)
    for t in range(ntiles):
        sidx = t // tiles_per_seq
        first = (t % tiles_per_seq == 0)
        X = xs[t % NB]
        if first:
            load_bcast(S, "sp", shift, mod_d[sidx, 0:1, :])
            load_bcast(S, "sp", gmod, mod_d[sidx, 1:2, :])
            load_bcast(S, "sp", gate, mod_d[sidx, 2:3, :])
            S.op("dve", lambda e: e.scalar_tensor_tensor(out=gmod[:], in0=gmod[:], scalar=1.0, in1=grow[:],
                                                         op0=ALU.add, op1=ALU.mult), reads=[gmod, grow], writes=[gmod])
            S.op("pool", lambda e: e.memset(cb[:, :, 0:3], 0.0), writes=[cb])
            S.op("pool", lambda e: e.memset(Cst[:], 0.0), writes=[Cst])
            S.op("pool", lambda e: e.memset(Cbf[:], 0.0), writes=[Cbf])
        S.dma("sp", lambda e: e.dma_start(out=X[:], in_=x_in[t * 128:(t + 1) * 128, :]), writes=[X])
        if side is not None:
            next(side, None)
            next(side, None)
        emit_norm_mod(S, X, gmod, shift, hb, st, junk)
        emit_transpose8(S, hb, pT, hT, identb)
        for half in range(2):
            P = pg[pgi % 2]
            pgi += 1
            for j in range(4):
                fc = half * 4 + j
                for kc in range(8):
                    S.op("pe", lambda e: e.matmul(P[:, j * 128:(j + 1) * 128], lhsT=win[:, kc, fc * 128:(fc + 1) * 128],
                                                  rhs=hT[:, kc, :], start=(kc == 0), stop=(kc == 7)),
                         reads=[win, hT], writes=[P])
            S.op("act", lambda e: e.copy(out=cb[:, half * 4:(half + 1) * 4, 3:131],
                                         in_=P[:].rearrange("p (j n) -> p j n", n=128)), reads=[P], writes=[cb])
        def cw(w):
            return cwT[:, :, w:w + 1].to_broadcast([128, 8, 128])
        S.op("dve", lambda e: e.tensor_tensor(out=cacc[:], in0=cb[:, :, 3:131], in1=cw(3), op=ALU.mult), reads=[cb, cwT], writes=[cacc])
        for w in range(3):
            S.op("pool", lambda e: e.tensor_tensor(out=ctmp[:], in0=cb[:, :, w:w + 128], in1=cw(w), op=ALU.mult),
                 reads=[cb, cwT], writes=[ctmp])
            S.op("dve", lambda e: e.tensor_add(out=cacc[:], in0=cacc[:], in1=ctmp[:]), reads=[cacc, ctmp], writes=[cacc])
        S.op("act", lambda e: e.activation(out=qkT[:, 4:8, :], in_=cacc[:, 4:8, :], func=AF.Silu), reads=[cacc], writes=[qkT])
        qm4 = qm[:].rearrange("p (f two) n -> p f two n", two=2)
        S.op("act", lambda e: e.activation(out=qm4[0:64, :, 0, :], in_=cacc[0:64, 0:4, :], func=AF.Silu), reads=[cacc], writes=[qm])
        S.op("act", lambda e: e.activation(out=qm4[64:128, :, 1, :], in_=cacc[64:128, 0:4, :], func=AF.Silu), reads=[cacc], writes=[qm])
        S.op("pool", lambda e: e.tensor_copy(out=cb[:, :, 0:3], in_=cb[:, :, 128:131]), reads=[cb], writes=[cb])
        for j in range(4):
            S.op("pe", lambda e: e.transpose(out=pT[:, j, :], in_=qkT[:, 4 + j, :], identity=identb[:]),
                 reads=[qkT, identb], writes=[pT])
        S.op("act", lambda e: e.copy(out=ktok[:], in_=pT[:, 0:4, :]), reads=[pT], writes=[ktok])
        P = pg[pgi % 2]
        pgi += 1
        for kc in range(8):
            S.op("pe", lambda e: e.matmul(P[:, 0:16], lhsT=hT[:, kc, :], rhs=win[:, kc, 3072:3088], start=(kc == 0), stop=(kc == 7)),
                 reads=[hT, win], writes=[P])
        S.op("dve", lambda e: e.tensor_add(out=gts[:], in0=P[:, 0:16], in1=bif[:]), reads=[P, bif], writes=[gts])
        S.op("act", lambda e: e.activation(out=spf[:], in_=gts[:, 8:16], func=AF.Exp, scale=-1.0), reads=[gts], writes=[spf])
        S.op("act", lambda e: e.activation(out=spf[:], in_=spf[:], func=AF.Ln, bias=1.0), reads=[spf], writes=[spf])
        S.op("pe", lambda e: e.matmul(P[:, 16:24], lhsT=utri[:], rhs=spf[:], start=True, stop=True), reads=[utri, spf], writes=[P])
        S.op("pe", lambda e: e.matmul(P[:, 32:40], lhsT=ones[:], rhs=spf[:], start=True, stop=True), reads=[ones, spf], writes=[P])
        S.op("dve", lambda e: e.tensor_add(out=wexp[:], in0=P[:, 16:24], in1=gts[:, 0:8]), reads=[P, gts], writes=[wexp])
        S.op("act", lambda e: e.activation(out=wexp[:], in_=wexp[:], func=AF.Exp), reads=[wexp], writes=[wexp])
        S.op("act", lambda e: e.activation(out=eb[:], in_=P[:, 16:24], func=AF.Exp, scale=-1.0, bias=LN8), reads=[P], writes=[eb])
        S.op("act", lambda e: e.activation(out=ebL[:], in_=P[:, 32:40], func=AF.Exp, scale=-1.0), reads=[P], writes=[ebL])
        for half in range(2):
            P = pg[pgi % 2]
            pgi += 1
            for kc in range(8):
                S.op("pe", lambda e: e.matmul(P[:], lhsT=hT[:, kc, :], rhs=win[:, kc, 1024 + half * 512:1024 + (half + 1) * 512],
                                              start=(kc == 0), stop=(kc == 7)), reads=[hT, win], writes=[P])
            S.op("dve", lambda e: e.tensor_tensor(out=vaug[:, half * 4:(half + 1) * 4, 0:128],
                                                  in0=P[:].rearrange("p (h n) -> p h n", n=128),
                                                  in1=wexp[:, half * 4:(half + 1) * 4].unsqueeze(2).to_broadcast([128, 4, 128]),
                                                  op=ALU.mult), reads=[P, wexp], writes=[vaug])
        S.op("pool", lambda e: e.tensor_copy(out=vaug[:, :, 128:129], in_=wexp[:].unsqueeze(2)), reads=[wexp], writes=[vaug])
        for half in range(2):
            P = pg[pgi % 2]
            pgi += 1
            for kc in range(8):
                S.op("pe", lambda e: e.matmul(P[:], lhsT=hT[:, kc, :], rhs=win[:, kc, 2048 + half * 512:2048 + (half + 1) * 512],
                                              start=(kc == 0), stop=(kc == 7)), reads=[hT, win], writes=[P])
            S.op("act", lambda e: e.activation(out=sig[:, half * 512:(half + 1) * 512], in_=P[:], func=AF.Sigmoid),
                 reads=[P], writes=[sig])
        for h in range(8):
            po, fc = (h % 2) * 64, h // 2
            S.op("pe", lambda e: e.matmul(pAT[h // 4][:, h % 4, :], lhsT=qkT[:, 4 + fc, :], rhs=qm[:, h, :],
                                          start=True, stop=True), reads=[qkT, qm], writes=[pAT[h // 4]])
        for half in range(2):
            S.op(("dve", "pool")[0], lambda e: e.tensor_tensor(out=ATs[:, half * 4:(half + 1) * 4, :], in0=pAT[half][:],
                                                  in1=utri[:].unsqueeze(1).to_broadcast([128, 4, 128]), op=ALU.mult),
                 reads=[pAT[half], utri], writes=[ATs])
        for h in range(8):
            po, fc = (h % 2) * 64, h // 2
            PB, sl = PO(h)
            S.op("pe", lambda e: e.matmul(PB[:, sl, 0:129], lhsT=ATs[:, h, :], rhs=vaug[:, h, :], start=True, stop=False),
                 reads=[ATs, vaug], writes=[PB])
            S.op("pe", lambda e: e.matmul(PB[:, sl, 0:129], lhsT=qm[:, h, :], rhs=Cbf[:, h, :],
                                          start=False, stop=True), reads=[qm, Cbf], writes=[PB])
        for bk in range(3):
            nh = 3 if bk < 2 else 2
            S.op("dve", lambda e: e.tensor_tensor(out=den[:, bk * 3:bk * 3 + nh].unsqueeze(2), in0=pOS[bk][:, 0:nh, 128:129],
                                                  in1=eb[:, bk * 3:bk * 3 + nh].unsqueeze(2), op=ALU.mult),
                 reads=[pOS[bk], eb], writes=[den])
        S.op("act", lambda e: e.activation(out=den[:], in_=den[:], func=AF.Abs), reads=[den], writes=[den])
        S.op("dve", lambda e: e.tensor_scalar_max(out=den[:], in0=den[:], scalar1=1.0), reads=[den], writes=[den])
        S.op("dve", lambda e: e.reciprocal(out=den[:], in_=den[:]), reads=[den], writes=[den])
        S.op("dve", lambda e: e.tensor_mul(out=den[:], in0=den[:], in1=eb[:]), reads=[den, eb], writes=[den])
        for bk in range(3):
            nh = 3 if bk < 2 else 2
            S.op("dve", lambda e: e.tensor_tensor(out=hh[:, bk * 3:bk * 3 + nh, :], in0=pOS[bk][:, 0:nh, 0:128],
                                                  in1=den[:, bk * 3:bk * 3 + nh].unsqueeze(2).to_broadcast([128, nh, 128]),
                                                  op=ALU.mult), reads=[pOS[bk], den], writes=[hh])
        for h in range(8):
            PB, sl = PO(h)
            fc = h // 2
            S.op("pe", lambda e: e.matmul(PB[:, sl, 0:129], lhsT=ktok[:, fc, :], rhs=vaug[:, h, :], start=True, stop=True),
                 reads=[ktok, vaug], writes=[PB])
        for bk in range(3):
            nh = 3 if bk < 2 else 2
            S.op("dve", lambda e: e.tensor_tensor(out=Cst[:, bk * 3:bk * 3 + nh, :], in0=pOS[bk][:, 0:nh, 0:129],
                                                  in1=Cst[:, bk * 3:bk * 3 + nh, :], op=ALU.add), reads=[pOS[bk], Cst], writes=[Cst])
        S.op("pool", lambda e: e.tensor_tensor(out=Cst[:], in0=Cst[:], in1=ebL[:].unsqueeze(2).to_broadcast([128, 8, 129]),
                                               op=ALU.mult), reads=[Cst, ebL], writes=[Cst])
        S.op("act", lambda e: e.copy(out=Cbf[:], in_=Cst[:]), reads=[Cst], writes=[Cbf])
        S.op("pool", lambda e: e.tensor_tensor(out=sq[:], in0=hh[:], in1=hh[:], op=ALU.mult), reads=[hh], writes=[sq])
        S.op("dve", lambda e: e.reduce_sum(out=hss[:], in_=sq[:], axis=AX.X), reads=[sq], writes=[hss])
        S.op("act", lambda e: e.activation(out=hss[:], in_=hss[:], func=AF.Sqrt, scale=1.0 / 128, bias=EPS), reads=[hss], writes=[hss])
        S.op("dve", lambda e: e.reciprocal(out=hss[:], in_=hss[:]), reads=[hss], writes=[hss])
        S.op("dve", lambda e: e.tensor_tensor(out=hh[:], in0=hh[:], in1=hss[:].unsqueeze(2).to_broadcast([128, 8, 128]), op=ALU.mult),
             reads=[hh, hss], writes=[hh])
        hh2 = hh[:].rearrange("p h n -> p (h n)")
        S.op("pool", lambda e: e.tensor_tensor(out=hh2, in0=hh2, in1=hg[:], op=ALU.mult), reads=[hh, hg], writes=[hh])
        S.op("dve", lambda e: e.tensor_tensor(out=hob[:], in0=hh2, in1=sig[:], op=ALU.mult), reads=[hh, sig], writes=[hob])
        emit_transpose8(S, hob, pT, hoT, identb)
        Y = yo[t % NB]
        for half in range(2):
            P = pg[pgi % 2]
            pgi += 1
            for kc in range(8):
                S.op("pe", lambda e: e.matmul(P[:], lhsT=hoT[:, kc, :], rhs=wout[:, kc, half * 512:(half + 1) * 512],
                                              start=(kc == 0), stop=(kc == 7)), reads=[hoT, wout], writes=[P])
            S.op("dve", lambda e: e.tensor_mul(out=Y[:, half * 512:(half + 1) * 512], in0=P[:], in1=gate[:, half * 512:(half + 1) * 512]),
                 reads=[P, gate], writes=[Y])
        S.op("pool", lambda e: e.tensor_add(out=Y[:], in0=Y[:], in1=X[:]), reads=[Y, X], writes=[Y])
        S.dma("sp", lambda e: e.dma_start(out=x_out[t * 128:(t + 1) * 128, :], in_=Y[:]), reads=[Y], writes=[])


def drain(gen):
    if gen is not None:
        for _ in gen:
            pass


def emit_headnorm(S, src, gsm, dst, sq, hss, nh, hd):
    s3 = src[:].rearrange("p (h d) -> p h d", d=hd)
    q3 = sq[:].rearrange("p (h d) -> p h d", d=hd)
    d3 = dst[:].rearrange("p (h d) -> p h d", d=hd)
    S.op("pool", lambda e: e.tensor_tensor(out=sq[:], in0=src[:], in1=src[:], op=ALU.mult), reads=[src], writes=[sq])
    S.op("dve", lambda e: e.reduce_sum(out=hss[:], in_=q3, axis=AX.X), reads=[sq], writes=[hss])
    S.op("act", lambda e: e.activation(out=hss[:], in_=hss[:], func=AF.Sqrt, scale=1.0 / hd, bias=EPS), reads=[hss], writes=[hss])
    S.op("dve", lambda e: e.reciprocal(out=hss[:], in_=hss[:]), reads=[hss], writes=[hss])
    S.op("dve", lambda e: e.tensor_tensor(out=q3, in0=s3, in1=hss[:].unsqueeze(2).to_broadcast([128, nh, hd]), op=ALU.mult),
         reads=[src, hss], writes=[sq])
    S.op("pool", lambda e: e.tensor_tensor(out=d3, in0=q3, in1=gsm[:].unsqueeze(1).to_broadcast([128, nh, hd]), op=ALU.mult),
         reads=[sq, gsm], writes=[dst])


def emit_sb(S, nc, x_in, x_out, kvg_row, kvw_d, kng_row, gmix_row, wq_d, qng_row, wout_d, mod_d, nseq, TPS,
            identb, sutri, sltri, ones):
    NH, HD = 16, 64
    kT_all = S.sbuf("kT_all", [128, 8, TPS * 128], BF16)
    v_all = S.sbuf("v_all", [128, TPS, D], BF16)
    xs = [S.sbuf(f"xs{i}", [128, D], F32) for i in range(2)]
    hb = S.sbuf("hb", [128, D], BF16)
    hT = S.sbuf("hT", [128, 8, 128], BF16)
    junk = S.sbuf("junk", [128, D], F32)
    st = S.sbuf("st", [128, 4], F32)
    grow = S.sbuf("grow", [128, D], F32)
    gmod = S.sbuf("gmod", [128, D], F32)
    shift = S.sbuf("shift", [128, D], F32)
    gsm = S.sbuf("gsm", [128, HD], F32)
    pf = S.sbuf("pf", [128, D], F32)
    sq = S.sbuf("sq", [128, D], F32)
    hss = S.sbuf("hss", [128, NH], F32)
    pb = S.sbuf("pb", [128, D], BF16)
    pT = S.psum("pT", [128, 8, 128], BF16)
    pg = S.psum("pg", [128, 512], F32)

    for sidx in range(nseq):
        t0 = sidx * TPS
        with S.scope():
            kvw = S.sbuf("kvw", [128, 8, 2048], BF16)
            kvw_v = kvw_d.rearrange("(k p) n -> p k n", p=128)
            for kc in range(8):
                S.dma("pool", lambda e: e.dma_start(out=kvw[:, kc, :], in_=kvw_v[:, kc, :]), writes=[kvw])
            load_bcast(S, "sp", grow, kvg_row)
            load_bcast(S, "sp", shift, mod_d[sidx, 12:13, :])
            load_bcast(S, "sp", gmod, mod_d[sidx, 13:14, :])
            load_bcast(S, "sp", gsm, kng_row)
            S.op("dve", lambda e: e.scalar_tensor_tensor(out=gmod[:], in0=gmod[:], scalar=1.0, in1=grow[:],
                                                         op0=ALU.add, op1=ALU.mult), reads=[gmod, grow], writes=[gmod])
            for t in range(TPS):
                X = xs[t % 2]
                S.dma("sp", lambda e: e.dma_start(out=X[:], in_=x_in[(t0 + t) * 128:(t0 + t + 1) * 128, :]), writes=[X])
                emit_norm_mod(S, X, gmod, shift, hb, st, junk)
                emit_transpose8(S, hb, pT, hT, identb)
                for ch in range(4):
                    for kc in range(8):
                        S.op("pe", lambda e: e.matmul(pg[:], lhsT=hT[:, kc, :], rhs=kvw[:, kc, ch * 512:(ch + 1) * 512],
                                                      start=(kc == 0), stop=(kc == 7)), reads=[hT, kvw], writes=[pg])
                    if ch < 2:
                        S.op("act", lambda e: e.copy(out=pf[:, ch * 512:(ch + 1) * 512], in_=pg[:]), reads=[pg], writes=[pf])
                    else:
                        S.op("act", lambda e: e.copy(out=v_all[:, t, (ch - 2) * 512:(ch - 1) * 512], in_=pg[:]), reads=[pg], writes=[v_all])
                emit_headnorm(S, pf, gsm, pb, sq, hss, NH, HD)
                for kc in range(8):
                    S.op("pe", lambda e: e.transpose(out=pT[:, kc, :], in_=pb[:, kc * 128:(kc + 1) * 128], identity=identb[:]),
                         reads=[pb, identb], writes=[pT])
                S.op("act", lambda e: e.copy(out=kT_all[:, :, t * 128:(t + 1) * 128], in_=pT[:]), reads=[pT], writes=[kT_all])
        with S.scope():
            wq = S.sbuf("wq", [128, 8, D], BF16)
            wout = S.sbuf("wout", [128, 8, D], BF16)
            wq_v = wq_d.rearrange("(k p) n -> p k n", p=128)
            wout_v = wout_d.rearrange("(k p) n -> p k n", p=128)
            for kc in range(8):
                S.dma("pool", lambda e: e.dma_start(out=wq[:, kc, :], in_=wq_v[:, kc, :]), writes=[wq])
                S.dma("pool", lambda e: e.dma_start(out=wout[:, kc, :], in_=wout_v[:, kc, :]), writes=[wout])
            gate = S.sbuf("gate", [128, D], F32)
            load_bcast(S, "sp", grow, gmix_row)
            load_bcast(S, "sp", shift, mod_d[sidx, 6:7, :])
            load_bcast(S, "sp", gmod, mod_d[sidx, 7:8, :])
            load_bcast(S, "sp", gate, mod_d[sidx, 8:9, :])
            load_bcast(S, "sp", gsm, qng_row)
            S.op("dve", lambda e: e.scalar_tensor_tensor(out=gmod[:], in0=gmod[:], scalar=1.0, in1=grow[:],
                                                         op0=ALU.add, op1=ALU.mult), reads=[gmod, grow], writes=[gmod])
            S.op("dve", lambda e: e.tensor_scalar_mul(out=gsm[:], in0=gsm[:], scalar1=HD ** -0.5), reads=[gsm], writes=[gsm])
            qm = S.sbuf("qm", [128, NH, 128], BF16)
            S.op("pool", lambda e: e.memset(qm[:], 0.0), writes=[qm])
            qm4 = qm[:].rearrange("p (f two) n -> p f two n", two=2)
            E = [S.sbuf(f"E{i}", [128, 512], F32) for i in range(4)]
            SP = [S.sbuf(f"SP{i}", [128, 512], F32) for i in range(4)]
            ARG = [S.sbuf(f"ARG{i}", [128, 512], F32) for i in range(2)]
            XB = [S.sbuf(f"XB{i}", [128, 512], F32) for i in range(2)]
            A = [S.sbuf(f"A{i}", [128, 512], BF16) for i in range(3)]
            SPcum = [S.sbuf(f"SPcum{i}", [128, 512], F32) for i in range(4)]
            yo = [S.sbuf(f"yo{i}", [128, D], F32) for i in range(2)]
            oT = S.sbuf("oT", [128, 8, 128], BF16)
            pz = [S.psum(f"pz{i}", [128, 4, 128], F32) for i in range(2)]
            pnb = [S.psum(f"pnb{i}", [128, 512], F32) for i in range(2)]
            po = [S.psum(f"po{i}", [128, 512], F32) for i in range(2)]
            m3 = sutri[:].unsqueeze(1).to_broadcast([128, 4, 128])
            for qt in range(TPS):
                X = xs[qt % 2]
                S.dma("sp", lambda e: e.dma_start(out=X[:], in_=x_in[(t0 + qt) * 128:(t0 + qt + 1) * 128, :]), writes=[X])
                emit_norm_mod(S, X, gmod, shift, hb, st, junk)
                emit_transpose8(S, hb, pT, hT, identb)
                for ch in range(2):
                    for kc in range(8):
                        S.op("pe", lambda e: e.matmul(pg[:], lhsT=hT[:, kc, :], rhs=wq[:, kc, ch * 512:(ch + 1) * 512],
                                                      start=(kc == 0), stop=(kc == 7)), reads=[hT, wq], writes=[pg])
                    S.op("act", lambda e: e.copy(out=pf[:, ch * 512:(ch + 1) * 512], in_=pg[:]), reads=[pg], writes=[pf])
                emit_headnorm(S, pf, gsm, pb, sq, hss, NH, HD)
                for kc in range(8):
                    S.op("pe", lambda e: e.transpose(out=pT[:, kc, :], in_=pb[:, kc * 128:(kc + 1) * 128], identity=identb[:]),
                         reads=[pb, identb], writes=[pT])
                S.op("act", lambda e: e.copy(out=qm4[0:64, :, 0, :], in_=pT[0:64, :, :]), reads=[pT], writes=[qm])
                S.op("act", lambda e: e.copy(out=qm4[64:128, :, 1, :], in_=pT[64:128, :, :]), reads=[pT], writes=[qm])
                items = [(kt, g) for kt in range(qt, -1, -1) for g in range(4)]
                NI = len(items)

                def S1(i):
                    kt, g = items[i]
                    Z = pz[i % 2]
                    for j in range(4):
                        h = 4 * g + j
                        S.op("pe", lambda e: e.matmul(Z[:, j, :], lhsT=kT_all[:, h // 2, kt * 128:(kt + 1) * 128], rhs=qm[:, h, :],
                                                      start=True, stop=True), reads=[kT_all, qm], writes=[Z])

                def S2(i):
                    kt, g = items[i]
                    Z, Eb, SPb = pz[i % 2], E[i % 4], SP[i % 4]
                    Z2 = Z[:].rearrange("p j n -> p (j n)")
                    S.op("act", lambda e: e.activation(out=Eb[:], in_=Z2, func=AF.Exp), reads=[Z], writes=[Eb])
                    S.op("act", lambda e: e.activation(out=SPb[:], in_=Eb[:], func=AF.Ln, bias=1.0), reads=[Eb], writes=[SPb])
                    if kt == qt:
                        S.op("dve", lambda e: e.tensor_tensor(out=SPb[:].rearrange("p (j n) -> p j n", n=128),
                                                              in0=SPb[:].rearrange("p (j n) -> p j n", n=128), in1=m3, op=ALU.mult),
                             reads=[SPb, sutri], writes=[SPb])

                def S3(i):
                    kt, g = items[i]
                    diag = (kt == qt)
                    NBp, SPb = pnb[i % 2], SP[i % 4]
                    S.op("pe", lambda e: e.matmul(NBp[:], lhsT=sltri[:], rhs=SPb[:], start=True, stop=diag), reads=[sltri, SPb], writes=[NBp])
                    if not diag:
                        S.op("pe", lambda e: e.matmul(NBp[:], lhsT=ones[:], rhs=SPcum[g][:], start=False, stop=True),
                             reads=[ones, SPcum[g]], writes=[NBp])
                    if kt > 0:
                        if diag:
                            S.op("pool", lambda e: e.tensor_copy(out=SPcum[g][:], in_=SPb[:]), reads=[SPb], writes=[SPcum[g]])
                        else:
                            S.op("pool", lambda e: e.tensor_add(out=SPcum[g][:], in0=SPcum[g][:], in1=SPb[:]), reads=[SPb, SPcum[g]], writes=[SPcum[g]])

                def S4(i):
                    kt, g = items[i]
                    diag = (kt == qt)
                    NBp, Eb, SPb, ARGb, Xb, Ab = pnb[i % 2], E[i % 4], SP[i % 4], ARG[i % 2], XB[i % 2], A[i % 3]
                    S.op("dve", lambda e: e.tensor_tensor(out=ARGb[:], in0=SPb[:], in1=NBp[:], op=ALU.add), reads=[SPb, NBp], writes=[ARGb])
                    S.op("act", lambda e: e.activation(out=Xb[:], in_=ARGb[:], func=AF.Exp, scale=-1.0), reads=[ARGb], writes=[Xb])
                    if diag:
                        S.op("dve", lambda e: e.tensor_tensor(out=ARGb[:], in0=Xb[:], in1=Eb[:], op=ALU.mult), reads=[Xb, Eb], writes=[ARGb])
                        S.op("pool", lambda e: e.tensor_tensor(out=Ab[:].rearrange("p (j n) -> p j n", n=128),
                                                               in0=ARGb[:].rearrange("p (j n) -> p j n", n=128), in1=m3, op=ALU.mult),
                             reads=[ARGb, sutri], writes=[Ab])
                    else:
                        S.op("dve", lambda e: e.tensor_tensor(out=Ab[:], in0=Xb[:], in1=Eb[:], op=ALU.mult), reads=[Xb, Eb], writes=[Ab])

                def S5(i):
                    kt, g = items[i]
                    Ab = A[i % 3]
                    for j in range(4):
                        h = 4 * g + j
                        PO = po[h // 8]
                        S.op("pe", lambda e: e.matmul(PO[:, (h % 8) * 64:(h % 8 + 1) * 64], lhsT=Ab[:, j * 128:(j + 1) * 128],
                                                      rhs=v_all[:, kt, h * 64:(h + 1) * 64], start=(kt == qt and h % 8 == 0), stop=(kt == 0)),
                             reads=[Ab, v_all], writes=[PO])

                for n in range(NI + 4):
                    if n < NI:
                        S1(n)
                    if 0 <= n - 1 < NI:
                        S2(n - 1)
                    if 0 <= n - 2 < NI:
                        S3(n - 2)
                    if 0 <= n - 3 < NI:
                        S4(n - 3)
                    if 0 <= n - 4 < NI:
                        S5(n - 4)
                for half in range(2):
                    S.op("act", lambda e: e.copy(out=pb[:, half * 512:(half + 1) * 512], in_=po[half][:]), reads=[po[half]], writes=[pb])
                emit_transpose8(S, pb, pT, oT, identb)
                Y = yo[qt % 2]
                for half in range(2):
                    for kc in range(8):
                        S.op("pe", lambda e: e.matmul(pg[:], lhsT=oT[:, kc, :], rhs=wout[:, kc, half * 512:(half + 1) * 512],
                                                      start=(kc == 0), stop=(kc == 7)), reads=[oT, wout], writes=[pg])
                    S.op("dve", lambda e: e.tensor_mul(out=Y[:, half * 512:(half + 1) * 512], in0=pg[:], in1=gate[:, half * 512:(half + 1) * 512]),
                         reads=[pg, gate], writes=[Y])
                S.op("pool", lambda e: e.tensor_add(out=Y[:], in0=Y[:], in1=X[:]), reads=[Y, X], writes=[Y])
                S.dma("sp", lambda e: e.dma_start(out=x_out[(t0 + qt) * 128:(t0 + qt + 1) * 128, :], in_=Y[:]), reads=[Y], writes=[])


NCORES = 8
SEQ = 2048
TPS = SEQ // 128
NSEQ = 2
NT = NSEQ * TPS
_NC_CACHE = {}


def build_program():
    nc = bass.Bass("TRN2", target_bir_lowering=False)

    def din(name, shape):
        return nc.dram_tensor(name, list(shape), F32, kind="ExternalInput").ap()

    x = din("x", [NT * 128, D])
    cT = din("cT", [128, 8, 2])
    ident = din("ident", [128, 128]); utri_d = din("utri", [128, 128]); sutri_d = din("sutri", [128, 128])
    sltri_d = din("sltri", [128, 128]); ones_d = din("ones", [128, 128]); iota_d = din("iota16", [128, 16])
    ada_w = din("ada_w", [2, D, 6 * D]); ada_b = din("ada_b", [2, 6 * D])
    norm_mix_g = din("norm_mix_g", [2, D]); norm_ffn_g = din("norm_ffn_g", [2, D])
    ma_w_in = din("ma_w_in", [D, 3088]); cwT = din("cwT", [128, 8, 4]); ma_b_if = din("ma_b_if", [1, 16])
    ma_hnorm_g = din("ma_hnorm_g", [1, D]); ma_w_out = din("ma_w_out", [D, D])
    kv_ada_w = din("kv_ada_w", [D, 2 * D]); kv_ada_b = din("kv_ada_b", [1, 2 * D]); kv_norm_g = din("kv_norm_g", [1, D])
    kv_w = din("kv_w", [D, 2 * D]); k_norm_g = din("k_norm_g", [1, 64])
    sb_w_q = din("sb_w_q", [D, D]); sb_q_norm_g = din("sb_q_norm_g", [1, 64]); sb_w_out = din("sb_w_out", [D, D])
    peer_w_q = din("peer_w_q", [2, D, 2 * D]); peer_sub_keys = din("peer_sub_keys", [2, 2, 128, 128])
    peer_u = din("peer_u", [2, 16384, D]); peer_v = din("peer_v", [2, 16384, D])
    pu_flat = peer_u.rearrange("l e d -> (l e) d")
    pv_flat = peer_v.rearrange("l e d -> (l e) d")
    out = nc.dram_tensor("out", [NT * 128, D], F32, kind="ExternalOutput").ap()
    mod = nc.dram_tensor("mod_scr", [2, 14, D], F32, kind="Internal").ap()
    xa = nc.dram_tensor("xa_scr", [NT * 128, D], F32, kind="Internal").ap()
    xb = nc.dram_tensor("xb_scr", [NT * 128, D], F32, kind="Internal").ap()
    xc = nc.dram_tensor("xc_scr", [NT * 128, D], F32, kind="Internal").ap()

    with ExitStack() as es:
        S = Sched(nc, es)
        identb = S.sbuf("identb", [128, 128], BF16)
        S.dma("pool", lambda e: e.dma_start(out=identb[:], in_=ident), writes=[identb])
        cs = {}
        for nm, d in (("utri", utri_d), ("sutri", sutri_d), ("sltri", sltri_d), ("ones", ones_d)):
            cs[nm] = S.sbuf(nm, [128, 128], F32)
            S.dma("sp", lambda e: e.dma_start(out=cs[nm][:], in_=d), writes=[cs[nm]])
        iota16 = S.sbuf("iota16", [128, 16], F32)
        S.dma("sp", lambda e: e.dma_start(out=iota16[:], in_=iota_d), writes=[iota16])
        puv = pu_flat.bitcast(BF16)
        with S.scope():
            emit_ada(S, nc, cT, ada_w, ada_b, kv_ada_w, kv_ada_b, mod)
        with S.scope():
            side = gen_convert_uv(S, nc, pu_flat, pv_flat, 32768)
            emit_mlstm(S, nc, x, xa, ma_w_in, cwT, ma_b_if, ma_hnorm_g, ma_w_out, norm_mix_g[0:1, :], mod, NT, TPS,
                       identb, cs["utri"], cs["ones"], side)
            drain(side)
        with S.scope():
            emit_peer(S, nc, xa, xb, peer_w_q[0], peer_sub_keys[0], puv, norm_ffn_g[0:1, :], mod, 3, NT, TPS, identb, iota16, 0)
        with S.scope():
            emit_sb(S, nc, xb, xc, kv_norm_g, kv_w, k_norm_g, norm_mix_g[1:2, :], sb_w_q, sb_q_norm_g, sb_w_out, mod, NSEQ, TPS,
                    identb, cs["sutri"], cs["sltri"], cs["ones"])
        with S.scope():
            emit_peer(S, nc, xc, out, peer_w_q[1], peer_sub_keys[1], puv, norm_ffn_g[1:2, :], mod, 9, NT, TPS, identb, iota16, 16384)
        S.barrier()
    return nc


def kernel(x, c, ada_w, ada_b, norm_mix_g, norm_ffn_g, ma_w_in, ma_conv_w, ma_b_if, ma_hnorm_g, ma_w_out,
           kv_ada_w, kv_ada_b, kv_norm_g, kv_w, k_norm_g, sb_w_q, sb_q_norm_g, sb_w_out,
           peer_w_q, peer_sub_keys, peer_u, peer_v):
    f = lambda a: np.ascontiguousarray(np.asarray(a, dtype=np.float32))
    x = f(x); c = f(c)
    one = np.ones((128, 128), np.float32)
    shared = {
        "ident": np.eye(128, dtype=np.float32), "utri": np.triu(one), "sutri": np.triu(one, 1), "sltri": np.tril(one, -1), "ones": one,
        "iota16": np.tile(np.arange(16, dtype=np.float32), (128, 1)),
        "ada_w": f(ada_w), "ada_b": f(ada_b), "norm_mix_g": f(norm_mix_g), "norm_ffn_g": f(norm_ffn_g),
        "ma_w_in": f(ma_w_in)[0], "cwT": np.ascontiguousarray(f(ma_conv_w)[0].reshape(4, 8, 128).transpose(2, 1, 0)),
        "ma_b_if": f(ma_b_if).reshape(1, 16), "ma_hnorm_g": f(ma_hnorm_g).reshape(1, D), "ma_w_out": f(ma_w_out)[0],
        "kv_ada_w": f(kv_ada_w), "kv_ada_b": f(kv_ada_b).reshape(1, 2 * D), "kv_norm_g": f(kv_norm_g).reshape(1, D),
        "kv_w": f(kv_w), "k_norm_g": f(k_norm_g).reshape(1, 64),
        "sb_w_q": f(sb_w_q)[0], "sb_q_norm_g": f(sb_q_norm_g).reshape(1, 64), "sb_w_out": f(sb_w_out)[0],
        "peer_w_q": f(peer_w_q), "peer_sub_keys": f(peer_sub_keys), "peer_u": f(peer_u), "peer_v": f(peer_v),
    }
    in_maps = []
    for i in range(NCORES):
        m = dict(shared)
        m["x"] = x[NSEQ * i:NSEQ * (i + 1)].reshape(NT * 128, D)
        m["cT"] = np.ascontiguousarray(c[NSEQ * i:NSEQ * (i + 1)].T.reshape(8, 128, NSEQ).transpose(1, 0, 2))
        in_maps.append(m)
    if "nc" not in _NC_CACHE:
        _NC_CACHE["nc"] = build_program()
    res = run_bass_kernel_spmd(_NC_CACHE["nc"], in_maps, core_ids=list(range(NCORES)))
    return np.concatenate([r["out"].reshape(NSEQ, SEQ, D) for r in res.results], axis=0)
```

```python
import numpy as np
from contextlib import ExitStack
import concourse.bass as bass
import concourse.mybir as mybir
from concourse.bass_utils import run_bass_kernel_spmd

F32 = mybir.dt.float32
BF16 = mybir.dt.bfloat16
U32 = mybir.dt.uint32
I32 = mybir.dt.int32
AF = mybir.ActivationFunctionType
ALU = mybir.AluOpType
AX = mybir.AxisListType


class Buf:
    __slots__ = ("name", "w", "r", "t")

    def __init__(self, name, t=None):
        self.name = name
        self.w = None
        self.r = {}
        self.t = t

    def __getitem__(self, idx):
        return self.t[idx]


class Sched:
    NDMA = 12

    def __init__(self, nc, es):
        self.nc = nc
        self.es = es
        self.engs = {"pe": nc.tensor, "act": nc.scalar, "dve": nc.vector, "pool": nc.gpsimd, "sp": nc.sync}
        self.sem = {}
        self.cnt = {}
        for k in ("pe", "act", "dve", "pool"):
            self.sem[k] = es.enter_context(nc.semaphore("s_" + k))
            self.cnt[k] = 0
        self.dq = {}
        for q in ("sp", "act", "pool"):
            sems = [es.enter_context(nc.semaphore(f"d_{q}{i}")) for i in range(self.NDMA)]
            self.dq[q] = {"sems": sems, "n": 0}
            for i in range(self.NDMA):
                self.sem[("dma", q, i)] = sems[i]
                self.cnt[("dma", q, i)] = 0
        self.waited = {e: {} for e in self.engs}
        self.nins = 0

    def sbuf(self, name, shape, dt):
        self.nins += 0
        self._uid = getattr(self, "_uid", 0) + 1
        name = f"sb{self._uid}_{name}"
        t = self.es.enter_context(self.nc.sbuf_tensor(name, list(shape), dt))
        return Buf(name, t)

    def psum(self, name, shape, dt=F32):
        self._uid = getattr(self, "_uid", 0) + 1
        name = f"ps{self._uid}_{name}"
        t = self.es.enter_context(self.nc.psum_tensor(name, list(shape), dt))
        return Buf(name, t)

    def view(self, name):
        return Buf(name)

    def scope(self):
        return _Scope(self)

    def _wait(self, engine, key, c):
        if c <= 0:
            return
        w = self.waited[engine]
        if w.get(key, 0) >= c:
            return
        self.engs[engine].wait_ge(self.sem[key], c)
        w[key] = c

    def _deps(self, engine, reads, writes):
        deps = {}
        for b in reads:
            if b.w is not None:
                k, c = b.w
                deps[k] = max(deps.get(k, 0), c)
        for b in writes:
            if b.w is not None:
                k, c = b.w
                deps[k] = max(deps.get(k, 0), c)
            for k, c in b.r.items():
                deps[k] = max(deps.get(k, 0), c)
        return deps

    def op(self, engine, fn, reads=(), writes=()):
        deps = self._deps(engine, reads, writes)
        for k, c in deps.items():
            if k == engine and engine == "pe":
                continue
            self._wait(engine, k, c)
        ins = fn(self.engs[engine])
        self.cnt[engine] += 1
        ins.then_inc(self.sem[engine], 1)
        me = (engine, self.cnt[engine])
        for b in reads:
            b.r[engine] = self.cnt[engine]
        for b in writes:
            b.w = me
            b.r = {}
        self.nins += 1
        return ins

    def dma(self, q, fn, reads=(), writes=()):
        d = self.dq[q]
        i = d["n"] % self.NDMA
        d["n"] += 1
        key = ("dma", q, i)
        deps = self._deps(q, reads, writes)
        deps[key] = max(deps.get(key, 0), self.cnt[key])
        for k, c in deps.items():
            self._wait(q, k, c)
        ins = fn(self.engs[q])
        self.cnt[key] += 16
        ins.then_inc(self.sem[key], 16)
        me = (key, self.cnt[key])
        for b in reads:
            b.r[key] = self.cnt[key]
        for b in writes:
            b.w = me
            b.r = {}
        self.nins += 1
        return ins

    def finish(self, bufs):
        for b in bufs:
            if b.w is not None:
                k, c = b.w
                for e in ("sp", "act", "pool", "dve", "pe"):
                    self._wait(e, k, c)

    def barrier(self):
        for e in self.engs:
            for k, c in self.cnt.items():
                if c > 0 and k != e:
                    self._wait(e, k, c)


class _Scope:
    def __init__(self, S):
        self.S = S

    def __enter__(self):
        self.old = self.S.es
        self.es2 = ExitStack()
        self.es2.__enter__()
        self.S.es = self.es2
        return self

    def __exit__(self, *a):
        self.S.barrier()
        self.S.es = self.old
        return self.es2.__exit__(*a)


EPS = 1e-6
D = 1024


def load_bcast(S, q, dst, row_ap):
    S.dma(q, lambda e: e.dma_start(out=dst[:], in_=row_ap.partition_broadcast(128)), writes=[dst])


def emit_norm_mod(S, xs, gmod, shift, hb, st, junk):
    S.op("act", lambda e: e.activation(out=junk[:], in_=xs[:], func=AF.Square, accum_out=st[:, 0:1]),
         reads=[xs], writes=[junk, st])
    S.op("act", lambda e: e.activation(out=st[:, 1:2], in_=st[:, 0:1], func=AF.Sqrt, scale=1.0 / D, bias=EPS),
         reads=[st], writes=[st])
    S.op("dve", lambda e: e.reciprocal(out=st[:, 2:3], in_=st[:, 1:2]), reads=[st], writes=[st])
    S.op("dve", lambda e: e.scalar_tensor_tensor(out=junk[:], in0=xs[:], scalar=st[:, 2:3], in1=gmod[:],
                                                 op0=ALU.mult, op1=ALU.mult), reads=[xs, st, gmod], writes=[junk])
    S.op("pool", lambda e: e.tensor_add(out=hb[:], in0=junk[:], in1=shift[:]), reads=[junk, shift], writes=[hb])


def emit_transpose8(S, hb, pT, hT, identb):
    for kc in range(8):
        S.op("pe", lambda e: e.transpose(out=pT[:, kc, :], in_=hb[:, kc * 128:(kc + 1) * 128], identity=identb[:]),
             reads=[hb, identb], writes=[pT])
    S.op("act", lambda e: e.copy(out=hT[:], in_=pT[:]), reads=[pT], writes=[hT])


def emit_peer(S, nc, x_in, x_out, wq_d, sk_d, puv_d, gffn_row, mod_d, mod_base, ntiles, tiles_per_batch,
              identb, iota16, idx_base=0):
    wq = S.sbuf("wq", [128, 8, 2048], BF16)
    wq_v = wq_d.rearrange("(k p) n -> p k n", p=128)
    for kc in range(8):
        S.dma("pool", lambda e: e.dma_start(out=wq[:, kc, :], in_=wq_v[:, kc, :]), writes=[wq])
    skn = S.sbuf("skn", [128, 2, 128], BF16)
    S.dma("pool", lambda e: e.dma_start(out=skn[:], in_=sk_d.rearrange("p n k -> n p k")), writes=[skn])
    skT = S.sbuf("skT", [128, 2, 128], BF16)
    pT = S.psum("pT", [128, 8, 128], BF16)
    for p in range(2):
        S.op("pe", lambda e: e.transpose(out=pT[:, p, :], in_=skn[:, p, :], identity=identb[:]),
             reads=[skn, identb], writes=[pT])
    S.op("act", lambda e: e.copy(out=skT[:], in_=pT[:, 0:2, :]), reads=[pT], writes=[skT])

    grow = S.sbuf("grow", [128, D], F32)
    load_bcast(S, "sp", grow, gffn_row)
    gmod = S.sbuf("gmod", [128, D], F32)
    shift = S.sbuf("shift", [128, D], F32)
    nbatch = (ntiles + tiles_per_batch - 1) // tiles_per_batch
    gates = [S.sbuf(f"gate{i}", [128, D], F32) for i in range(nbatch)]
    for i in range(nbatch):
        load_bcast(S, "sp", gates[i], mod_d[i, mod_base + 2:mod_base + 3, :])

    NB = 2
    xs = [S.sbuf(f"xs{i}", [128, D], F32) for i in range(NB)]
    hb = [S.sbuf(f"hb{i}", [128, D], BF16) for i in range(NB)]
    hT = [S.sbuf(f"hT{i}", [128, 8, 128], BF16) for i in range(NB)]
    junk = S.sbuf("junk", [128, D], F32)
    junkb = S.sbuf("junkb", [128, D], BF16)
    st = [S.sbuf(f"st{i}", [128, 4], F32) for i in range(NB)]
    qT = S.sbuf("qT", [128, 16, 128], BF16)
    pq = [S.psum(f"pq{i}", [128, 4, 128], F32) for i in range(2)]
    ps = [S.psum(f"ps{i}", [128, 4, 128], F32) for i in range(2)]
    pout = [S.psum(f"po{i}", [128, 512], F32) for i in range(2)]
    s_sb = S.sbuf("s_sb", [128, 16, 128], F32)
    s2 = S.sbuf("s2", [128, 16, 128], F32)
    top = S.sbuf("top", [128, 16, 16], F32)
    tix = S.sbuf("tix", [128, 16, 16], U32)
    tixf = S.sbuf("tixf", [128, 16, 16], F32)
    cand = S.sbuf("cand", [128, 8, 256], F32)
    cand2 = S.sbuf("cand2", [128, 8, 256], F32)
    pos = S.sbuf("pos", [128, 8, 16], U32)
    posf = S.sbuf("posf", [128, 8, 16], F32)
    ai = S.sbuf("ai", [128, 8, 16], I32)
    af = S.sbuf("af", [128, 8, 16], F32)
    bf_ = S.sbuf("bf_", [128, 8, 16], F32)
    ia = S.sbuf("ia", [128, 8, 16], F32)
    ja = S.sbuf("ja", [128, 8, 16], F32)
    oh = S.sbuf("oh", [128, 2048], F32)
    g = S.sbuf("g", [128, 8, 16], F32)
    ge = S.sbuf("ge", [128, 8, 16], F32)
    gsum = S.sbuf("gsum", [128, 8], F32)
    ef = S.sbuf("ef", [128, 128], F32)
    eidx = [S.sbuf(f"eidx{i}", [128, 128], I32) for i in range(NB)]
    gs = [S.sbuf(f"gs{i}", [128, 128], F32) for i in range(NB)]
    act = S.sbuf("act", [128, 128], F32)
    wv = S.sbuf("wv", [128, 128], F32)
    NG = 14
    ug = [S.sbuf(f"ug{i}", [128, 2 * D], BF16) for i in range(NG)]
    actg = [S.sbuf(f"actg{i}", [128, 4], F32) for i in range(4)]
    wvg = [S.sbuf(f"wvg{i}", [128, 4], F32) for i in range(4)]
    ND = 8
    dg = [S.sbuf(f"dg{i}", [128, 128], BF16) for i in range(ND)]
    yo = [S.sbuf(f"yo{i}", [128, D], F32) for i in range(NB)]
    gi = 0
    di = 0

    def stage_I(t):
        b = t // tiles_per_batch
        i2 = t % NB
        if t % tiles_per_batch == 0:
            load_bcast(S, "sp", shift, mod_d[b, mod_base + 0:mod_base + 1, :])
            load_bcast(S, "sp", gmod, mod_d[b, mod_base + 1:mod_base + 2, :])
            S.op("dve", lambda e: e.scalar_tensor_tensor(out=gmod[:], in0=gmod[:], scalar=1.0, in1=grow[:],
                                                         op0=ALU.add, op1=ALU.mult), reads=[gmod, grow], writes=[gmod])
        X, HB, HT, ST = xs[i2], hb[i2], hT[i2], st[i2]
        S.dma("sp", lambda e: e.dma_start(out=X[:], in_=x_in[t * 128:(t + 1) * 128, :]), writes=[X])
        yield
        emit_norm_mod(S, X, gmod, shift, HB, ST, junk)
        yield
        emit_transpose8(S, HB, pT, HT, identb)
        yield
        for gq in range(4):
            P = pq[gq % 2]
            for j in range(4):
                hp = gq * 4 + j
                for kc in range(8):
                    S.op("pe", lambda e: e.matmul(P[:, j, :], lhsT=wq[:, kc, hp * 128:(hp + 1) * 128], rhs=HT[:, kc, :],
                                                  start=(kc == 0), stop=(kc == 7)), reads=[wq, HT], writes=[P])
            S.op("act", lambda e: e.copy(out=qT[:, gq * 4:(gq + 1) * 4, :], in_=P[:]), reads=[P], writes=[qT])
            yield
        for gq in range(4):
            P = ps[gq % 2]
            for j in range(4):
                hp = gq * 4 + j
                S.op("pe", lambda e: e.matmul(P[:, j, :], lhsT=qT[:, hp, :], rhs=skT[:, hp % 2, :], start=True, stop=True),
                     reads=[qT, skT], writes=[P])
            S.op("act", lambda e: e.copy(out=s_sb[:, gq * 4:(gq + 1) * 4, :], in_=P[:]), reads=[P], writes=[s_sb])
            yield
        for hp in range(16):
            S.op("dve", lambda e: e.max(out=top[:, hp, 0:8], in_=s_sb[:, hp, :]), reads=[s_sb], writes=[top])
            S.op("dve", lambda e: e.max_index(out=tix[:, hp, 0:8], in_max=top[:, hp, 0:8], in_values=s_sb[:, hp, :]),
                 reads=[s_sb, top], writes=[tix])
            S.op("dve", lambda e: e.match_replace(out=s2[:, hp, :], in_to_replace=top[:, hp, 0:8], in_values=s_sb[:, hp, :],
                                                  imm_value=-1e30), reads=[s_sb, top], writes=[s2])
            S.op("dve", lambda e: e.max(out=top[:, hp, 8:16], in_=s2[:, hp, :]), reads=[s2], writes=[top])
            S.op("dve", lambda e: e.max_index(out=tix[:, hp, 8:16], in_max=top[:, hp, 8:16], in_values=s2[:, hp, :]),
                 reads=[s2, top], writes=[tix])
            yield
        S.op("dve", lambda e: e.tensor_copy(out=tixf[:], in_=tix[:]), reads=[tix], writes=[tixf])
        top4 = top[:].rearrange("q (h p) a -> q h p a", p=2)
        tix4 = tixf[:].rearrange("q (h p) a -> q h p a", p=2)
        c4 = cand[:].rearrange("q h (a b) -> q h a b", b=16)
        S.op("dve", lambda e: e.tensor_tensor(out=c4, in0=top4[:, :, 0, :].unsqueeze(3).to_broadcast([128, 8, 16, 16]),
                                              in1=top4[:, :, 1, :].unsqueeze(2).to_broadcast([128, 8, 16, 16]), op=ALU.add),
             reads=[top], writes=[cand])
        for h in range(8):
            S.op("dve", lambda e: e.max(out=g[:, h, 0:8], in_=cand[:, h, :]), reads=[cand], writes=[g])
            S.op("dve", lambda e: e.match_replace(out=cand2[:, h, :], in_to_replace=g[:, h, 0:8], in_values=cand[:, h, :],
                                                  imm_value=-1e30), reads=[cand, g], writes=[cand2])
            S.op("dve", lambda e: e.max(out=g[:, h, 8:16], in_=cand2[:, h, :]), reads=[cand2], writes=[g])
            yield
        for h in range(8):
            S.op("dve", lambda e: e.max_index(out=pos[:, h, 0:8], in_max=g[:, h, 0:8], in_values=cand[:, h, :]),
                 reads=[cand, g], writes=[pos])
            S.op("dve", lambda e: e.max_index(out=pos[:, h, 8:16], in_max=g[:, h, 8:16], in_values=cand2[:, h, :]),
                 reads=[cand2, g], writes=[pos])
            yield
        S.op("dve", lambda e: e.tensor_copy(out=posf[:], in_=pos[:]), reads=[pos], writes=[posf])
        S.op("dve", lambda e: e.tensor_scalar(out=ai[:], in0=posf[:], scalar1=0.0625, scalar2=-0.46875, op0=ALU.mult, op1=ALU.add),
             reads=[posf], writes=[ai])
        S.op("dve", lambda e: e.tensor_copy(out=af[:], in_=ai[:]), reads=[ai], writes=[af])
        S.op("dve", lambda e: e.scalar_tensor_tensor(out=bf_[:], in0=af[:], scalar=-16.0, in1=posf[:], op0=ALU.mult, op1=ALU.add),
             reads=[af, posf], writes=[bf_])
        yield
        oh4 = oh[:].rearrange("q (h k a) -> q h k a", k=16, a=16)
        io4 = iota16[:].unsqueeze(1).unsqueeze(1).to_broadcast([128, 8, 16, 16])
        for (src, pidx, dst) in ((af, 0, ia), (bf_, 1, ja)):
            S.op("dve", lambda e: e.tensor_tensor(out=oh4, in0=src[:].unsqueeze(3).to_broadcast([128, 8, 16, 16]), in1=io4, op=ALU.is_equal),
                 reads=[src, iota16], writes=[oh])
            S.op("dve", lambda e: e.tensor_tensor(out=oh4, in0=oh4, in1=tix4[:, :, pidx, :].unsqueeze(2).to_broadcast([128, 8, 16, 16]), op=ALU.mult),
                 reads=[oh, tixf], writes=[oh])
            S.op("dve", lambda e: e.reduce_sum(out=dst[:], in_=oh4, axis=AX.X), reads=[oh], writes=[dst])
            yield
        S.op("dve", lambda e: e.scalar_tensor_tensor(out=ef[:].rearrange("q (h k) -> q h k", k=16), in0=ia[:], scalar=128.0, in1=ja[:],
                                                     op0=ALU.mult, op1=ALU.add), reads=[ia, ja], writes=[ef])
        EI, GS = eidx[i2], gs[i2]
        S.op("dve", lambda e: e.tensor_scalar(out=ef[:], in0=ef[:], scalar1=16383.0, scalar2=0.0, op0=ALU.min, op1=ALU.max),
             reads=[ef], writes=[ef])
        if idx_base:
            S.op("dve", lambda e: e.tensor_scalar_add(out=ef[:], in0=ef[:], scalar1=float(idx_base)), reads=[ef], writes=[ef])
        S.op("dve", lambda e: e.tensor_copy(out=EI[:], in_=ef[:]), reads=[ef], writes=[EI])
        S.op("dve", lambda e: e.tensor_tensor(out=ge[:], in0=g[:], in1=g[:, :, 0:1].to_broadcast([128, 8, 16]), op=ALU.subtract),
             reads=[g], writes=[ge])
        S.op("act", lambda e: e.activation(out=ge[:], in_=ge[:], func=AF.Exp), reads=[ge], writes=[ge])
        S.op("dve", lambda e: e.reduce_sum(out=gsum[:], in_=ge[:], axis=AX.X), reads=[ge], writes=[gsum])
        S.op("dve", lambda e: e.reciprocal(out=gsum[:], in_=gsum[:]), reads=[gsum], writes=[gsum])
        S.op("dve", lambda e: e.tensor_tensor(out=GS[:].rearrange("q (h k) -> q h k", k=16), in0=ge[:],
                                              in1=gsum[:].unsqueeze(2).to_broadcast([128, 8, 16]), op=ALU.mult),
             reads=[ge, gsum], writes=[GS])
    def stage_UV(t, nxt):
        nonlocal gi, di
        i2 = t % NB
        X, HB, EI, GS = xs[i2], hb[i2], eidx[i2], gs[i2]
        gate = gates[t // tiles_per_batch]
        for grp in range(32):
            AG, WG = actg[grp % 4], wvg[grp % 4]
            UVs = []
            for j in range(4):
                s = grp * 4 + j
                UV = ug[gi % NG]
                gi += 1
                UVs.append(UV)
                S.dma("pool", lambda e: e.indirect_dma_start(out=UV[:], out_offset=None, in_=puv_d,
                                                             in_offset=bass.IndirectOffsetOnAxis(ap=EI[:, s:s + 1], axis=0)),
                      reads=[EI], writes=[UV])
                S.op("dve", lambda e: e.scalar_tensor_tensor(out=junkb[:], in0=UV[:, 0:D], scalar=1.0, in1=HB[:], op0=ALU.mult,
                                                             op1=ALU.mult, accum_out=AG[:, j:j + 1]),
                     reads=[UV, HB], writes=[junkb, AG])
            S.op("act", lambda e: e.activation(out=WG[:], in_=AG[:], func=AF.Gelu), reads=[AG], writes=[WG])
            for j in range(4):
                S.op("act", lambda e: e.mul(out=WG[:, j:j + 1], in_=WG[:, j:j + 1], mul=GS[:, grp * 4 + j:grp * 4 + j + 1]),
                     reads=[WG, GS], writes=[WG])
            for j in range(4):
                s = grp * 4 + j
                DG = dg[di % ND]
                di += 1
                S.op("act", lambda e: e.mul(out=DG[:], in_=identb[:], mul=WG[:, j:j + 1]), reads=[identb, WG], writes=[DG])
                for hh in range(2):
                    S.op("pe", lambda e: e.matmul(pout[hh][:], lhsT=DG[:], rhs=UVs[j][:, D + hh * 512:D + (hh + 1) * 512],
                                                  start=(s == 0), stop=(s == 127)), reads=[DG, UVs[j]], writes=[pout[hh]])
            for _ in range(6):
                next(nxt, None)
        Y = yo[i2]
        for hh in range(2):
            S.op("dve", lambda e: e.tensor_mul(out=Y[:, hh * 512:(hh + 1) * 512], in0=pout[hh][:],
                                               in1=gate[:, hh * 512:(hh + 1) * 512]), reads=[pout[hh], gate], writes=[Y])
        S.op("pool", lambda e: e.tensor_add(out=Y[:], in0=Y[:], in1=X[:]), reads=[Y, X], writes=[Y])
        S.dma("sp", lambda e: e.dma_start(out=x_out[t * 128:(t + 1) * 128, :], in_=Y[:]), reads=[Y], writes=[])
        for _ in nxt:
            pass

    g0 = stage_I(0)
    for _ in g0:
        pass
    for t in range(ntiles):
        nxt = stage_I(t + 1) if t + 1 < ntiles else iter(())
        stage_UV(t, nxt)


def emit_convert(S, nc, pairs, rows):
    NBUF = 4
    bufs = [S.sbuf(f"cv{i}", [128, 4, D], BF16) for i in range(NBUF)]
    i = 0
    for src, dst in pairs:
        sv = src.rearrange("(c p r) d -> c p r d", p=128, r=4)
        dv = dst.rearrange("(c p r) d -> c p r d", p=128, r=4)
        for c in range(rows // 512):
            B = bufs[i % NBUF]
            S.dma("pool", lambda e: e.dma_start(out=B[:], in_=sv[c]), writes=[B])
            S.dma(("sp", "act")[i % 2], lambda e: e.dma_start(out=dv[c], in_=B[:]), reads=[B], writes=[])
            i += 1


def emit_convert_inplace(S, nc, tabs, rows):
    NBUF = 4
    bufs = [S.sbuf(f"cv{i}", [128, 4, D], BF16) for i in range(NBUF)]
    views = []
    i = 0
    for tab in tabs:
        sv = tab.rearrange("(c p r) d -> c p r d", p=128, r=4)
        v16 = tab.bitcast(BF16).rearrange("n (two d) -> (n two) d", two=2)
        dv = v16[0:rows, :].rearrange("(c p r) d -> c p r d", p=128, r=4)
        views.append(v16)
        for c in range(rows // 512):
            B = bufs[i % NBUF]
            S.dma("pool", lambda e: e.dma_start(out=B[:], in_=sv[c]), writes=[B])
            S.dma(("sp", "act")[i % 2], lambda e: e.dma_start(out=dv[c], in_=B[:]), reads=[B], writes=[])
            i += 1
    return views


def emit_convert_uv(S, nc, tab_u, tab_v, rows):
    NBUF = 3
    bufs = [S.sbuf(f"cuv{i}", [128, 4, 2 * D], BF16) for i in range(NBUF)]
    su = tab_u.rearrange("(c p r) d -> c p r d", p=128, r=4)
    sv = tab_v.rearrange("(c p r) d -> c p r d", p=128, r=4)
    uv = tab_u.bitcast(BF16)
    dv = uv.rearrange("(c p r) d -> c p r d", p=128, r=4)
    for c in range(rows // 512):
        B = bufs[c % NBUF]
        S.dma("pool", lambda e: e.dma_start(out=B[:, :, 0:D], in_=su[c]), writes=[B])
        S.dma("pool", lambda e: e.dma_start(out=B[:, :, D:2 * D], in_=sv[c]), writes=[B])
        S.dma(("sp", "act")[c % 2], lambda e: e.dma_start(out=dv[c], in_=B[:]), reads=[B], writes=[])
    return uv


def gen_convert_uv(S, nc, tab_u, tab_v, rows):
    NBUF = 3
    bufs = [S.sbuf(f"cuv{i}", [128, 4, 2 * D], BF16) for i in range(NBUF)]
    su = tab_u.rearrange("(c p r) d -> c p r d", p=128, r=4)
    sv = tab_v.rearrange("(c p r) d -> c p r d", p=128, r=4)
    uv = tab_u.bitcast(BF16)
    dv = uv.rearrange("(c p r) d -> c p r d", p=128, r=4)
    for c in range(rows // 512):
        B = bufs[c % NBUF]
        S.dma("pool", lambda e: e.dma_start(out=B[:, :, 0:D], in_=su[c]), writes=[B])
        S.dma("pool", lambda e: e.dma_start(out=B[:, :, D:2 * D], in_=sv[c]), writes=[B])
        S.dma("sp", lambda e: e.dma_start(out=dv[c], in_=B[:]), reads=[B], writes=[])
        yield


import math


def emit_ada(S, nc, cT_d, ada_w_d, ada_b_d, kvw_d, kvb_d, mod_d):
    cT = S.sbuf("cT", [128, 8, 2], F32)
    S.dma("sp", lambda e: e.dma_start(out=cT[:], in_=cT_d), writes=[cT])
    cs = S.sbuf("cs", [128, 8, 2], F32)
    S.op("act", lambda e: e.activation(out=cs[:], in_=cT[:], func=AF.Silu), reads=[cT], writes=[cs])
    wch = [S.sbuf(f"wch{i}", [128, 8, 512], F32) for i in range(2)]
    bch = [S.sbuf(f"bch{i}", [2, 512], F32) for i in range(2)]
    och = [S.sbuf(f"och{i}", [2, 512], F32) for i in range(2)]
    pa = [S.psum(f"pada{i}", [128, 512], F32) for i in range(2)]
    jobs = []
    for l in range(2):
        for ch in range(12):
            jobs.append((ada_w_d[l], ada_b_d[l:l + 1, :], ch, 6 * l + ch // 2, ch % 2))
    for ch in range(4):
        jobs.append((kvw_d, kvb_d, ch, 12 + ch // 2, ch % 2))
    for i, (w_d, b_d, ch, row, half) in enumerate(jobs):
        W, B, O, P = wch[i % 2], bch[i % 2], och[i % 2], pa[i % 2]
        wv = w_d.rearrange("(k p) n -> p k n", p=128)
        q = ("sp", "act")[i % 2]
        S.dma(q, lambda e: e.dma_start(out=W[:], in_=wv[:, :, ch * 512:(ch + 1) * 512]), writes=[W])
        S.dma(q, lambda e: e.dma_start(out=B[:], in_=b_d[:, ch * 512:(ch + 1) * 512].partition_broadcast(2)), writes=[B])
        for kc in range(8):
            S.op("pe", lambda e: e.matmul(P[0:2, :], lhsT=cs[:, kc, :], rhs=W[:, kc, :], start=(kc == 0), stop=(kc == 7)),
                 reads=[cs, W], writes=[P])
        S.op("dve", lambda e: e.tensor_add(out=O[:], in0=P[0:2, :], in1=B[:]), reads=[P, B], writes=[O])
        S.dma(q, lambda e: e.dma_start(out=mod_d[:, row, half * 512:(half + 1) * 512], in_=O[:]), reads=[O], writes=[])


def emit_mlstm(S, nc, x_in, x_out, win_d, cwT_d, bif_d, hg_d, wout_d, gmix_row, mod_d, ntiles, tiles_per_seq,
               identb, utri, ones, side=None):
    win = S.sbuf("win", [128, 8, 3088], BF16)
    win_v = win_d.rearrange("(k p) n -> p k n", p=128)
    for kc in range(8):
        S.dma("pool", lambda e: e.dma_start(out=win[:, kc, 0:2048], in_=win_v[:, kc, 0:2048]), writes=[win])
        S.dma("pool", lambda e: e.dma_start(out=win[:, kc, 2048:3088], in_=win_v[:, kc, 2048:3088]), writes=[win])
    wout = S.sbuf("wout", [128, 8, 1024], BF16)
    wout_v = wout_d.rearrange("(k p) n -> p k n", p=128)
    for kc in range(8):
        S.dma("pool", lambda e: e.dma_start(out=wout[:, kc, :], in_=wout_v[:, kc, :]), writes=[wout])
    cwT = S.sbuf("cwT", [128, 8, 4], F32)
    S.dma("sp", lambda e: e.dma_start(out=cwT[:], in_=cwT_d), writes=[cwT])
    bif = S.sbuf("bif", [128, 16], F32)
    load_bcast(S, "sp", bif, bif_d)
    hg = S.sbuf("hg", [128, D], F32)
    load_bcast(S, "sp", hg, hg_d)
    grow = S.sbuf("grow", [128, D], F32)
    load_bcast(S, "sp", grow, gmix_row)
    gmod = S.sbuf("gmod", [128, D], F32)
    shift = S.sbuf("shift", [128, D], F32)
    gate = S.sbuf("gate", [128, D], F32)

    NB = 2
    xs = [S.sbuf(f"xs{i}", [128, D], F32) for i in range(NB)]
    hb = S.sbuf("hb", [128, D], BF16)
    hT = S.sbuf("hT", [128, 8, 128], BF16)
    junk = S.sbuf("junk", [128, D], F32)
    st = S.sbuf("st", [128, 4], F32)
    cb = S.sbuf("cb", [128, 8, 131], F32)
    cacc = S.sbuf("cacc", [128, 8, 128], F32)
    ctmp = S.sbuf("ctmp", [128, 8, 128], F32)
    qkT = S.sbuf("qkT", [128, 8, 128], BF16)
    ktok = S.sbuf("ktok", [128, 4, 128], BF16)
    qm = S.sbuf("qm", [128, 8, 128], BF16)
    S.op("pool", lambda e: e.memset(qm[:], 0.0), writes=[qm])
    gts = S.sbuf("gts", [128, 16], F32)
    spf = S.sbuf("spf", [128, 8], F32)
    wexp = S.sbuf("wexp", [128, 8], F32)
    eb = S.sbuf("eb", [128, 8], F32)
    ebL = S.sbuf("ebL", [128, 8], F32)
    vaug = S.sbuf("vaug", [128, 8, 129], BF16)
    sig = S.sbuf("sig", [128, D], F32)
    ATs = S.sbuf("ATs", [128, 8, 128], BF16)
    Cst = S.sbuf("Cst", [128, 8, 129], F32)
    Cbf = S.sbuf("Cbf", [128, 8, 129], BF16)
    den = S.sbuf("den", [128, 8], F32)
    hh = S.sbuf("hh", [128, 8, 128], F32)
    sq = S.sbuf("sq", [128, 8, 128], F32)
    hss = S.sbuf("hss", [128, 8], F32)
    hob = S.sbuf("hob", [128, D], BF16)
    hoT = S.sbuf("hoT", [128, 8, 128], BF16)
    yo = [S.sbuf(f"yo{i}", [128, D], F32) for i in range(NB)]

    pT = S.psum("pT", [128, 8, 128], BF16)
    pg = [S.psum(f"pg{i}", [128, 512], F32) for i in range(2)]
    pAT = [S.psum(f"pAT{i}", [128, 4, 128], F32) for i in range(2)]
    pOS = [S.psum(f"pOS{i}", [128, 3, 160], F32) for i in range(3)]
    pgi = 0

    def PO(h):
        return pOS[h // 3], h % 3

    LN8 = math.log(0.125)
    for t in range(ntiles):
        sidx = t // tiles_per_seq
        first = (t % tiles_per_seq == 0)
        X = xs[t % NB]
        if first:
            load_bcast(S, "sp", shift, mod_d[sidx, 0:1, :])
            load_bcast(S, "sp", gmod, mod_d[sidx, 1:2, :])
            load_bcast(S, "sp", gate, mod_d[sidx, 2:3, :])
            S.op("dve", lambda e: e.scalar_tensor_tensor(out=gmod[:], in0=gmod[:], scalar=1.0, in1=grow[:],
                                                         op0=ALU.add, op1=ALU.mult), reads=[gmod, grow], writes=[gmod])
            S.op("pool", lambda e: e.memset(cb[:, :, 0:3], 0.0), writes=[cb])
            S.op("pool", lambda e: e.memset(Cst[:], 0.0), writes=[Cst])
            S.op("pool", lambda e: e.memset(Cbf[:], 0.0), writes=[Cbf])
        S.dma("sp", lambda e: e.dma_start(out=X[:], in_=x_in[t * 128:(t + 1) * 128, :]), writes=[X])
        if side is not None:
            next(side, None)
            next(side, None)
        emit_norm_mod(S, X, gmod, shift, hb, st, junk)
        emit_transpose8(S, hb, pT, hT, identb)
        for half in range(2):
            P = pg[pgi % 2]
            pgi += 1
            for j in range(4):
                fc = half * 4 + j
                for kc in range(8):
                    S.op("pe", lambda e: e.matmul(P[:, j * 128:(j + 1) * 128], lhsT=win[:, kc, fc * 128:(fc + 1) * 128],
                                                  rhs=hT[:, kc, :], start=(kc == 0), stop=(kc == 7)),
                         reads=[win, hT], writes=[P])
            S.op("act", lambda e: e.copy(out=cb[:, half * 4:(half + 1) * 4, 3:131],
                                         in_=P[:].rearrange("p (j n) -> p j n", n=128)), reads=[P], writes=[cb])
        def cw(w):
            return cwT[:, :, w:w + 1].to_broadcast([128, 8, 128])
        S.op("dve", lambda e: e.tensor_tensor(out=cacc[:], in0=cb[:, :, 3:131], in1=cw(3), op=ALU.mult), reads=[cb, cwT], writes=[cacc])
        for w in range(3):
            S.op("pool", lambda e: e.tensor_tensor(out=ctmp[:], in0=cb[:, :, w:w + 128], in1=cw(w), op=ALU.mult),
                 reads=[cb, cwT], writes=[ctmp])
            S.op("dve", lambda e: e.tensor_add(out=cacc[:], in0=cacc[:], in1=ctmp[:]), reads=[cacc, ctmp], writes=[cacc])
        S.op("act", lambda e: e.activation(out=qkT[:, 4:8, :], in_=cacc[:, 4:8, :], func=AF.Silu), reads=[cacc], writes=[qkT])
        qm4 = qm[:].rearrange("p (f two) n -> p f two n", two=2)
        S.op("act", lambda e: e.activation(out=qm4[0:64, :, 0, :], in_=cacc[0:64, 0:4, :], func=AF.Silu), reads=[cacc], writes=[qm])
        S.op("act", lambda e: e.activation(out=qm4[64:128, :, 1, :], in_=cacc[64:128, 0:4, :], func=AF.Silu), reads=[cacc], writes=[qm])
        S.op("pool", lambda e: e.tensor_copy(out=cb[:, :, 0:3], in_=cb[:, :, 128:131]), reads=[cb], writes=[cb])
        for j in range(4):
            S.op("pe", lambda e: e.transpose(out=pT[:, j, :], in_=qkT[:, 4 + j, :], identity=identb[:]),
                 reads=[qkT, identb], writes=[pT])
        S.op("act", lambda e: e.copy(out=ktok[:], in_=pT[:, 0:4, :]), reads=[pT], writes=[ktok])
        P = pg[pgi % 2]
        pgi += 1
        for kc in range(8):
            S.op("pe", lambda e: e.matmul(P[:, 0:16], lhsT=hT[:, kc, :], rhs=win[:, kc, 3072:3088], start=(kc == 0), stop=(kc == 7)),
                 reads=[hT, win], writes=[P])
        S.op("dve", lambda e: e.tensor_add(out=gts[:], in0=P[:, 0:16], in1=bif[:]), reads=[P, bif], writes=[gts])
        S.op("act", lambda e: e.activation(out=spf[:], in_=gts[:, 8:16], func=AF.Exp, scale=-1.0), reads=[gts], writes=[spf])
        S.op("act", lambda e: e.activation(out=spf[:], in_=spf[:], func=AF.Ln, bias=1.0), reads=[spf], writes=[spf])
        S.op("pe", lambda e: e.matmul(P[:, 16:24], lhsT=utri[:], rhs=spf[:], start=True, stop=True), reads=[utri, spf], writes=[P])
        S.op("pe", lambda e: e.matmul(P[:, 32:40], lhsT=ones[:], rhs=spf[:], start=True, stop=True), reads=[ones, spf], writes=[P])
        S.op("dve", lambda e: e.tensor_add(out=wexp[:], in0=P[:, 16:24], in1=gts[:, 0:8]), reads=[P, gts], writes=[wexp])
        S.op("act", lambda e: e.activation(out=wexp[:], in_=wexp[:], func=AF.Exp), reads=[wexp], writes=[wexp])
        S.op("act", lambda e: e.activation(out=eb[:], in_=P[:, 16:24], func=AF.Exp, scale=-1.0, bias=LN8), reads=[P], writes=[eb])
        S.op("act", lambda e: e.activation(out=ebL[:], in_=P[:, 32:40], func=AF.Exp, scale=-1.0), reads=[P], writes=[ebL])
        for half in range(2):
            P = pg[pgi % 2]
            pgi += 1
            for kc in range(8):
                S.op("pe", lambda e: e.matmul(P[:], lhsT=hT[:, kc, :], rhs=win[:, kc, 1024 + half * 512:1024 + (half + 1) * 512],
                                              start=(kc == 0), stop=(kc == 7)), reads=[hT, win], writes=[P])
            S.op("dve", lambda e: e.tensor_tensor(out=vaug[:, half * 4:(half + 1) * 4, 0:128],
                                                  in0=P[:].rearrange("p (h n) -> p h n", n=128),
                                                  in1=wexp[:, half * 4:(half + 1) * 4].unsqueeze(2).to_broadcast([128, 4, 128]),
                                                  op=ALU.mult), reads=[P, wexp], writes=[vaug])
        S.op("pool", lambda e: e.tensor_copy(out=vaug[:, :, 128:129], in_=wexp[:].unsqueeze(2)), reads=[wexp], writes=[vaug])
        for half in range(2):
            P = pg[pgi % 2]
            pgi += 1
            for kc in range(8):
                S.op("pe", lambda e: e.matmul(P[:], lhsT=hT[:, kc, :], rhs=win[:, kc, 2048 + half * 512:2048 + (half + 1) * 512],
                                              start=(kc == 0), stop=(kc == 7)), reads=[hT, win], writes=[P])
            S.op("act", lambda e: e.activation(out=sig[:, half * 512:(half + 1) * 512], in_=P[:], func=AF.Sigmoid),
                 reads=[P], writes=[sig])
        for h in range(8):
            po, fc = (h % 2) * 64, h // 2
            S.op("pe", lambda e: e.matmul(pAT[h // 4][:, h % 4, :], lhsT=qkT[:, 4 + fc, :], rhs=qm[:, h, :],
                                          start=True, stop=True), reads=[qkT, qm], writes=[pAT[h // 4]])
        for half in range(2):
            S.op(("dve", "pool")[0], lambda e: e.tensor_tensor(out=ATs[:, half * 4:(half + 1) * 4, :], in0=pAT[half][:],
                                                  in1=utri[:].unsqueeze(1).to_broadcast([128, 4, 128]), op=ALU.mult),
                 reads=[pAT[half], utri], writes=[ATs])
        for h in range(8):
            po, fc = (h % 2) * 64, h // 2
            PB, sl = PO(h)
            S.op("pe", lambda e: e.matmul(PB[:, sl, 0:129], lhsT=ATs[:, h, :], rhs=vaug[:, h, :], start=True, stop=False),
                 reads=[ATs, vaug], writes=[PB])
            S.op("pe", lambda e: e.matmul(PB[:, sl, 0:129], lhsT=qm[:, h, :], rhs=Cbf[:, h, :],
                                          start=False, stop=True), reads=[qm, Cbf], writes=[PB])
        for bk in range(3):
            nh = 3 if bk < 2 else 2
            S.op("dve", lambda e: e.tensor_tensor(out=den[:, bk * 3:bk * 3 + nh].unsqueeze(2), in0=pOS[bk][:, 0:nh, 128:129],
                                                  in1=eb[:, bk * 3:bk * 3 + nh].unsqueeze(2), op=ALU.mult),
                 reads=[pOS[bk], eb], writes=[den])
        S.op("act", lambda e: e.activation(out=den[:], in_=den[:], func=AF.Abs), reads=[den], writes=[den])
        S.op("dve", lambda e: e.tensor_scalar_max(out=den[:], in0=den[:], scalar1=1.0), reads=[den], writes=[den])
        S.op("dve", lambda e: e.reciprocal(out=den[:], in_=den[:]), reads=[den], writes=[den])
        S.op("dve", lambda e: e.tensor_mul(out=den[:], in0=den[:], in1=eb[:]), reads=[den, eb], writes=[den])
        for bk in range(3):
            nh = 3 if bk < 2 else 2
            S.op("dve", lambda e: e.tensor_tensor(out=hh[:, bk * 3:bk * 3 + nh, :], in0=pOS[bk][:, 0:nh, 0:128],
                                                  in1=den[:, bk * 3:bk * 3 + nh].unsqueeze(2).to_broadcast([128, nh, 128]),
                                                  op=ALU.mult), reads=[pOS[bk], den], writes=[hh])
        for h in range(8):
            PB, sl = PO(h)
            fc = h // 2
            S.op("pe", lambda e: e.matmul(PB[:, sl, 0:129], lhsT=ktok[:, fc, :], rhs=vaug[:, h, :], start=True, stop=True),
                 reads=[ktok, vaug], writes=[PB])
        for bk in range(3):
            nh = 3 if bk < 2 else 2
            S.op("dve", lambda e: e.tensor_tensor(out=Cst[:, bk * 3:bk * 3 + nh, :], in0=pOS[bk][:, 0:nh, 0:129],
                                                  in1=Cst[:, bk * 3:bk * 3 + nh, :], op=ALU.add), reads=[pOS[bk], Cst], writes=[Cst])
        S.op("pool", lambda e: e.tensor_tensor(out=Cst[:], in0=Cst[:], in1=ebL[:].unsqueeze(2).to_broadcast([128, 8, 129]),
                                               op=ALU.mult), reads=[Cst, ebL], writes=[Cst])
        S.op("act", lambda e: e.copy(out=Cbf[:], in_=Cst[:]), reads=[Cst], writes=[Cbf])
        S.op("pool", lambda e: e.tensor_tensor(out=sq[:], in0=hh[:], in1=hh[:], op=ALU.mult), reads=[hh], writes=[sq])
        S.op("dve", lambda e: e.reduce_sum(out=hss[:], in_=sq[:], axis=AX.X), reads=[sq], writes=[hss])
        S.op("act", lambda e: e.activation(out=hss[:], in_=hss[:], func=AF.Sqrt, scale=1.0 / 128, bias=EPS), reads=[hss], writes=[hss])
        S.op("dve", lambda e: e.reciprocal(out=hss[:], in_=hss[:]), reads=[hss], writes=[hss])
        S.op("dve", lambda e: e.tensor_tensor(out=hh[:], in0=hh[:], in1=hss[:].unsqueeze(2).to_broadcast([128, 8, 128]), op=ALU.mult),
             reads=[hh, hss], writes=[hh])
        hh2 = hh[:].rearrange("p h n -> p (h n)")
        S.op("pool", lambda e: e.tensor_tensor(out=hh2, in0=hh2, in1=hg[:], op=ALU.mult), reads=[hh, hg], writes=[hh])
        S.op("dve", lambda e: e.tensor_tensor(out=hob[:], in0=hh2, in1=sig[:], op=ALU.mult), reads=[hh, sig], writes=[hob])
        emit_transpose8(S, hob, pT, hoT, identb)
        Y = yo[t % NB]
        for half in range(2):
            P = pg[pgi % 2]
            pgi += 1
            for kc in range(8):
                S.op("pe", lambda e: e.matmul(P[:], lhsT=hoT[:, kc, :], rhs=wout[:, kc, half * 512:(half + 1) * 512],
                                              start=(kc == 0), stop=(kc == 7)), reads=[hoT, wout], writes=[P])
            S.op("dve", lambda e: e.tensor_mul(out=Y[:, half * 512:(half + 1) * 512], in0=P[:], in1=gate[:, half * 512:(half + 1) * 512]),
                 reads=[P, gate], writes=[Y])
        S.op("pool", lambda e: e.tensor_add(out=Y[:], in0=Y[:], in1=X[:]), reads=[Y, X], writes=[Y])
        S.dma("sp", lambda e: e.dma_start(out=x_out[t * 128:(t + 1) * 128, :], in_=Y[:]), reads=[Y], writes=[])


def drain(gen):
    if gen is not None:
        for _ in gen:
            pass


def emit_headnorm(S, src, gsm, dst, sq, hss, nh, hd):
    s3 = src[:].rearrange("p (h d) -> p h d", d=hd)
    q3 = sq[:].rearrange("p (h d) -> p h d", d=hd)
    d3 = dst[:].rearrange("p (h d) -> p h d", d=hd)
    S.op("pool", lambda e: e.tensor_tensor(out=sq[:], in0=src[:], in1=src[:], op=ALU.mult), reads=[src], writes=[sq])
    S.op("dve", lambda e: e.reduce_sum(out=hss[:], in_=q3, axis=AX.X), reads=[sq], writes=[hss])
    S.op("act", lambda e: e.activation(out=hss[:], in_=hss[:], func=AF.Sqrt, scale=1.0 / hd, bias=EPS), reads=[hss], writes=[hss])
    S.op("dve", lambda e: e.reciprocal(out=hss[:], in_=hss[:]), reads=[hss], writes=[hss])
    S.op("dve", lambda e: e.tensor_tensor(out=q3, in0=s3, in1=hss[:].unsqueeze(2).to_broadcast([128, nh, hd]), op=ALU.mult),
         reads=[src, hss], writes=[sq])
    S.op("pool", lambda e: e.tensor_tensor(out=d3, in0=q3, in1=gsm[:].unsqueeze(1).to_broadcast([128, nh, hd]), op=ALU.mult),
         reads=[sq, gsm], writes=[dst])


def emit_sb(S, nc, x_in, x_out, kvg_row, kvw_d, kng_row, gmix_row, wq_d, qng_row, wout_d, mod_d, nseq, TPS,
            identb, sutri, sltri, ones):
    NH, HD = 16, 64
    kT_all = S.sbuf("kT_all", [128, 8, TPS * 128], BF16)
    v_all = S.sbuf("v_all", [128, TPS, D], BF16)
    xs = [S.sbuf(f"xs{i}", [128, D], F32) for i in range(2)]
    hb = S.sbuf("hb", [128, D], BF16)
    hT = S.sbuf("hT", [128, 8, 128], BF16)
    junk = S.sbuf("junk", [128, D], F32)
    st = S.sbuf("st", [128, 4], F32)
    grow = S.sbuf("grow", [128, D], F32)
    gmod = S.sbuf("gmod", [128, D], F32)
    shift = S.sbuf("shift", [128, D], F32)
    gsm = S.sbuf("gsm", [128, HD], F32)
    pf = S.sbuf("pf", [128, D], F32)
    sq = S.sbuf("sq", [128, D], F32)
    hss = S.sbuf("hss", [128, NH], F32)
    pb = S.sbuf("pb", [128, D], BF16)
    pT = S.psum("pT", [128, 8, 128], BF16)
    pg = S.psum("pg", [128, 512], F32)

    for sidx in range(nseq):
        t0 = sidx * TPS
        with S.scope():
            kvw = S.sbuf("kvw", [128, 8, 2048], BF16)
            kvw_v = kvw_d.rearrange("(k p) n -> p k n", p=128)
            for kc in range(8):
                S.dma("pool", lambda e: e.dma_start(out=kvw[:, kc, :], in_=kvw_v[:, kc, :]), writes=[kvw])
            load_bcast(S, "sp", grow, kvg_row)
            load_bcast(S, "sp", shift, mod_d[sidx, 12:13, :])
            load_bcast(S, "sp", gmod, mod_d[sidx, 13:14, :])
            load_bcast(S, "sp", gsm, kng_row)
            S.op("dve", lambda e: e.scalar_tensor_tensor(out=gmod[:], in0=gmod[:], scalar=1.0, in1=grow[:],
                                                         op0=ALU.add, op1=ALU.mult), reads=[gmod, grow], writes=[gmod])
            for t in range(TPS):
                X = xs[t % 2]
                S.dma("sp", lambda e: e.dma_start(out=X[:], in_=x_in[(t0 + t) * 128:(t0 + t + 1) * 128, :]), writes=[X])
                emit_norm_mod(S, X, gmod, shift, hb, st, junk)
                emit_transpose8(S, hb, pT, hT, identb)
                for ch in range(4):
                    for kc in range(8):
                        S.op("pe", lambda e: e.matmul(pg[:], lhsT=hT[:, kc, :], rhs=kvw[:, kc, ch * 512:(ch + 1) * 512],
                                                      start=(kc == 0), stop=(kc == 7)), reads=[hT, kvw], writes=[pg])
                    if ch < 2:
                        S.op("act", lambda e: e.copy(out=pf[:, ch * 512:(ch + 1) * 512], in_=pg[:]), reads=[pg], writes=[pf])
                    else:
                        S.op("act", lambda e: e.copy(out=v_all[:, t, (ch - 2) * 512:(ch - 1) * 512], in_=pg[:]), reads=[pg], writes=[v_all])
                emit_headnorm(S, pf, gsm, pb, sq, hss, NH, HD)
                for kc in range(8):
                    S.op("pe", lambda e: e.transpose(out=pT[:, kc, :], in_=pb[:, kc * 128:(kc + 1) * 128], identity=identb[:]),
                         reads=[pb, identb], writes=[pT])
                S.op("act", lambda e: e.copy(out=kT_all[:, :, t * 128:(t + 1) * 128], in_=pT[:]), reads=[pT], writes=[kT_all])
        with S.scope():
            wq = S.sbuf("wq", [128, 8, D], BF16)
            wout = S.sbuf("wout", [128, 8, D], BF16)
            wq_v = wq_d.rearrange("(k p) n -> p k n", p=128)
            wout_v = wout_d.rearrange("(k p) n -> p k n", p=128)
            for kc in range(8):
                S.dma("pool", lambda e: e.dma_start(out=wq[:, kc, :], in_=wq_v[:, kc, :]), writes=[wq])
                S.dma("pool", lambda e: e.dma_start(out=wout[:, kc, :], in_=wout_v[:, kc, :]), writes=[wout])
            gate = S.sbuf("gate", [128, D], F32)
            load_bcast(S, "sp", grow, gmix_row)
            load_bcast(S, "sp", shift, mod_d[sidx, 6:7, :])
            load_bcast(S, "sp", gmod, mod_d[sidx, 7:8, :])
            load_bcast(S, "sp", gate, mod_d[sidx, 8:9, :])
            load_bcast(S, "sp", gsm, qng_row)
            S.op("dve", lambda e: e.scalar_tensor_tensor(out=gmod[:], in0=gmod[:], scalar=1.0, in1=grow[:],
                                                         op0=ALU.add, op1=ALU.mult), reads=[gmod, grow], writes=[gmod])
            S.op("dve", lambda e: e.tensor_scalar_mul(out=gsm[:], in0=gsm[:], scalar1=HD ** -0.5), reads=[gsm], writes=[gsm])
            qm = S.sbuf("qm", [128, NH, 128], BF16)
            S.op("pool", lambda e: e.memset(qm[:], 0.0), writes=[qm])
            qm4 = qm[:].rearrange("p (f two) n -> p f two n", two=2)
            E = [S.sbuf(f"E{i}", [128, 512], F32) for i in range(4)]
            SP = [S.sbuf(f"SP{i}", [128, 512], F32) for i in range(4)]
            ARG = [S.sbuf(f"ARG{i}", [128, 512], F32) for i in range(2)]
            XB = [S.sbuf(f"XB{i}", [128, 512], F32) for i in range(2)]
            A = [S.sbuf(f"A{i}", [128, 512], BF16) for i in range(3)]
            SPcum = [S.sbuf(f"SPcum{i}", [128, 512], F32) for i in range(4)]
            yo = [S.sbuf(f"yo{i}", [128, D], F32) for i in range(2)]
            oT = S.sbuf("oT", [128, 8, 128], BF16)
            pz = [S.psum(f"pz{i}", [128, 4, 128], F32) for i in range(2)]
            pnb = [S.psum(f"pnb{i}", [128, 512], F32) for i in range(2)]
            po = [S.psum(f"po{i}", [128, 512], F32) for i in range(2)]
            m3 = sutri[:].unsqueeze(1).to_broadcast([128, 4, 128])
            for qt in range(TPS):
                X = xs[qt % 2]
                S.dma("sp", lambda e: e.dma_start(out=X[:], in_=x_in[(t0 + qt) * 128:(t0 + qt + 1) * 128, :]), writes=[X])
                emit_norm_mod(S, X, gmod, shift, hb, st, junk)
                emit_transpose8(S, hb, pT, hT, identb)
                for ch in range(2):
                    for kc in range(8):
                        S.op("pe", lambda e: e.matmul(pg[:], lhsT=hT[:, kc, :], rhs=wq[:, kc, ch * 512:(ch + 1) * 512],
                                                      start=(kc == 0), stop=(kc == 7)), reads=[hT, wq], writes=[pg])
                    S.op("act", lambda e: e.copy(out=pf[:, ch * 512:(ch + 1) * 512], in_=pg[:]), reads=[pg], writes=[pf])
                emit_headnorm(S, pf, gsm, pb, sq, hss, NH, HD)
                for kc in range(8):
                    S.op("pe", lambda e: e.transpose(out=pT[:, kc, :], in_=pb[:, kc * 128:(kc + 1) * 128], identity=identb[:]),
                         reads=[pb, identb], writes=[pT])
                S.op("act", lambda e: e.copy(out=qm4[0:64, :, 0, :], in_=pT[0:64, :, :]), reads=[pT], writes=[qm])
                S.op("act", lambda e: e.copy(out=qm4[64:128, :, 1, :], in_=pT[64:128, :, :]), reads=[pT], writes=[qm])
                items = [(kt, g) for kt in range(qt, -1, -1) for g in range(4)]
                NI = len(items)

                def S1(i):
                    kt, g = items[i]
                    Z = pz[i % 2]
                    for j in range(4):
                        h = 4 * g + j
                        S.op("pe", lambda e: e.matmul(Z[:, j, :], lhsT=kT_all[:, h // 2, kt * 128:(kt + 1) * 128], rhs=qm[:, h, :],
                                                      start=True, stop=True), reads=[kT_all, qm], writes=[Z])

                def S2(i):
                    kt, g = items[i]
                    Z, Eb, SPb = pz[i % 2], E[i % 4], SP[i % 4]
                    Z2 = Z[:].rearrange("p j n -> p (j n)")
                    S.op("act", lambda e: e.activation(out=Eb[:], in_=Z2, func=AF.Exp), reads=[Z], writes=[Eb])
                    S.op("act", lambda e: e.activation(out=SPb[:], in_=Eb[:], func=AF.Ln, bias=1.0), reads=[Eb], writes=[SPb])
                    if kt == qt:
                        S.op("dve", lambda e: e.tensor_tensor(out=SPb[:].rearrange("p (j n) -> p j n", n=128),
                                                              in0=SPb[:].rearrange("p (j n) -> p j n", n=128), in1=m3, op=ALU.mult),
                             reads=[SPb, sutri], writes=[SPb])

                def S3(i):
                    kt, g = items[i]
                    diag = (kt == qt)
                    NBp, SPb = pnb[i % 2], SP[i % 4]
                    S.op("pe", lambda e: e.matmul(NBp[:], lhsT=sltri[:], rhs=SPb[:], start=True, stop=diag), reads=[sltri, SPb], writes=[NBp])
                    if not diag:
                        S.op("pe", lambda e: e.matmul(NBp[:], lhsT=ones[:], rhs=SPcum[g][:], start=False, stop=True),
                             reads=[ones, SPcum[g]], writes=[NBp])
                    if kt > 0:
                        if diag:
                            S.op("pool", lambda e: e.tensor_copy(out=SPcum[g][:], in_=SPb[:]), reads=[SPb], writes=[SPcum[g]])
                        else:
                            S.op("pool", lambda e: e.tensor_add(out=SPcum[g][:], in0=SPcum[g][:], in1=SPb[:]), reads=[SPb, SPcum[g]], writes=[SPcum[g]])

                def S4(i):
                    kt, g = items[i]
                    diag = (kt == qt)
                    NBp, Eb, SPb, ARGb, Xb, Ab = pnb[i % 2], E[i % 4], SP[i % 4], ARG[i % 2], XB[i % 2], A[i % 3]
                    S.op("dve", lambda e: e.tensor_tensor(out=ARGb[:], in0=SPb[:], in1=NBp[:], op=ALU.add), reads=[SPb, NBp], writes=[ARGb])
                    S.op("act", lambda e: e.activation(out=Xb[:], in_=ARGb[:], func=AF.Exp, scale=-1.0), reads=[ARGb], writes=[Xb])
                    if diag:
                        S.op("dve", lambda e: e.tensor_tensor(out=ARGb[:], in0=Xb[:], in1=Eb[:], op=ALU.mult), reads=[Xb, Eb], writes=[ARGb])
                        S.op("pool", lambda e: e.tensor_tensor(out=Ab[:].rearrange("p (j n) -> p j n", n=128),
                                                               in0=ARGb[:].rearrange("p (j n) -> p j n", n=128), in1=m3, op=ALU.mult),
                             reads=[ARGb, sutri], writes=[Ab])
                    else:
                        S.op("dve", lambda e: e.tensor_tensor(out=Ab[:], in0=Xb[:], in1=Eb[:], op=ALU.mult), reads=[Xb, Eb], writes=[Ab])

                def S5(i):
                    kt, g = items[i]
                    Ab = A[i % 3]
                    for j in range(4):
                        h = 4 * g + j
                        PO = po[h // 8]
                        S.op("pe", lambda e: e.matmul(PO[:, (h % 8) * 64:(h % 8 + 1) * 64], lhsT=Ab[:, j * 128:(j + 1) * 128],
                                                      rhs=v_all[:, kt, h * 64:(h + 1) * 64], start=(kt == qt and h % 8 == 0), stop=(kt == 0)),
                             reads=[Ab, v_all], writes=[PO])

                for n in range(NI + 4):
                    if n < NI:
                        S1(n)
                    if 0 <= n - 1 < NI:
                        S2(n - 1)
                    if 0 <= n - 2 < NI:
                        S3(n - 2)
                    if 0 <= n - 3 < NI:
                        S4(n - 3)
                    if 0 <= n - 4 < NI:
                        S5(n - 4)
                for half in range(2):
                    S.op("act", lambda e: e.copy(out=pb[:, half * 512:(half + 1) * 512], in_=po[half][:]), reads=[po[half]], writes=[pb])
                emit_transpose8(S, pb, pT, oT, identb)
                Y = yo[qt % 2]
                for half in range(2):
                    for kc in range(8):
                        S.op("pe", lambda e: e.matmul(pg[:], lhsT=oT[:, kc, :], rhs=wout[:, kc, half * 512:(half + 1) * 512],
                                                      start=(kc == 0), stop=(kc == 7)), reads=[oT, wout], writes=[pg])
                    S.op("dve", lambda e: e.tensor_mul(out=Y[:, half * 512:(half + 1) * 512], in0=pg[:], in1=gate[:, half * 512:(half + 1) * 512]),
                         reads=[pg, gate], writes=[Y])
                S.op("pool", lambda e: e.tensor_add(out=Y[:], in0=Y[:], in1=X[:]), reads=[Y, X], writes=[Y])
                S.dma("sp", lambda e: e.dma_start(out=x_out[(t0 + qt) * 128:(t0 + qt + 1) * 128, :], in_=Y[:]), reads=[Y], writes=[])


NCORES = 8
SEQ = 2048
TPS = SEQ // 128
NSEQ = 2
NT = NSEQ * TPS
_NC_CACHE = {}


def build_program():
    nc = bass.Bass("TRN2", target_bir_lowering=False)

    def din(name, shape):
        return nc.dram_tensor(name, list(shape), F32, kind="ExternalInput").ap()

    x = din("x", [NT * 128, D])
    cT = din("cT", [128, 8, 2])
    ident = din("ident", [128, 128]); utri_d = din("utri", [128, 128]); sutri_d = din("sutri", [128, 128])
    sltri_d = din("sltri", [128, 128]); ones_d = din("ones", [128, 128]); iota_d = din("iota16", [128, 16])
    ada_w = din("ada_w", [2, D, 6 * D]); ada_b = din("ada_b", [2, 6 * D])
    norm_mix_g = din("norm_mix_g", [2, D]); norm_ffn_g = din("norm_ffn_g", [2, D])
    ma_w_in = din("ma_w_in", [D, 3088]); cwT = din("cwT", [128, 8, 4]); ma_b_if = din("ma_b_if", [1, 16])
    ma_hnorm_g = din("ma_hnorm_g", [1, D]); ma_w_out = din("ma_w_out", [D, D])
    kv_ada_w = din("kv_ada_w", [D, 2 * D]); kv_ada_b = din("kv_ada_b", [1, 2 * D]); kv_norm_g = din("kv_norm_g", [1, D])
    kv_w = din("kv_w", [D, 2 * D]); k_norm_g = din("k_norm_g", [1, 64])
    sb_w_q = din("sb_w_q", [D, D]); sb_q_norm_g = din("sb_q_norm_g", [1, 64]); sb_w_out = din("sb_w_out", [D, D])
    peer_w_q = din("peer_w_q", [2, D, 2 * D]); peer_sub_keys = din("peer_sub_keys", [2, 2, 128, 128])
    peer_u = din("peer_u", [2, 16384, D]); peer_v = din("peer_v", [2, 16384, D])
    pu_flat = peer_u.rearrange("l e d -> (l e) d")
    pv_flat = peer_v.rearrange("l e d -> (l e) d")
    out = nc.dram_tensor("out", [NT * 128, D], F32, kind="ExternalOutput").ap()
    mod = nc.dram_tensor("mod_scr", [2, 14, D], F32, kind="Internal").ap()
    xa = nc.dram_tensor("xa_scr", [NT * 128, D], F32, kind="Internal").ap()
    xb = nc.dram_tensor("xb_scr", [NT * 128, D], F32, kind="Internal").ap()
    xc = nc.dram_tensor("xc_scr", [NT * 128, D], F32, kind="Internal").ap()

    with ExitStack() as es:
        S = Sched(nc, es)
        identb = S.sbuf("identb", [128, 128], BF16)
        S.dma("pool", lambda e: e.dma_start(out=identb[:], in_=ident), writes=[identb])
        cs = {}
        for nm, d in (("utri", utri_d), ("sutri", sutri_d), ("sltri", sltri_d), ("ones", ones_d)):
            cs[nm] = S.sbuf(nm, [128, 128], F32)
            S.dma("sp", lambda e: e.dma_start(out=cs[nm][:], in_=d), writes=[cs[nm]])
        iota16 = S.sbuf("iota16", [128, 16], F32)
        S.dma("sp", lambda e: e.dma_start(out=iota16[:], in_=iota_d), writes=[iota16])
        puv = pu_flat.bitcast(BF16)
        with S.scope():
            emit_ada(S, nc, cT, ada_w, ada_b, kv_ada_w, kv_ada_b, mod)
        with S.scope():
            side = gen_convert_uv(S, nc, pu_flat, pv_flat, 32768)
            emit_mlstm(S, nc, x, xa, ma_w_in, cwT, ma_b_if, ma_hnorm_g, ma_w_out, norm_mix_g[0:1, :], mod, NT, TPS,
                       identb, cs["utri"], cs["ones"], side)
            drain(side)
        with S.scope():
            emit_peer(S, nc, xa, xb, peer_w_q[0], peer_sub_keys[0], puv, norm_ffn_g[0:1, :], mod, 3, NT, TPS, identb, iota16, 0)
        with S.scope():
            emit_sb(S, nc, xb, xc, kv_norm_g, kv_w, k_norm_g, norm_mix_g[1:2, :], sb_w_q, sb_q_norm_g, sb_w_out, mod, NSEQ, TPS,
                    identb, cs["sutri"], cs["sltri"], cs["ones"])
        with S.scope():
            emit_peer(S, nc, xc, out, peer_w_q[1], peer_sub_keys[1], puv, norm_ffn_g[1:2, :], mod, 9, NT, TPS, identb, iota16, 16384)
        S.barrier()
    return nc


def kernel(x, c, ada_w, ada_b, norm_mix_g, norm_ffn_g, ma_w_in, ma_conv_w, ma_b_if, ma_hnorm_g, ma_w_out,
           kv_ada_w, kv_ada_b, kv_norm_g, kv_w, k_norm_g, sb_w_q, sb_q_norm_g, sb_w_out,
           peer_w_q, peer_sub_keys, peer_u, peer_v):
    f = lambda a: np.ascontiguousarray(np.asarray(a, dtype=np.float32))
    x = f(x); c = f(c)
    one = np.ones((128, 128), np.float32)
    shared = {
        "ident": np.eye(128, dtype=np.float32), "utri": np.triu(one), "sutri": np.triu(one, 1), "sltri": np.tril(one, -1), "ones": one,
        "iota16": np.tile(np.arange(16, dtype=np.float32), (128, 1)),
        "ada_w": f(ada_w), "ada_b": f(ada_b), "norm_mix_g": f(norm_mix_g), "norm_ffn_g": f(norm_ffn_g),
        "ma_w_in": f(ma_w_in)[0], "cwT": np.ascontiguousarray(f(ma_conv_w)[0].reshape(4, 8, 128).transpose(2, 1, 0)),
        "ma_b_if": f(ma_b_if).reshape(1, 16), "ma_hnorm_g": f(ma_hnorm_g).reshape(1, D), "ma_w_out": f(ma_w_out)[0],
        "kv_ada_w": f(kv_ada_w), "kv_ada_b": f(kv_ada_b).reshape(1, 2 * D), "kv_norm_g": f(kv_norm_g).reshape(1, D),
        "kv_w": f(kv_w), "k_norm_g": f(k_norm_g).reshape(1, 64),
        "sb_w_q": f(sb_w_q)[0], "sb_q_norm_g": f(sb_q_norm_g).reshape(1, 64), "sb_w_out": f(sb_w_out)[0],
        "peer_w_q": f(peer_w_q), "peer_sub_keys": f(peer_sub_keys), "peer_u": f(peer_u), "peer_v": f(peer_v),
    }
    in_maps = []
    for i in range(NCORES):
        m = dict(shared)
        m["x"] = x[NSEQ * i:NSEQ * (i + 1)].reshape(NT * 128, D)
        m["cT"] = np.ascontiguousarray(c[NSEQ * i:NSEQ * (i + 1)].T.reshape(8, 128, NSEQ).transpose(1, 0, 2))
        in_maps.append(m)
    if "nc" not in _NC_CACHE:
        _NC_CACHE["nc"] = build_program()
    res = run_bass_kernel_spmd(_NC_CACHE["nc"], in_maps, core_ids=list(range(NCORES)))
    return np.concatenate([r["out"].reshape(NSEQ, SEQ, D) for r in res.results], axis=0)
```

```python
import numpy as np
from contextlib import ExitStack
import concourse.bass as bass
import concourse.mybir as mybir
from concourse.bass_utils import run_bass_kernel_spmd

F32 = mybir.dt.float32
BF16 = mybir.dt.bfloat16
U32 = mybir.dt.uint32
I32 = mybir.dt.int32
AF = mybir.ActivationFunctionType
ALU = mybir.AluOpType
AX = mybir.AxisListType


class Buf:
    __slots__ = ("name", "w", "r", "t")

    def __init__(self, name, t=None):
        self.name = name
        self.w = None
        self.r = {}
        self.t = t

    def __getitem__(self, idx):
        return self.t[idx]


class Sched:
    NDMA = 12

    def __init__(self, nc, es):
        self.nc = nc
        self.es = es
        self.engs = {"pe": nc.tensor, "act": nc.scalar, "dve": nc.vector, "pool": nc.gpsimd, "sp": nc.sync}
        self.sem = {}
        self.cnt = {}
        for k in ("pe", "act", "dve", "pool"):
            self.sem[k] = es.enter_context(nc.semaphore("s_" + k))
            self.cnt[k] = 0
        self.dq = {}
        for q in ("sp", "act", "pool"):
            sems = [es.enter_context(nc.semaphore(f"d_{q}{i}")) for i in range(self.NDMA)]
            self.dq[q] = {"sems": sems, "n": 0}
            for i in range(self.NDMA):
                self.sem[("dma", q, i)] = sems[i]
                self.cnt[("dma", q, i)] = 0
        self.waited = {e: {} for e in self.engs}
        self.nins = 0

    def sbuf(self, name, shape, dt):
        self.nins += 0
        self._uid = getattr(self, "_uid", 0) + 1
        name = f"sb{self._uid}_{name}"
        t = self.es.enter_context(self.nc.sbuf_tensor(name, list(shape), dt))
        return Buf(name, t)

    def psum(self, name, shape, dt=F32):
        self._uid = getattr(self, "_uid", 0) + 1
        name = f"ps{self._uid}_{name}"
        t = self.es.enter_context(self.nc.psum_tensor(name, list(shape), dt))
        return Buf(name, t)

    def view(self, name):
        return Buf(name)

    def scope(self):
        return _Scope(self)

    def _wait(self, engine, key, c):
        if c <= 0:
            return
        w = self.waited[engine]
        if w.get(key, 0) >= c:
            return
        self.engs[engine].wait_ge(self.sem[key], c)
        w[key] = c

    def _deps(self, engine, reads, writes):
        deps = {}
        for b in reads:
            if b.w is not None:
                k, c = b.w
                deps[k] = max(deps.get(k, 0), c)
        for b in writes:
            if b.w is not None:
                k, c = b.w
                deps[k] = max(deps.get(k, 0), c)
            for k, c in b.r.items():
                deps[k] = max(deps.get(k, 0), c)
        return deps

    def op(self, engine, fn, reads=(), writes=()):
        deps = self._deps(engine, reads, writes)
        for k, c in deps.items():
            if k == engine and engine == "pe":
                continue
            self._wait(engine, k, c)
        ins = fn(self.engs[engine])
        self.cnt[engine] += 1
        ins.then_inc(self.sem[engine], 1)
        me = (engine, self.cnt[engine])
        for b in reads:
            b.r[engine] = self.cnt[engine]
        for b in writes:
            b.w = me
            b.r = {}
        self.nins += 1
        return ins

    def dma(self, q, fn, reads=(), writes=()):
        d = self.dq[q]
        i = d["n"] % self.NDMA
        d["n"] += 1
        key = ("dma", q, i)
        deps = self._deps(q, reads, writes)
        deps[key] = max(deps.get(key, 0), self.cnt[key])
        for k, c in deps.items():
            self._wait(q, k, c)
        ins = fn(self.engs[q])
        self.cnt[key] += 16
        ins.then_inc(self.sem[key], 16)
        me = (key, self.cnt[key])
        for b in reads:
            b.r[key] = self.cnt[key]
        for b in writes:
            b.w = me
            b.r = {}
        self.nins += 1
        return ins

    def finish(self, bufs):
        for b in bufs:
            if b.w is not None:
                k, c = b.w
                for e in ("sp", "act", "pool", "dve", "pe"):
                    self._wait(e, k, c)

    def barrier(self):
        for e in self.engs:
            for k, c in self.cnt.items():
                if c > 0 and k != e:
                    self._wait(e, k, c)


class _Scope:
    def __init__(self, S):
        self.S = S

    def __enter__(self):
        self.old = self.S.es
        self.es2 = ExitStack()
        self.es2.__enter__()
        self.S.es = self.es2
        return self

    def __exit__(self, *a):
        self.S.barrier()
        self.S.es = self.old
        return self.es2.__exit__(*a)


EPS = 1e-6
D = 1024


def load_bcast(S, q, dst, row_ap):
    S.dma(q, lambda e: e.dma_start(out=dst[:], in_=row_ap.partition_broadcast(128)), writes=[dst])


def emit_norm_mod(S, xs, gmod, shift, hb, st, junk):
    S.op("act", lambda e: e.activation(out=junk[:], in_=xs[:], func=AF.Square, accum_out=st[:, 0:1]),
         reads=[xs], writes=[junk, st])
    S.op("act", lambda e: e.activation(out=st[:, 1:2], in_=st[:, 0:1], func=AF.Sqrt, scale=1.0 / D, bias=EPS),
         reads=[st], writes=[st])
    S.op("dve", lambda e: e.reciprocal(out=st[:, 2:3], in_=st[:, 1:2]), reads=[st], writes=[st])
    S.op("dve", lambda e: e.scalar_tensor_tensor(out=junk[:], in0=xs[:], scalar=st[:, 2:3], in1=gmod[:],
                                                 op0=ALU.mult, op1=ALU.mult), reads=[xs, st, gmod], writes=[junk])
    S.op("dve", lambda e: e.tensor_add(out=hb[:], in0=junk[:], in1=shift[:]), reads=[junk, shift], writes=[hb])


def emit_transpose8(S, hb, pT, hT, identb):
    for kc in range(8):
        S.op("pe", lambda e: e.transpose(out=pT[:, kc, :], in_=hb[:, kc * 128:(kc + 1) * 128], identity=identb[:]),
             reads=[hb, identb], writes=[pT])
    S.op("act", lambda e: e.copy(out=hT[:], in_=pT[:]), reads=[pT], writes=[hT])


def emit_peer(S, nc, x_in, x_out, wq_d, sk_d, puv_d, gffn_row, mod_d, mod_base, ntiles, tiles_per_batch,
              identb, iota16, idx_base=0):
    wq = S.sbuf("wq", [128, 8, 2048], BF16)
    wq_v = wq_d.rearrange("(k p) n -> p k n", p=128)
    for kc in range(8):
        S.dma("pool", lambda e: e.dma_start(out=wq[:, kc, :], in_=wq_v[:, kc, :]), writes=[wq])
    skn = S.sbuf("skn", [128, 2, 128], BF16)
    S.dma("pool", lambda e: e.dma_start(out=skn[:], in_=sk_d.rearrange("p n k -> n p k")), writes=[skn])
    skT = S.sbuf("skT", [128, 2, 128], BF16)
    pT = S.psum("pT", [128, 8, 128], BF16)
    for p in range(2):
        S.op("pe", lambda e: e.transpose(out=pT[:, p, :], in_=skn[:, p, :], identity=identb[:]),
             reads=[skn, identb], writes=[pT])
    S.op("act", lambda e: e.copy(out=skT[:], in_=pT[:, 0:2, :]), reads=[pT], writes=[skT])

    grow = S.sbuf("grow", [128, D], F32)
    load_bcast(S, "sp", grow, gffn_row)
    gmod = S.sbuf("gmod", [128, D], F32)
    shift = S.sbuf("shift", [128, D], F32)
    nbatch = (ntiles + tiles_per_batch - 1) // tiles_per_batch
    gates = [S.sbuf(f"gate{i}", [128, D], F32) for i in range(nbatch)]
    for i in range(nbatch):
        load_bcast(S, "sp", gates[i], mod_d[i, mod_base + 2:mod_base + 3, :])

    NB = 2
    xs = [S.sbuf(f"xs{i}", [128, D], F32) for i in range(NB)]
    hb = [S.sbuf(f"hb{i}", [128, D], BF16) for i in range(NB)]
    hT = [S.sbuf(f"hT{i}", [128, 8, 128], BF16) for i in range(NB)]
    junk = S.sbuf("junk", [128, D], F32)
    junkb = S.sbuf("junkb", [128, D], BF16)
    st = [S.sbuf(f"st{i}", [128, 4], F32) for i in range(NB)]
    qT = S.sbuf("qT", [128, 16, 128], BF16)
    pq = [S.psum(f"pq{i}", [128, 4, 128], F32) for i in range(2)]
    ps = [S.psum(f"ps{i}", [128, 4, 128], F32) for i in range(2)]
    pout = [S.psum(f"po{i}", [128, 512], F32) for i in range(2)]
    s_sb = S.sbuf("s_sb", [128, 16, 128], F32)
    s2 = S.sbuf("s2", [128, 16, 128], F32)
    top = S.sbuf("top", [128, 16, 16], F32)
    tix = S.sbuf("tix", [128, 16, 16], U32)
    tixf = S.sbuf("tixf", [128, 16, 16], F32)
    cand = S.sbuf("cand", [128, 8, 256], F32)
    cand2 = S.sbuf("cand2", [128, 8, 256], F32)
    pos = S.sbuf("pos", [128, 8, 16], U32)
    posf = S.sbuf("posf", [128, 8, 16], F32)
    ai = S.sbuf("ai", [128, 8, 16], I32)
    af = S.sbuf("af", [128, 8, 16], F32)
    bf_ = S.sbuf("bf_", [128, 8, 16], F32)
    ia = S.sbuf("ia", [128, 8, 16], F32)
    ja = S.sbuf("ja", [128, 8, 16], F32)
    oh = S.sbuf("oh", [128, 2048], F32)
    g = S.sbuf("g", [128, 8, 16], F32)
    ge = S.sbuf("ge", [128, 8, 16], F32)
    gsum = S.sbuf("gsum", [128, 8], F32)
    ef = S.sbuf("ef", [128, 128], F32)
    eidx = [S.sbuf(f"eidx{i}", [128, 128], I32) for i in range(NB)]
    gs = [S.sbuf(f"gs{i}", [128, 128], F32) for i in range(NB)]
    act = S.sbuf("act", [128, 128], F32)
    wv = S.sbuf("wv", [128, 128], F32)
    NG = 14
    ug = [S.sbuf(f"ug{i}", [128, 2 * D], BF16) for i in range(NG)]
    actg = [S.sbuf(f"actg{i}", [128, 4], F32) for i in range(4)]
    wvg = [S.sbuf(f"wvg{i}", [128, 4], F32) for i in range(4)]
    ND = 8
    dg = [S.sbuf(f"dg{i}", [128, 128], BF16) for i in range(ND)]
    yo = [S.sbuf(f"yo{i}", [128, D], F32) for i in range(NB)]
    gi = 0
    di = 0

    def stage_I(t):
        b = t // tiles_per_batch
        i2 = t % NB
        if t % tiles_per_batch == 0:
            load_bcast(S, "sp", shift, mod_d[b, mod_base + 0:mod_base + 1, :])
            load_bcast(S, "sp", gmod, mod_d[b, mod_base + 1:mod_base + 2, :])
            S.op("dve", lambda e: e.scalar_tensor_tensor(out=gmod[:], in0=gmod[:], scalar=1.0, in1=grow[:],
                                                         op0=ALU.add, op1=ALU.mult), reads=[gmod, grow], writes=[gmod])
        X, HB, HT, ST = xs[i2], hb[i2], hT[i2], st[i2]
        S.dma("sp", lambda e: e.dma_start(out=X[:], in_=x_in[t * 128:(t + 1) * 128, :]), writes=[X])
        yield
        emit_norm_mod(S, X, gmod, shift, HB, ST, junk)
        yield
        emit_transpose8(S, HB, pT, HT, identb)
        yield
        for gq in range(4):
            P = pq[gq % 2]
            for j in range(4):
                hp = gq * 4 + j
                for kc in range(8):
                    S.op("pe", lambda e: e.matmul(P[:, j, :], lhsT=wq[:, kc, hp * 128:(hp + 1) * 128], rhs=HT[:, kc, :],
                                                  start=(kc == 0), stop=(kc == 7)), reads=[wq, HT], writes=[P])
            S.op("act", lambda e: e.copy(out=qT[:, gq * 4:(gq + 1) * 4, :], in_=P[:]), reads=[P], writes=[qT])
            yield
        for gq in range(4):
            P = ps[gq % 2]
            for j in range(4):
                hp = gq * 4 + j
                S.op("pe", lambda e: e.matmul(P[:, j, :], lhsT=qT[:, hp, :], rhs=skT[:, hp % 2, :], start=True, stop=True),
                     reads=[qT, skT], writes=[P])
            S.op("act", lambda e: e.copy(out=s_sb[:, gq * 4:(gq + 1) * 4, :], in_=P[:]), reads=[P], writes=[s_sb])
            yield
        for hp in range(16):
            S.op("dve", lambda e: e.max(out=top[:, hp, 0:8], in_=s_sb[:, hp, :]), reads=[s_sb], writes=[top])
            S.op("dve", lambda e: e.max_index(out=tix[:, hp, 0:8], in_max=top[:, hp, 0:8], in_values=s_sb[:, hp, :]),
                 reads=[s_sb, top], writes=[tix])
            S.op("dve", lambda e: e.match_replace(out=s2[:, hp, :], in_to_replace=top[:, hp, 0:8], in_values=s_sb[:, hp, :],
                                                  imm_value=-1e30), reads=[s_sb, top], writes=[s2])
            S.op("dve", lambda e: e.max(out=top[:, hp, 8:16], in_=s2[:, hp, :]), reads=[s2], writes=[top])
            S.op("dve", lambda e: e.max_index(out=tix[:, hp, 8:16], in_max=top[:, hp, 8:16], in_values=s2[:, hp, :]),
                 reads=[s2, top], writes=[tix])
            yield
        S.op("dve", lambda e: e.tensor_copy(out=tixf[:], in_=tix[:]), reads=[tix], writes=[tixf])
        top4 = top[:].rearrange("q (h p) a -> q h p a", p=2)
        tix4 = tixf[:].rearrange("q (h p) a -> q h p a", p=2)
        c4 = cand[:].rearrange("q h (a b) -> q h a b", b=16)
        S.op("dve", lambda e: e.tensor_tensor(out=c4, in0=top4[:, :, 0, :].unsqueeze(3).to_broadcast([128, 8, 16, 16]),
                                              in1=top4[:, :, 1, :].unsqueeze(2).to_broadcast([128, 8, 16, 16]), op=ALU.add),
             reads=[top], writes=[cand])
        for h in range(8):
            S.op("dve", lambda e: e.max(out=g[:, h, 0:8], in_=cand[:, h, :]), reads=[cand], writes=[g])
            S.op("dve", lambda e: e.match_replace(out=cand2[:, h, :], in_to_replace=g[:, h, 0:8], in_values=cand[:, h, :],
                                                  imm_value=-1e30), reads=[cand, g], writes=[cand2])
            S.op("dve", lambda e: e.max(out=g[:, h, 8:16], in_=cand2[:, h, :]), reads=[cand2], writes=[g])
            yield
        for h in range(8):
            S.op("dve", lambda e: e.max_index(out=pos[:, h, 0:8], in_max=g[:, h, 0:8], in_values=cand[:, h, :]),
                 reads=[cand, g], writes=[pos])
            S.op("dve", lambda e: e.max_index(out=pos[:, h, 8:16], in_max=g[:, h, 8:16], in_values=cand2[:, h, :]),
                 reads=[cand2, g], writes=[pos])
            yield
        S.op("dve", lambda e: e.tensor_copy(out=posf[:], in_=pos[:]), reads=[pos], writes=[posf])
        S.op("dve", lambda e: e.tensor_scalar(out=ai[:], in0=posf[:], scalar1=0.0625, scalar2=-0.46875, op0=ALU.mult, op1=ALU.add),
             reads=[posf], writes=[ai])
        S.op("dve", lambda e: e.tensor_copy(out=af[:], in_=ai[:]), reads=[ai], writes=[af])
        S.op("dve", lambda e: e.scalar_tensor_tensor(out=bf_[:], in0=af[:], scalar=-16.0, in1=posf[:], op0=ALU.mult, op1=ALU.add),
             reads=[af, posf], writes=[bf_])
        yield
        oh4 = oh[:].rearrange("q (h k a) -> q h k a", k=16, a=16)
        io4 = iota16[:].unsqueeze(1).unsqueeze(1).to_broadcast([128, 8, 16, 16])
        for (src, pidx, dst) in ((af, 0, ia), (bf_, 1, ja)):
            S.op("dve", lambda e: e.tensor_tensor(out=oh4, in0=src[:].unsqueeze(3).to_broadcast([128, 8, 16, 16]), in1=io4, op=ALU.is_equal),
                 reads=[src, iota16], writes=[oh])
            S.op("dve", lambda e: e.tensor_tensor(out=oh4, in0=oh4, in1=tix4[:, :, pidx, :].unsqueeze(2).to_broadcast([128, 8, 16, 16]), op=ALU.mult),
                 reads=[oh, tixf], writes=[oh])
            S.op("dve", lambda e: e.reduce_sum(out=dst[:], in_=oh4, axis=AX.X), reads=[oh], writes=[dst])
            yield
        S.op("dve", lambda e: e.scalar_tensor_tensor(out=ef[:].rearrange("q (h k) -> q h k", k=16), in0=ia[:], scalar=128.0, in1=ja[:],
                                                     op0=ALU.mult, op1=ALU.add), reads=[ia, ja], writes=[ef])
        EI, GS = eidx[i2], gs[i2]
        S.op("dve", lambda e: e.tensor_scalar(out=ef[:], in0=ef[:], scalar1=16383.0, scalar2=0.0, op0=ALU.min, op1=ALU.max),
             reads=[ef], writes=[ef])
        if idx_base:
            S.op("dve", lambda e: e.tensor_scalar_add(out=ef[:], in0=ef[:], scalar1=float(idx_base)), reads=[ef], writes=[ef])
        S.op("dve", lambda e: e.tensor_copy(out=EI[:], in_=ef[:]), reads=[ef], writes=[EI])
        S.op("dve", lambda e: e.tensor_tensor(out=ge[:], in0=g[:], in1=g[:, :, 0:1].to_broadcast([128, 8, 16]), op=ALU.subtract),
             reads=[g], writes=[ge])
        S.op("act", lambda e: e.activation(out=ge[:], in_=ge[:], func=AF.Exp), reads=[ge], writes=[ge])
        S.op("dve", lambda e: e.reduce_sum(out=gsum[:], in_=ge[:], axis=AX.X), reads=[ge], writes=[gsum])
        S.op("dve", lambda e: e.reciprocal(out=gsum[:], in_=gsum[:]), reads=[gsum], writes=[gsum])
        S.op("dve", lambda e: e.tensor_tensor(out=GS[:].rearrange("q (h k) -> q h k", k=16), in0=ge[:],
                                              in1=gsum[:].unsqueeze(2).to_broadcast([128, 8, 16]), op=ALU.mult),
             reads=[ge, gsum], writes=[GS])
    def stage_UV(t, nxt):
        nonlocal gi, di
        i2 = t % NB
        X, HB, EI, GS = xs[i2], hb[i2], eidx[i2], gs[i2]
        gate = gates[t // tiles_per_batch]
        for grp in range(32):
            AG, WG = actg[grp % 4], wvg[grp % 4]
            UVs = []
            for j in range(4):
                s = grp * 4 + j
                UV = ug[gi % NG]
                gi += 1
                UVs.append(UV)
                S.dma("pool", lambda e: e.indirect_dma_start(out=UV[:], out_offset=None, in_=puv_d,
                                                             in_offset=bass.IndirectOffsetOnAxis(ap=EI[:, s:s + 1], axis=0)),
                      reads=[EI], writes=[UV])
                S.op("dve", lambda e: e.scalar_tensor_tensor(out=junkb[:], in0=UV[:, 0:D], scalar=1.0, in1=HB[:], op0=ALU.mult,
                                                             op1=ALU.mult, accum_out=AG[:, j:j + 1]),
                     reads=[UV, HB], writes=[junkb, AG])
            S.op("act", lambda e: e.activation(out=WG[:], in_=AG[:], func=AF.Gelu), reads=[AG], writes=[WG])
            for j in range(4):
                S.op("act", lambda e: e.mul(out=WG[:, j:j + 1], in_=WG[:, j:j + 1], mul=GS[:, grp * 4 + j:grp * 4 + j + 1]),
                     reads=[WG, GS], writes=[WG])
            for j in range(4):
                s = grp * 4 + j
                DG = dg[di % ND]
                di += 1
                S.op("act", lambda e: e.mul(out=DG[:], in_=identb[:], mul=WG[:, j:j + 1]), reads=[identb, WG], writes=[DG])
                for hh in range(2):
                    S.op("pe", lambda e: e.matmul(pout[hh][:], lhsT=DG[:], rhs=UVs[j][:, D + hh * 512:D + (hh + 1) * 512],
                                                  start=(s == 0), stop=(s == 127)), reads=[DG, UVs[j]], writes=[pout[hh]])
            for _ in range(6):
                next(nxt, None)
        Y = yo[i2]
        for hh in range(2):
            S.op("dve", lambda e: e.tensor_mul(out=Y[:, hh * 512:(hh + 1) * 512], in0=pout[hh][:],
                                               in1=gate[:, hh * 512:(hh + 1) * 512]), reads=[pout[hh], gate], writes=[Y])
        S.op("dve", lambda e: e.tensor_add(out=Y[:], in0=Y[:], in1=X[:]), reads=[Y, X], writes=[Y])
        S.dma("sp", lambda e: e.dma_start(out=x_out[t * 128:(t + 1) * 128, :], in_=Y[:]), reads=[Y], writes=[])
        for _ in nxt:
            pass

    g0 = stage_I(0)
    for _ in g0:
        pass
    for t in range(ntiles):
        nxt = stage_I(t + 1) if t + 1 < ntiles else iter(())
        stage_UV(t, nxt)


def emit_convert(S, nc, pairs, rows):
    NBUF = 4
    bufs = [S.sbuf(f"cv{i}", [128, 4, D], BF16) for i in range(NBUF)]
    i = 0
    for src, dst in pairs:
        sv = src.rearrange("(c p r) d -> c p r d", p=128, r=4)
        dv = dst.rearrange("(c p r) d -> c p r d", p=128, r=4)
        for c in range(rows // 512):
            B = bufs[i % NBUF]
            S.dma("pool", lambda e: e.dma_start(out=B[:], in_=sv[c]), writes=[B])
            S.dma(("sp", "act")[i % 2], lambda e: e.dma_start(out=dv[c], in_=B[:]), reads=[B], writes=[])
            i += 1


def emit_convert_inplace(S, nc, tabs, rows):
    NBUF = 4
    bufs = [S.sbuf(f"cv{i}", [128, 4, D], BF16) for i in range(NBUF)]
    views = []
    i = 0
    for tab in tabs:
        sv = tab.rearrange("(c p r) d -> c p r d", p=128, r=4)
        v16 = tab.bitcast(BF16).rearrange("n (two d) -> (n two) d", two=2)
        dv = v16[0:rows, :].rearrange("(c p r) d -> c p r d", p=128, r=4)
        views.append(v16)
        for c in range(rows // 512):
            B = bufs[i % NBUF]
            S.dma("pool", lambda e: e.dma_start(out=B[:], in_=sv[c]), writes=[B])
            S.dma(("sp", "act")[i % 2], lambda e: e.dma_start(out=dv[c], in_=B[:]), reads=[B], writes=[])
            i += 1
    return views


def emit_convert_uv(S, nc, tab_u, tab_v, rows):
    NBUF = 3
    bufs = [S.sbuf(f"cuv{i}", [128, 4, 2 * D], BF16) for i in range(NBUF)]
    su = tab_u.rearrange("(c p r) d -> c p r d", p=128, r=4)
    sv = tab_v.rearrange("(c p r) d -> c p r d", p=128, r=4)
    uv = tab_u.bitcast(BF16)
    dv = uv.rearrange("(c p r) d -> c p r d", p=128, r=4)
    for c in range(rows // 512):
        B = bufs[c % NBUF]
        S.dma("pool", lambda e: e.dma_start(out=B[:, :, 0:D], in_=su[c]), writes=[B])
        S.dma("pool", lambda e: e.dma_start(out=B[:, :, D:2 * D], in_=sv[c]), writes=[B])
        S.dma(("sp", "act")[c % 2], lambda e: e.dma_start(out=dv[c], in_=B[:]), reads=[B], writes=[])
    return uv


def gen_convert_uv(S, nc, tab_u, tab_v, rows):
    NBUF = 3
    bufs = [S.sbuf(f"cuv{i}", [128, 4, 2 * D], BF16) for i in range(NBUF)]
    su = tab_u.rearrange("(c p r) d -> c p r d", p=128, r=4)
    sv = tab_v.rearrange("(c p r) d -> c p r d", p=128, r=4)
    uv = tab_u.bitcast(BF16)
    dv = uv.rearrange("(c p r) d -> c p r d", p=128, r=4)
    for c in range(rows // 512):
        B = bufs[c % NBUF]
        S.dma("pool", lambda e: e.dma_start(out=B[:, :, 0:D], in_=su[c]), writes=[B])
        S.dma("pool", lambda e: e.dma_start(out=B[:, :, D:2 * D], in_=sv[c]), writes=[B])
        S.dma("sp", lambda e: e.dma_start(out=dv[c], in_=B[:]), reads=[B], writes=[])
        yield


import math


def emit_ada(S, nc, cT_d, ada_w_d, ada_b_d, kvw_d, kvb_d, mod_d):
    cT = S.sbuf("cT", [128, 8, 2], F32)
    S.dma("sp", lambda e: e.dma_start(out=cT[:], in_=cT_d), writes=[cT])
    cs = S.sbuf("cs", [128, 8, 2], F32)
    S.op("act", lambda e: e.activation(out=cs[:], in_=cT[:], func=AF.Silu), reads=[cT], writes=[cs])
    wch = [S.sbuf(f"wch{i}", [128, 8, 512], F32) for i in range(2)]
    bch = [S.sbuf(f"bch{i}", [2, 512], F32) for i in range(2)]
    och = [S.sbuf(f"och{i}", [2, 512], F32) for i in range(2)]
    pa = [S.psum(f"pada{i}", [128, 512], F32) for i in range(2)]
    jobs = []
    for l in range(2):
        for ch in range(12):
            jobs.append((ada_w_d[l], ada_b_d[l:l + 1, :], ch, 6 * l + ch // 2, ch % 2))
    for ch in range(4):
        jobs.append((kvw_d, kvb_d, ch, 12 + ch // 2, ch % 2))
    for i, (w_d, b_d, ch, row, half) in enumerate(jobs):
        W, B, O, P = wch[i % 2], bch[i % 2], och[i % 2], pa[i % 2]
        wv = w_d.rearrange("(k p) n -> p k n", p=128)
        q = ("sp", "act")[i % 2]
        S.dma(q, lambda e: e.dma_start(out=W[:], in_=wv[:, :, ch * 512:(ch + 1) * 512]), writes=[W])
        S.dma(q, lambda e: e.dma_start(out=B[:], in_=b_d[:, ch * 512:(ch + 1) * 512].partition_broadcast(2)), writes=[B])
        for kc in range(8):
            S.op("pe", lambda e: e.matmul(P[0:2, :], lhsT=cs[:, kc, :], rhs=W[:, kc, :], start=(kc == 0), stop=(kc == 7)),
                 reads=[cs, W], writes=[P])
        S.op("dve", lambda e: e.tensor_add(out=O[:], in0=P[0:2, :], in1=B[:]), reads=[P, B], writes=[O])
        S.dma(q, lambda e: e.dma_start(out=mod_d[:, row, half * 512:(half + 1) * 512], in_=O[:]), reads=[O], writes=[])


def emit_mlstm(S, nc, x_in, x_out, win_d, cwT_d, bif_d, hg_d, wout_d, gmix_row, mod_d, ntiles, tiles_per_seq,
               identb, utri, ones, side=None):
    win = S.sbuf("win", [128, 8, 3088], BF16)
    win_v = win_d.rearrange("(k p) n -> p k n", p=128)
    for kc in range(8):
        S.dma("pool", lambda e: e.dma_start(out=win[:, kc, 0:2048], in_=win_v[:, kc, 0:2048]), writes=[win])
        S.dma("pool", lambda e: e.dma_start(out=win[:, kc, 2048:3088], in_=win_v[:, kc, 2048:3088]), writes=[win])
    wout = S.sbuf("wout", [128, 8, 1024], BF16)
    wout_v = wout_d.rearrange("(k p) n -> p k n", p=128)
    for kc in range(8):
        S.dma("pool", lambda e: e.dma_start(out=wout[:, kc, :], in_=wout_v[:, kc, :]), writes=[wout])
    cwT = S.sbuf("cwT", [128, 8, 4], F32)
    S.dma("sp", lambda e: e.dma_start(out=cwT[:], in_=cwT_d), writes=[cwT])
    bif = S.sbuf("bif", [128, 16], F32)
    load_bcast(S, "sp", bif, bif_d)
    hg = S.sbuf("hg", [128, D], F32)
    load_bcast(S, "sp", hg, hg_d)
    grow = S.sbuf("grow", [128, D], F32)
    load_bcast(S, "sp", grow, gmix_row)
    gmod = S.sbuf("gmod", [128, D], F32)
    shift = S.sbuf("shift", [128, D], F32)
    gate = S.sbuf("gate", [128, D], F32)

    NB = 2
    xs = [S.sbuf(f"xs{i}", [128, D], F32) for i in range(NB)]
    hb = S.sbuf("hb", [128, D], BF16)
    hT = S.sbuf("hT", [128, 8, 128], BF16)
    junk = S.sbuf("junk", [128, D], F32)
    st = S.sbuf("st", [128, 4], F32)
    cb = S.sbuf("cb", [128, 8, 131], F32)
    cacc = S.sbuf("cacc", [128, 8, 128], F32)
    ctmp = S.sbuf("ctmp", [128, 8, 128], F32)
    qkT = S.sbuf("qkT", [128, 8, 128], BF16)
    ktok = S.sbuf("ktok", [128, 4, 128], BF16)
    qm = S.sbuf("qm", [128, 8, 128], BF16)
    S.op("pool", lambda e: e.memset(qm[:], 0.0), writes=[qm])
    gts = S.sbuf("gts", [128, 16], F32)
    spf = S.sbuf("spf", [128, 8], F32)
    wexp = S.sbuf("wexp", [128, 8], F32)
    eb = S.sbuf("eb", [128, 8], F32)
    ebL = S.sbuf("ebL", [128, 8], F32)
    vaug = S.sbuf("vaug", [128, 8, 129], BF16)
    sig = S.sbuf("sig", [128, D], F32)
    ATs = S.sbuf("ATs", [128, 8, 128], BF16)
    Cst = S.sbuf("Cst", [128, 8, 129], F32)
    Cbf = S.sbuf("Cbf", [128, 8, 129], BF16)
    den = S.sbuf("den", [128, 8], F32)
    hh = S.sbuf("hh", [128, 8, 128], F32)
    sq = S.sbuf("sq", [128, 8, 128], F32)
    hss = S.sbuf("hss", [128, 8], F32)
    hob = S.sbuf("hob", [128, D], BF16)
    hoT = S.sbuf("hoT", [128, 8, 128], BF16)
    yo = [S.sbuf(f"yo{i}", [128, D], F32) for i in range(NB)]

    pT = S.psum("pT", [128, 8, 128], BF16)
    pg = [S.psum(f"pg{i}", [128, 512], F32) for i in range(2)]
    pAT = [S.psum(f"pAT{i}", [128, 4, 128], F32) for i in range(2)]
    pOS = [S.psum(f"pOS{i}", [128, 3, 160], F32) for i in range(3)]
    pgi = 0

    def PO(h):
        return pOS[h // 3], h % 3

    LN8 = math.log(0.125)
    for t in range(ntiles):
        sidx = t // tiles_per_seq
        first = (t % tiles_per_seq == 0)
        X = xs[t % NB]
        if first:
            load_bcast(S, "sp", shift, mod_d[sidx, 0:1, :])
            load_bcast(S, "sp", gmod, mod_d[sidx, 1:2, :])
            load_bcast(S, "sp", gate, mod_d[sidx, 2:3, :])
            S.op("dve", lambda e: e.scalar_tensor_tensor(out=gmod[:], in0=gmod[:], scalar=1.0, in1=grow[:],
                                                         op0=ALU.add, op1=ALU.mult), reads=[gmod, grow], writes=[gmod])
            S.op("pool", lambda e: e.memset(cb[:, :, 0:3], 0.0), writes=[cb])
            S.op("pool", lambda e: e.memset(Cst[:], 0.0), writes=[Cst])
            S.op("pool", lambda e: e.memset(Cbf[:], 0.0), writes=[Cbf])
        S.dma("sp", lambda e: e.dma_start(out=X[:], in_=x_in[t * 128:(t + 1) * 128, :]), writes=[X])
        if side is not None:
            next(side, None)
            next(side, None)
        emit_norm_mod(S, X, gmod, shift, hb, st, junk)
        emit_transpose8(S, hb, pT, hT, identb)
        for half in range(2):
            P = pg[pgi % 2]
            pgi += 1
            for j in range(4):
                fc = half * 4 + j
                for kc in range(8):
                    S.op("pe", lambda e: e.matmul(P[:, j * 128:(j + 1) * 128], lhsT=win[:, kc, fc * 128:(fc + 1) * 128],
                                                  rhs=hT[:, kc, :], start=(kc == 0), stop=(kc == 7)),
                         reads=[win, hT], writes=[P])
            S.op("act", lambda e: e.copy(out=cb[:, half * 4:(half + 1) * 4, 3:131],
                                         in_=P[:].rearrange("p (j n) -> p j n", n=128)), reads=[P], writes=[cb])
        def cw(w):
            return cwT[:, :, w:w + 1].to_broadcast([128, 8, 128])
        S.op("dve", lambda e: e.tensor_tensor(out=cacc[:], in0=cb[:, :, 3:131], in1=cw(3), op=ALU.mult), reads=[cb, cwT], writes=[cacc])
        for w in range(3):
            S.op("pool", lambda e: e.tensor_tensor(out=ctmp[:], in0=cb[:, :, w:w + 128], in1=cw(w), op=ALU.mult),
                 reads=[cb, cwT], writes=[ctmp])
            S.op("dve", lambda e: e.tensor_add(out=cacc[:], in0=cacc[:], in1=ctmp[:]), reads=[cacc, ctmp], writes=[cacc])
        S.op("act", lambda e: e.activation(out=qkT[:, 4:8, :], in_=cacc[:, 4:8, :], func=AF.Silu), reads=[cacc], writes=[qkT])
        qm4 = qm[:].rearrange("p (f two) n -> p f two n", two=2)
        S.op("act", lambda e: e.activation(out=qm4[0:64, :, 0, :], in_=cacc[0:64, 0:4, :], func=AF.Silu), reads=[cacc], writes=[qm])
        S.op("act", lambda e: e.activation(out=qm4[64:128, :, 1, :], in_=cacc[64:128, 0:4, :], func=AF.Silu), reads=[cacc], writes=[qm])
        S.op("pool", lambda e: e.tensor_copy(out=cb[:, :, 0:3], in_=cb[:, :, 128:131]), reads=[cb], writes=[cb])
        for j in range(4):
            S.op("pe", lambda e: e.transpose(out=pT[:, j, :], in_=qkT[:, 4 + j, :], identity=identb[:]),
                 reads=[qkT, identb], writes=[pT])
        S.op("act", lambda e: e.copy(out=ktok[:], in_=pT[:, 0:4, :]), reads=[pT], writes=[ktok])
        P = pg[pgi % 2]
        pgi += 1
        for kc in range(8):
            S.op("pe", lambda e: e.matmul(P[:, 0:16], lhsT=hT[:, kc, :], rhs=win[:, kc, 3072:3088], start=(kc == 0), stop=(kc == 7)),
                 reads=[hT, win], writes=[P])
        S.op("dve", lambda e: e.tensor_add(out=gts[:], in0=P[:, 0:16], in1=bif[:]), reads=[P, bif], writes=[gts])
        S.op("act", lambda e: e.activation(out=spf[:], in_=gts[:, 8:16], func=AF.Exp, scale=-1.0), reads=[gts], writes=[spf])
        S.op("act", lambda e: e.activation(out=spf[:], in_=spf[:], func=AF.Ln, bias=1.0), reads=[spf], writes=[spf])
        S.op("pe", lambda e: e.matmul(P[:, 16:24], lhsT=utri[:], rhs=spf[:], start=True, stop=True), reads=[utri, spf], writes=[P])
        S.op("pe", lambda e: e.matmul(P[:, 32:40], lhsT=ones[:], rhs=spf[:], start=True, stop=True), reads=[ones, spf], writes=[P])
        S.op("dve", lambda e: e.tensor_add(out=wexp[:], in0=P[:, 16:24], in1=gts[:, 0:8]), reads=[P, gts], writes=[wexp])
        S.op("act", lambda e: e.activation(out=wexp[:], in_=wexp[:], func=AF.Exp), reads=[wexp], writes=[wexp])
        S.op("act", lambda e: e.activation(out=eb[:], in_=P[:, 16:24], func=AF.Exp, scale=-1.0, bias=LN8), reads=[P], writes=[eb])
        S.op("act", lambda e: e.activation(out=ebL[:], in_=P[:, 32:40], func=AF.Exp, scale=-1.0), reads=[P], writes=[ebL])
        for half in range(2):
            P = pg[pgi % 2]
            pgi += 1
            for kc in range(8):
                S.op("pe", lambda e: e.matmul(P[:], lhsT=hT[:, kc, :], rhs=win[:, kc, 1024 + half * 512:1024 + (half + 1) * 512],
                                              start=(kc == 0), stop=(kc == 7)), reads=[hT, win], writes=[P])
            S.op("dve", lambda e: e.tensor_tensor(out=vaug[:, half * 4:(half + 1) * 4, 0:128],
                                                  in0=P[:].rearrange("p (h n) -> p h n", n=128),
                                                  in1=wexp[:, half * 4:(half + 1) * 4].unsqueeze(2).to_broadcast([128, 4, 128]),
                                                  op=ALU.mult), reads=[P, wexp], writes=[vaug])
        S.op("pool", lambda e: e.tensor_copy(out=vaug[:, :, 128:129], in_=wexp[:].unsqueeze(2)), reads=[wexp], writes=[vaug])
        for half in range(2):
            P = pg[pgi % 2]
            pgi += 1
            for kc in range(8):
                S.op("pe", lambda e: e.matmul(P[:], lhsT=hT[:, kc, :], rhs=win[:, kc, 2048 + half * 512:2048 + (half + 1) * 512],
                                              start=(kc == 0), stop=(kc == 7)), reads=[hT, win], writes=[P])
            S.op("act", lambda e: e.activation(out=sig[:, half * 512:(half + 1) * 512], in_=P[:], func=AF.Sigmoid),
                 reads=[P], writes=[sig])
        for h in range(8):
            po, fc = (h % 2) * 64, h // 2
            S.op("pe", lambda e: e.matmul(pAT[h // 4][:, h % 4, :], lhsT=qkT[:, 4 + fc, :], rhs=qm[:, h, :],
                                          start=True, stop=True), reads=[qkT, qm], writes=[pAT[h // 4]])
        for half in range(2):
            S.op(("dve", "pool")[0], lambda e: e.tensor_tensor(out=ATs[:, half * 4:(half + 1) * 4, :], in0=pAT[half][:],
                                                  in1=utri[:].unsqueeze(1).to_broadcast([128, 4, 128]), op=ALU.mult),
                 reads=[pAT[half], utri], writes=[ATs])
        for h in range(8):
            po, fc = (h % 2) * 64, h // 2
            PB, sl = PO(h)
            S.op("pe", lambda e: e.matmul(PB[:, sl, 0:129], lhsT=ATs[:, h, :], rhs=vaug[:, h, :], start=True, stop=False),
                 reads=[ATs, vaug], writes=[PB])
            S.op("pe", lambda e: e.matmul(PB[:, sl, 0:129], lhsT=qm[:, h, :], rhs=Cbf[:, h, :],
                                          start=False, stop=True), reads=[qm, Cbf], writes=[PB])
        for bk in range(3):
            nh = 3 if bk < 2 else 2
            S.op("dve", lambda e: e.tensor_tensor(out=den[:, bk * 3:bk * 3 + nh].unsqueeze(2), in0=pOS[bk][:, 0:nh, 128:129],
                                                  in1=eb[:, bk * 3:bk * 3 + nh].unsqueeze(2), op=ALU.mult),
                 reads=[pOS[bk], eb], writes=[den])
        S.op("act", lambda e: e.activation(out=den[:], in_=den[:], func=AF.Abs), reads=[den], writes=[den])
        S.op("dve", lambda e: e.tensor_scalar_max(out=den[:], in0=den[:], scalar1=1.0), reads=[den], writes=[den])
        S.op("dve", lambda e: e.reciprocal(out=den[:], in_=den[:]), reads=[den], writes=[den])
        S.op("dve", lambda e: e.tensor_mul(out=den[:], in0=den[:], in1=eb[:]), reads=[den, eb], writes=[den])
        for bk in range(3):
            nh = 3 if bk < 2 else 2
            S.op("dve", lambda e: e.tensor_tensor(out=hh[:, bk * 3:bk * 3 + nh, :], in0=pOS[bk][:, 0:nh, 0:128],
                                                  in1=den[:, bk * 3:bk * 3 + nh].unsqueeze(2).to_broadcast([128, nh, 128]),
                                                  op=ALU.mult), reads=[pOS[bk], den], writes=[hh])
        for h in range(8):
            PB, sl = PO(h)
            fc = h // 2
            S.op("pe", lambda e: e.matmul(PB[:, sl, 0:129], lhsT=ktok[:, fc, :], rhs=vaug[:, h, :], start=True, stop=True),
                 reads=[ktok, vaug], writes=[PB])
        for bk in range(3):
            nh = 3 if bk < 2 else 2
            S.op("dve", lambda e: e.tensor_tensor(out=Cst[:, bk * 3:bk * 3 + nh, :], in0=pOS[bk][:, 0:nh, 0:129],
                                                  in1=Cst[:, bk * 3:bk * 3 + nh, :], op=ALU.add), reads=[pOS[bk], Cst], writes=[Cst])
        S.op("pool", lambda e: e.tensor_tensor(out=Cst[:], in0=Cst[:], in1=ebL[:].unsqueeze(2).to_broadcast([128, 8, 129]),
                                               op=ALU.mult), reads=[Cst, ebL], writes=[Cst])
        S.op("act", lambda e: e.copy(out=Cbf[:], in_=Cst[:]), reads=[Cst], writes=[Cbf])
        S.op("pool", lambda e: e.tensor_tensor(out=sq[:], in0=hh[:], in1=hh[:], op=ALU.mult), reads=[hh], writes=[sq])
        S.op("dve", lambda e: e.reduce_sum(out=hss[:], in_=sq[:], axis=AX.X), reads=[sq], writes=[hss])
        S.op("act", lambda e: e.activation(out=hss[:], in_=hss[:], func=AF.Sqrt, scale=1.0 / 128, bias=EPS), reads=[hss], writes=[hss])
        S.op("dve", lambda e: e.reciprocal(out=hss[:], in_=hss[:]), reads=[hss], writes=[hss])
        S.op("dve", lambda e: e.tensor_tensor(out=hh[:], in0=hh[:], in1=hss[:].unsqueeze(2).to_broadcast([128, 8, 128]), op=ALU.mult),
             reads=[hh, hss], writes=[hh])
        hh2 = hh[:].rearrange("p h n -> p (h n)")
        S.op("pool", lambda e: e.tensor_tensor(out=hh2, in0=hh2, in1=hg[:], op=ALU.mult), reads=[hh, hg], writes=[hh])
        S.op("dve", lambda e: e.tensor_tensor(out=hob[:], in0=hh2, in1=sig[:], op=ALU.mult), reads=[hh, sig], writes=[hob])
        emit_transpose8(S, hob, pT, hoT, identb)
        Y = yo[t % NB]
        for half in range(2):
            P = pg[pgi % 2]
            pgi += 1
            for kc in range(8):
                S.op("pe", lambda e: e.matmul(P[:], lhsT=hoT[:, kc, :], rhs=wout[:, kc, half * 512:(half + 1) * 512],
                                              start=(kc == 0), stop=(kc == 7)), reads=[hoT, wout], writes=[P])
            S.op("dve", lambda e: e.tensor_mul(out=Y[:, half * 512:(half + 1) * 512], in0=P[:], in1=gate[:, half * 512:(half + 1) * 512]),
                 reads=[P, gate], writes=[Y])
        S.op("pool", lambda e: e.tensor_add(out=Y[:], in0=Y[:], in1=X[:]), reads=[Y, X], writes=[Y])
        S.dma("sp", lambda e: e.dma_start(out=x_out[t * 128:(t + 1) * 128, :], in_=Y[:]), reads=[Y], writes=[])


def drain(gen):
    if gen is not None:
        for _ in gen:
            pass


def emit_headnorm(S, src, gsm, dst, sq, hss, nh, hd):
    s3 = src[:].rearrange("p (h d) -> p h d", d=hd)
    q3 = sq[:].rearrange("p (h d) -> p h d", d=hd)
    d3 = dst[:].rearrange("p (h d) -> p h d", d=hd)
    S.op("pool", lambda e: e.tensor_tensor(out=sq[:], in0=src[:], in1=src[:], op=ALU.mult), reads=[src], writes=[sq])
    S.op("dve", lambda e: e.reduce_sum(out=hss[:], in_=q3, axis=AX.X), reads=[sq], writes=[hss])
    S.op("act", lambda e: e.activation(out=hss[:], in_=hss[:], func=AF.Sqrt, scale=1.0 / hd, bias=EPS), reads=[hss], writes=[hss])
    S.op("dve", lambda e: e.reciprocal(out=hss[:], in_=hss[:]), reads=[hss], writes=[hss])
    S.op("dve", lambda e: e.tensor_tensor(out=q3, in0=s3, in1=hss[:].unsqueeze(2).to_broadcast([128, nh, hd]), op=ALU.mult),
         reads=[src, hss], writes=[sq])
    S.op("pool", lambda e: e.tensor_tensor(out=d3, in0=q3, in1=gsm[:].unsqueeze(1).to_broadcast([128, nh, hd]), op=ALU.mult),
         reads=[sq, gsm], writes=[dst])


def emit_sb(S, nc, x_in, x_out, kvg_row, kvw_d, kng_row, gmix_row, wq_d, qng_row, wout_d, mod_d, nseq, TPS,
            identb, sutri, sltri, ones):
    NH, HD = 16, 64
    kT_all = S.sbuf("kT_all", [128, 8, TPS * 128], BF16)
    v_all = S.sbuf("v_all", [128, TPS, D], BF16)
    xs = [S.sbuf(f"xs{i}", [128, D], F32) for i in range(2)]
    hb = S.sbuf("hb", [128, D], BF16)
    hT = S.sbuf("hT", [128, 8, 128], BF16)
    junk = S.sbuf("junk", [128, D], F32)
    st = S.sbuf("st", [128, 4], F32)
    grow = S.sbuf("grow", [128, D], F32)
    gmod = S.sbuf("gmod", [128, D], F32)
    shift = S.sbuf("shift", [128, D], F32)
    gsm = S.sbuf("gsm", [128, HD], F32)
    pf = S.sbuf("pf", [128, D], F32)
    sq = S.sbuf("sq", [128, D], F32)
    hss = S.sbuf("hss", [128, NH], F32)
    pb = S.sbuf("pb", [128, D], BF16)
    pT = S.psum("pT", [128, 8, 128], BF16)
    pg = S.psum("pg", [128, 512], F32)

    for sidx in range(nseq):
        t0 = sidx * TPS
        with S.scope():
            kvw = S.sbuf("kvw", [128, 8, 2048], BF16)
            kvw_v = kvw_d.rearrange("(k p) n -> p k n", p=128)
            for kc in range(8):
                S.dma("pool", lambda e: e.dma_start(out=kvw[:, kc, :], in_=kvw_v[:, kc, :]), writes=[kvw])
            load_bcast(S, "sp", grow, kvg_row)
            load_bcast(S, "sp", shift, mod_d[sidx, 12:13, :])
            load_bcast(S, "sp", gmod, mod_d[sidx, 13:14, :])
            load_bcast(S, "sp", gsm, kng_row)
            S.op("dve", lambda e: e.scalar_tensor_tensor(out=gmod[:], in0=gmod[:], scalar=1.0, in1=grow[:],
                                                         op0=ALU.add, op1=ALU.mult), reads=[gmod, grow], writes=[gmod])
            for t in range(TPS):
                X = xs[t % 2]
                S.dma("sp", lambda e: e.dma_start(out=X[:], in_=x_in[(t0 + t) * 128:(t0 + t + 1) * 128, :]), writes=[X])
                emit_norm_mod(S, X, gmod, shift, hb, st, junk)
                emit_transpose8(S, hb, pT, hT, identb)
                for ch in range(4):
                    for kc in range(8):
                        S.op("pe", lambda e: e.matmul(pg[:], lhsT=hT[:, kc, :], rhs=kvw[:, kc, ch * 512:(ch + 1) * 512],
                                                      start=(kc == 0), stop=(kc == 7)), reads=[hT, kvw], writes=[pg])
                    if ch < 2:
                        S.op("act", lambda e: e.copy(out=pf[:, ch * 512:(ch + 1) * 512], in_=pg[:]), reads=[pg], writes=[pf])
                    else:
                        S.op("act", lambda e: e.copy(out=v_all[:, t, (ch - 2) * 512:(ch - 1) * 512], in_=pg[:]), reads=[pg], writes=[v_all])
                emit_headnorm(S, pf, gsm, pb, sq, hss, NH, HD)
                for kc in range(8):
                    S.op("pe", lambda e: e.transpose(out=pT[:, kc, :], in_=pb[:, kc * 128:(kc + 1) * 128], identity=identb[:]),
                         reads=[pb, identb], writes=[pT])
                S.op("act", lambda e: e.copy(out=kT_all[:, :, t * 128:(t + 1) * 128], in_=pT[:]), reads=[pT], writes=[kT_all])
        with S.scope():
            wq = S.sbuf("wq", [128, 8, D], BF16)
            wout = S.sbuf("wout", [128, 8, D], BF16)
            wq_v = wq_d.rearrange("(k p) n -> p k n", p=128)
            wout_v = wout_d.rearrange("(k p) n -> p k n", p=128)
            for kc in range(8):
                S.dma("pool", lambda e: e.dma_start(out=wq[:, kc, :], in_=wq_v[:, kc, :]), writes=[wq])
                S.dma("pool", lambda e: e.dma_start(out=wout[:, kc, :], in_=wout_v[:, kc, :]), writes=[wout])
            gate = S.sbuf("gate", [128, D], F32)
            load_bcast(S, "sp", grow, gmix_row)
            load_bcast(S, "sp", shift, mod_d[sidx, 6:7, :])
            load_bcast(S, "sp", gmod, mod_d[sidx, 7:8, :])
            load_bcast(S, "sp", gate, mod_d[sidx, 8:9, :])
            load_bcast(S, "sp", gsm, qng_row)
            S.op("dve", lambda e: e.scalar_tensor_tensor(out=gmod[:], in0=gmod[:], scalar=1.0, in1=grow[:],
                                                         op0=ALU.add, op1=ALU.mult), reads=[gmod, grow], writes=[gmod])
            S.op("dve", lambda e: e.tensor_scalar_mul(out=gsm[:], in0=gsm[:], scalar1=HD ** -0.5), reads=[gsm], writes=[gsm])
            qm = S.sbuf("qm", [128, NH, 128], BF16)
            S.op("pool", lambda e: e.memset(qm[:], 0.0), writes=[qm])
            qm4 = qm[:].rearrange("p (f two) n -> p f two n", two=2)
            E = [S.sbuf(f"E{i}", [128, 512], F32) for i in range(4)]
            SP = [S.sbuf(f"SP{i}", [128, 512], F32) for i in range(4)]
            ARG = [S.sbuf(f"ARG{i}", [128, 512], F32) for i in range(2)]
            XB = [S.sbuf(f"XB{i}", [128, 512], F32) for i in range(2)]
            A = [S.sbuf(f"A{i}", [128, 512], BF16) for i in range(3)]
            SPcum = [S.sbuf(f"SPcum{i}", [128, 512], F32) for i in range(4)]
            yo = [S.sbuf(f"yo{i}", [128, D], F32) for i in range(2)]
            oT = S.sbuf("oT", [128, 8, 128], BF16)
            pz = [S.psum(f"pz{i}", [128, 4, 128], F32) for i in range(2)]
            pnb = [S.psum(f"pnb{i}", [128, 512], F32) for i in range(2)]
            po = [S.psum(f"po{i}", [128, 512], F32) for i in range(2)]
            m3 = sutri[:].unsqueeze(1).to_broadcast([128, 4, 128])
            for qt in range(TPS):
                X = xs[qt % 2]
                S.dma("sp", lambda e: e.dma_start(out=X[:], in_=x_in[(t0 + qt) * 128:(t0 + qt + 1) * 128, :]), writes=[X])
                emit_norm_mod(S, X, gmod, shift, hb, st, junk)
                emit_transpose8(S, hb, pT, hT, identb)
                for ch in range(2):
                    for kc in range(8):
                        S.op("pe", lambda e: e.matmul(pg[:], lhsT=hT[:, kc, :], rhs=wq[:, kc, ch * 512:(ch + 1) * 512],
                                                      start=(kc == 0), stop=(kc == 7)), reads=[hT, wq], writes=[pg])
                    S.op("act", lambda e: e.copy(out=pf[:, ch * 512:(ch + 1) * 512], in_=pg[:]), reads=[pg], writes=[pf])
                emit_headnorm(S, pf, gsm, pb, sq, hss, NH, HD)
                for kc in range(8):
                    S.op("pe", lambda e: e.transpose(out=pT[:, kc, :], in_=pb[:, kc * 128:(kc + 1) * 128], identity=identb[:]),
                         reads=[pb, identb], writes=[pT])
                S.op("act", lambda e: e.copy(out=qm4[0:64, :, 0, :], in_=pT[0:64, :, :]), reads=[pT], writes=[qm])
                S.op("act", lambda e: e.copy(out=qm4[64:128, :, 1, :], in_=pT[64:128, :, :]), reads=[pT], writes=[qm])
                items = [(kt, g) for kt in range(qt, -1, -1) for g in range(4)]
                NI = len(items)

                def S1(i):
                    kt, g = items[i]
                    Z = pz[i % 2]
                    for j in range(4):
                        h = 4 * g + j
                        S.op("pe", lambda e: e.matmul(Z[:, j, :], lhsT=kT_all[:, h // 2, kt * 128:(kt + 1) * 128], rhs=qm[:, h, :],
                                                      start=True, stop=True), reads=[kT_all, qm], writes=[Z])

                def S2(i):
                    kt, g = items[i]
                    Z, Eb, SPb = pz[i % 2], E[i % 4], SP[i % 4]
                    Z2 = Z[:].rearrange("p j n -> p (j n)")
                    S.op("act", lambda e: e.activation(out=Eb[:], in_=Z2, func=AF.Exp), reads=[Z], writes=[Eb])
                    S.op("act", lambda e: e.activation(out=SPb[:], in_=Eb[:], func=AF.Ln, bias=1.0), reads=[Eb], writes=[SPb])
                    if kt == qt:
                        S.op("dve", lambda e: e.tensor_tensor(out=SPb[:].rearrange("p (j n) -> p j n", n=128),
                                                              in0=SPb[:].rearrange("p (j n) -> p j n", n=128), in1=m3, op=ALU.mult),
                             reads=[SPb, sutri], writes=[SPb])

                def S3(i):
                    kt, g = items[i]
                    diag = (kt == qt)
                    NBp, SPb = pnb[i % 2], SP[i % 4]
                    S.op("pe", lambda e: e.matmul(NBp[:], lhsT=sltri[:], rhs=SPb[:], start=True, stop=diag), reads=[sltri, SPb], writes=[NBp])
                    if not diag:
                        S.op("pe", lambda e: e.matmul(NBp[:], lhsT=ones[:], rhs=SPcum[g][:], start=False, stop=True),
                             reads=[ones, SPcum[g]], writes=[NBp])
                    if kt > 0:
                        if diag:
                            S.op("pool", lambda e: e.tensor_copy(out=SPcum[g][:], in_=SPb[:]), reads=[SPb], writes=[SPcum[g]])
                        else:
                            S.op("pool", lambda e: e.tensor_add(out=SPcum[g][:], in0=SPcum[g][:], in1=SPb[:]), reads=[SPb, SPcum[g]], writes=[SPcum[g]])

                def S4(i):
                    kt, g = items[i]
                    diag = (kt == qt)
                    NBp, Eb, SPb, ARGb, Xb, Ab = pnb[i % 2], E[i % 4], SP[i % 4], ARG[i % 2], XB[i % 2], A[i % 3]
                    S.op("dve", lambda e: e.tensor_tensor(out=ARGb[:], in0=SPb[:], in1=NBp[:], op=ALU.add), reads=[SPb, NBp], writes=[ARGb])
                    S.op("act", lambda e: e.activation(out=Xb[:], in_=ARGb[:], func=AF.Exp, scale=-1.0), reads=[ARGb], writes=[Xb])
                    if diag:
                        S.op("dve", lambda e: e.tensor_tensor(out=ARGb[:], in0=Xb[:], in1=Eb[:], op=ALU.mult), reads=[Xb, Eb], writes=[ARGb])
                        S.op("pool", lambda e: e.tensor_tensor(out=Ab[:].rearrange("p (j n) -> p j n", n=128),
                                                               in0=ARGb[:].rearrange("p (j n) -> p j n", n=128), in1=m3, op=ALU.mult),
                             reads=[ARGb, sutri], writes=[Ab])
                    else:
                        S.op("dve", lambda e: e.tensor_tensor(out=Ab[:], in0=Xb[:], in1=Eb[:], op=ALU.mult), reads=[Xb, Eb], writes=[Ab])

                def S5(i):
                    kt, g = items[i]
                    Ab = A[i % 3]
                    for j in range(4):
                        h = 4 * g + j
                        PO = po[h // 8]
                        S.op("pe", lambda e: e.matmul(PO[:, (h % 8) * 64:(h % 8 + 1) * 64], lhsT=Ab[:, j * 128:(j + 1) * 128],
                                                      rhs=v_all[:, kt, h * 64:(h + 1) * 64], start=(kt == qt and h % 8 == 0), stop=(kt == 0)),
                             reads=[Ab, v_all], writes=[PO])

                for n in range(NI + 4):
                    if n < NI:
                        S1(n)
                    if 0 <= n - 1 < NI:
                        S2(n - 1)
                    if 0 <= n - 2 < NI:
                        S3(n - 2)
                    if 0 <= n - 3 < NI:
                        S4(n - 3)
                    if 0 <= n - 4 < NI:
                        S5(n - 4)
                for half in range(2):
                    S.op("act", lambda e: e.copy(out=pb[:, half * 512:(half + 1) * 512], in_=po[half][:]), reads=[po[half]], writes=[pb])
                emit_transpose8(S, pb, pT, oT, identb)
                Y = yo[qt % 2]
                for half in range(2):
                    for kc in range(8):
                        S.op("pe", lambda e: e.matmul(pg[:], lhsT=oT[:, kc, :], rhs=wout[:, kc, half * 512:(half + 1) * 512],
                                                      start=(kc == 0), stop=(kc == 7)), reads=[oT, wout], writes=[pg])
                    S.op("dve", lambda e: e.tensor_mul(out=Y[:, half * 512:(half + 1) * 512], in0=pg[:], in1=gate[:, half * 512:(half + 1) * 512]),
                         reads=[pg, gate], writes=[Y])
                S.op("pool", lambda e: e.tensor_add(out=Y[:], in0=Y[:], in1=X[:]), reads=[Y, X], writes=[Y])
                S.dma("sp", lambda e: e.dma_start(out=x_out[(t0 + qt) * 128:(t0 + qt + 1) * 128, :], in_=Y[:]), reads=[Y], writes=[])


NCORES = 8
SEQ = 2048
TPS = SEQ // 128
NSEQ = 2
NT = NSEQ * TPS
_NC_CACHE = {}


def build_program():
    nc = bass.Bass("TRN2", target_bir_lowering=False)

    def din(name, shape):
        return nc.dram_tensor(name, list(shape), F32, kind="ExternalInput").ap()

    x = din("x", [NT * 128, D])
    cT = din("cT", [128, 8, 2])
    ident = din("ident", [128, 128]); utri_d = din("utri", [128, 128]); sutri_d = din("sutri", [128, 128])
    sltri_d = din("sltri", [128, 128]); ones_d = din("ones", [128, 128]); iota_d = din("iota16", [128, 16])
    ada_w = din("ada_w", [2, D, 6 * D]); ada_b = din("ada_b", [2, 6 * D])
    norm_mix_g = din("norm_mix_g", [2, D]); norm_ffn_g = din("norm_ffn_g", [2, D])
    ma_w_in = din("ma_w_in", [D, 3088]); cwT = din("cwT", [128, 8, 4]); ma_b_if = din("ma_b_if", [1, 16])
    ma_hnorm_g = din("ma_hnorm_g", [1, D]); ma_w_out = din("ma_w_out", [D, D])
    kv_ada_w = din("kv_ada_w", [D, 2 * D]); kv_ada_b = din("kv_ada_b", [1, 2 * D]); kv_norm_g = din("kv_norm_g", [1, D])
    kv_w = din("kv_w", [D, 2 * D]); k_norm_g = din("k_norm_g", [1, 64])
    sb_w_q = din("sb_w_q", [D, D]); sb_q_norm_g = din("sb_q_norm_g", [1, 64]); sb_w_out = din("sb_w_out", [D, D])
    peer_w_q = din("peer_w_q", [2, D, 2 * D]); peer_sub_keys = din("peer_sub_keys", [2, 2, 128, 128])
    peer_u = din("peer_u", [2, 16384, D]); peer_v = din("peer_v", [2, 16384, D])
    pu_flat = peer_u.rearrange("l e d -> (l e) d")
    pv_flat = peer_v.rearrange("l e d -> (l e) d")
    out = nc.dram_tensor("out", [NT * 128, D], F32, kind="ExternalOutput").ap()
    mod = nc.dram_tensor("mod_scr", [2, 14, D], F32, kind="Internal").ap()
    xa = nc.dram_tensor("xa_scr", [NT * 128, D], F32, kind="Internal").ap()
    xb = nc.dram_tensor("xb_scr", [NT * 128, D], F32, kind="Internal").ap()
    xc = nc.dram_tensor("xc_scr", [NT * 128, D], F32, kind="Internal").ap()

    with ExitStack() as es:
        S = Sched(nc, es)
        identb = S.sbuf("identb", [128, 128], BF16)
        S.dma("pool", lambda e: e.dma_start(out=identb[:], in_=ident), writes=[identb])
        cs = {}
        for nm, d in (("utri", utri_d), ("sutri", sutri_d), ("sltri", sltri_d), ("ones", ones_d)):
            cs[nm] = S.sbuf(nm, [128, 128], F32)
            S.dma("sp", lambda e: e.dma_start(out=cs[nm][:], in_=d), writes=[cs[nm]])
        iota16 = S.sbuf("iota16", [128, 16], F32)
        S.dma("sp", lambda e: e.dma_start(out=iota16[:], in_=iota_d), writes=[iota16])
        puv = pu_flat.bitcast(BF16)
        with S.scope():
            emit_ada(S, nc, cT, ada_w, ada_b, kv_ada_w, kv_ada_b, mod)
        with S.scope():
            side = gen_convert_uv(S, nc, pu_flat, pv_flat, 32768)
            emit_mlstm(S, nc, x, xa, ma_w_in, cwT, ma_b_if, ma_hnorm_g, ma_w_out, norm_mix_g[0:1, :], mod, NT, TPS,
                       identb, cs["utri"], cs["ones"], side)
            drain(side)
        with S.scope():
            emit_peer(S, nc, xa, xb, peer_w_q[0], peer_sub_keys[0], puv, norm_ffn_g[0:1, :], mod, 3, NT, TPS, identb, iota16, 0)
        with S.scope():
            emit_sb(S, nc, xb, xc, kv_norm_g, kv_w, k_norm_g, norm_mix_g[1:2, :], sb_w_q, sb_q_norm_g, sb_w_out, mod, NSEQ, TPS,
                    identb, cs["sutri"], cs["sltri"], cs["ones"])
        with S.scope():
            emit_peer(S, nc, xc, out, peer_w_q[1], peer_sub_keys[1], puv, norm_ffn_g[1:2, :], mod, 9, NT, TPS, identb, iota16, 16384)
        S.barrier()
    return nc


def kernel(x, c, ada_w, ada_b, norm_mix_g, norm_ffn_g, ma_w_in, ma_conv_w, ma_b_if, ma_hnorm_g, ma_w_out,
           kv_ada_w, kv_ada_b, kv_norm_g, kv_w, k_norm_g, sb_w_q, sb_q_norm_g, sb_w_out,
           peer_w_q, peer_sub_keys, peer_u, peer_v):
    f = lambda a: np.ascontiguousarray(np.asarray(a, dtype=np.float32))
    x = f(x); c = f(c)
    one = np.ones((128, 128), np.float32)
    shared = {
        "ident": np.eye(128, dtype=np.float32), "utri": np.triu(one), "sutri": np.triu(one, 1), "sltri": np.tril(one, -1), "ones": one,
        "iota16": np.tile(np.arange(16, dtype=np.float32), (128, 1)),
        "ada_w": f(ada_w), "ada_b": f(ada_b), "norm_mix_g": f(norm_mix_g), "norm_ffn_g": f(norm_ffn_g),
        "ma_w_in": f(ma_w_in)[0], "cwT": np.ascontiguousarray(f(ma_conv_w)[0].reshape(4, 8, 128).transpose(2, 1, 0)),
        "ma_b_if": f(ma_b_if).reshape(1, 16), "ma_hnorm_g": f(ma_hnorm_g).reshape(1, D), "ma_w_out": f(ma_w_out)[0],
        "kv_ada_w": f(kv_ada_w), "kv_ada_b": f(kv_ada_b).reshape(1, 2 * D), "kv_norm_g": f(kv_norm_g).reshape(1, D),
        "kv_w": f(kv_w), "k_norm_g": f(k_norm_g).reshape(1, 64),
        "sb_w_q": f(sb_w_q)[0], "sb_q_norm_g": f(sb_q_norm_g).reshape(1, 64), "sb_w_out": f(sb_w_out)[0],
        "peer_w_q": f(peer_w_q), "peer_sub_keys": f(peer_sub_keys), "peer_u": f(peer_u), "peer_v": f(peer_v),
    }
    in_maps = []
    for i in range(NCORES):
        m = dict(shared)
        m["x"] = x[NSEQ * i:NSEQ * (i + 1)].reshape(NT * 128, D)
        m["cT"] = np.ascontiguousarray(c[NSEQ * i:NSEQ * (i + 1)].T.reshape(8, 128, NSEQ).transpose(1, 0, 2))
        in_maps.append(m)
    if "nc" not in _NC_CACHE:
        _NC_CACHE["nc"] = build_program()
    res = run_bass_kernel_spmd(_NC_CACHE["nc"], in_maps, core_ids=list(range(NCORES)))
    return np.concatenate([r["out"].reshape(NSEQ, SEQ, D) for r in res.results], axis=0)
```

```python
import numpy as np
from contextlib import ExitStack
import concourse.bass as bass
import concourse.mybir as mybir
from concourse.bass_utils import run_bass_kernel_spmd

F32 = mybir.dt.float32
BF16 = mybir.dt.bfloat16
U32 = mybir.dt.uint32
I32 = mybir.dt.int32
AF = mybir.ActivationFunctionType
ALU = mybir.AluOpType
AX = mybir.AxisListType


class Buf:
    __slots__ = ("name", "w", "r", "t")

    def __init__(self, name, t=None):
        self.name = name
        self.w = None
        self.r = {}
        self.t = t

    def __getitem__(self, idx):
        return self.t[idx]


class Sched:
    NDMA = 12

    def __init__(self, nc, es):
        self.nc = nc
        self.es = es
        self.engs = {"pe": nc.tensor, "act": nc.scalar, "dve": nc.vector, "pool": nc.gpsimd, "sp": nc.sync}
        self.sem = {}
        self.cnt = {}
        for k in ("pe", "act", "dve", "pool"):
            self.sem[k] = es.enter_context(nc.semaphore("s_" + k))
            self.cnt[k] = 0
        self.dq = {}
        for q in ("sp", "act", "pool"):
            sems = [es.enter_context(nc.semaphore(f"d_{q}{i}")) for i in range(self.NDMA)]
            self.dq[q] = {"sems": sems, "n": 0}
            for i in range(self.NDMA):
                self.sem[("dma", q, i)] = sems[i]
                self.cnt[("dma", q, i)] = 0
        self.waited = {e: {} for e in self.engs}
        self.nins = 0

    def sbuf(self, name, shape, dt):
        self.nins += 0
        self._uid = getattr(self, "_uid", 0) + 1
        name = f"sb{self._uid}_{name}"
        t = self.es.enter_context(self.nc.sbuf_tensor(name, list(shape), dt))
        return Buf(name, t)

    def psum(self, name, shape, dt=F32):
        self._uid = getattr(self, "_uid", 0) + 1
        name = f"ps{self._uid}_{name}"
        t = self.es.enter_context(self.nc.psum_tensor(name, list(shape), dt))
        return Buf(name, t)

    def view(self, name):
        return Buf(name)

    def scope(self):
        return _Scope(self)

    def _wait(self, engine, key, c):
        if c <= 0:
            return
        w = self.waited[engine]
        if w.get(key, 0) >= c:
            return
        self.engs[engine].wait_ge(self.sem[key], c)
        w[key] = c

    def _deps(self, engine, reads, writes):
        deps = {}
        for b in reads:
            if b.w is not None:
                k, c = b.w
                deps[k] = max(deps.get(k, 0), c)
        for b in writes:
            if b.w is not None:
                k, c = b.w
                deps[k] = max(deps.get(k, 0), c)
            for k, c in b.r.items():
                deps[k] = max(deps.get(k, 0), c)
        return deps

    def op(self, engine, fn, reads=(), writes=()):
        deps = self._deps(engine, reads, writes)
        for k, c in deps.items():
            if k == engine and engine == "pe":
                continue
            self._wait(engine, k, c)
        ins = fn(self.engs[engine])
        self.cnt[engine] += 1
        ins.then_inc(self.sem[engine], 1)
        me = (engine, self.cnt[engine])
        for b in reads:
            b.r[engine] = self.cnt[engine]
        for b in writes:
            b.w = me
            b.r = {}
        self.nins += 1
        return ins

    def dma(self, q, fn, reads=(), writes=()):
        d = self.dq[q]
        i = d["n"] % self.NDMA
        d["n"] += 1
        key = ("dma", q, i)
        deps = self._deps(q, reads, writes)
        deps[key] = max(deps.get(key, 0), self.cnt[key])
        for k, c in deps.items():
            self._wait(q, k, c)
        ins = fn(self.engs[q])
        self.cnt[key] += 16
        ins.then_inc(self.sem[key], 16)
        me = (key, self.cnt[key])
        for b in reads:
            b.r[key] = self.cnt[key]
        for b in writes:
            b.w = me
            b.r = {}
        self.nins += 1
        return ins

    def finish(self, bufs):
        for b in bufs:
            if b.w is not None:
                k, c = b.w
                for e in ("sp", "act", "pool", "dve", "pe"):
                    self._wait(e, k, c)

    def barrier(self):
        for e in self.engs:
            for k, c in self.cnt.items():
                if c > 0 and k != e:
                    self._wait(e, k, c)


class _Scope:
    def __init__(self, S):
        self.S = S

    def __enter__(self):
        self.old = self.S.es
        self.es2 = ExitStack()
        self.es2.__enter__()
        self.S.es = self.es2
        return self

    def __exit__(self, *a):
        self.S.barrier()
        self.S.es = self.old
        return self.es2.__exit__(*a)


EPS = 1e-6
D = 1024


def load_bcast(S, q, dst, row_ap):
    S.dma(q, lambda e: e.dma_start(out=dst[:], in_=row_ap.partition_broadcast(128)), writes=[dst])


def emit_norm_mod(S, xs, gmod, shift, hb, st, junk):
    S.op("act", lambda e: e.activation(out=junk[:], in_=xs[:], func=AF.Square, accum_out=st[:, 0:1]),
         reads=[xs], writes=[junk, st])
    S.op("act", lambda e: e.activation(out=st[:, 1:2], in_=st[:, 0:1], func=AF.Sqrt, scale=1.0 / D, bias=EPS),
         reads=[st], writes=[st])
    S.op("dve", lambda e: e.reciprocal(out=st[:, 2:3], in_=st[:, 1:2]), reads=[st], writes=[st])
    S.op("dve", lambda e: e.scalar_tensor_tensor(out=junk[:], in0=xs[:], scalar=st[:, 2:3], in1=gmod[:],
                                                 op0=ALU.mult, op1=ALU.mult), reads=[xs, st, gmod], writes=[junk])
    S.op("dve", lambda e: e.tensor_add(out=hb[:], in0=junk[:], in1=shift[:]), reads=[junk, shift], writes=[hb])


def emit_transpose8(S, hb, pT, hT, identb):
    for kc in range(8):
        S.op("pe", lambda e: e.transpose(out=pT[:, kc, :], in_=hb[:, kc * 128:(kc + 1) * 128], identity=identb[:]),
             reads=[hb, identb], writes=[pT])
    S.op("act", lambda e: e.copy(out=hT[:], in_=pT[:]), reads=[pT], writes=[hT])


def emit_peer(S, nc, x_in, x_out, wq_d, sk_d, puv_d, gffn_row, mod_d, mod_base, ntiles, tiles_per_batch,
              identb, iota16, idx_base=0):
    wq = S.sbuf("wq", [128, 8, 2048], BF16)
    wq_v = wq_d.rearrange("(k p) n -> p k n", p=128)
    for kc in range(8):
        S.dma("pool", lambda e: e.dma_start(out=wq[:, kc, :], in_=wq_v[:, kc, :]), writes=[wq])
    skn = S.sbuf("skn", [128, 2, 128], BF16)
    S.dma("pool", lambda e: e.dma_start(out=skn[:], in_=sk_d.rearrange("p n k -> n p k")), writes=[skn])
    skT = S.sbuf("skT", [128, 2, 128], BF16)
    pT = S.psum("pT", [128, 8, 128], BF16)
    for p in range(2):
        S.op("pe", lambda e: e.transpose(out=pT[:, p, :], in_=skn[:, p, :], identity=identb[:]),
             reads=[skn, identb], writes=[pT])
    S.op("act", lambda e: e.copy(out=skT[:], in_=pT[:, 0:2, :]), reads=[pT], writes=[skT])

    grow = S.sbuf("grow", [128, D], F32)
    load_bcast(S, "sp", grow, gffn_row)
    gmod = S.sbuf("gmod", [128, D], F32)
    shift = S.sbuf("shift", [128, D], F32)
    nbatch = (ntiles + tiles_per_batch - 1) // tiles_per_batch
    gates = [S.sbuf(f"gate{i}", [128, D], F32) for i in range(nbatch)]
    for i in range(nbatch):
        load_bcast(S, "sp", gates[i], mod_d[i, mod_base + 2:mod_base + 3, :])

    NB = 2
    xs = [S.sbuf(f"xs{i}", [128, D], F32) for i in range(NB)]
    hb = [S.sbuf(f"hb{i}", [128, D], BF16) for i in range(NB)]
    hT = [S.sbuf(f"hT{i}", [128, 8, 128], BF16) for i in range(NB)]
    junk = S.sbuf("junk", [128, D], F32)
    junkb = S.sbuf("junkb", [128, D], BF16)
    st = [S.sbuf(f"st{i}", [128, 4], F32) for i in range(NB)]
    qT = S.sbuf("qT", [128, 16, 128], BF16)
    pq = [S.psum(f"pq{i}", [128, 4, 128], F32) for i in range(2)]
    ps = [S.psum(f"ps{i}", [128, 4, 128], F32) for i in range(2)]
    pout = [S.psum(f"po{i}", [128, 512], F32) for i in range(2)]
    s_sb = S.sbuf("s_sb", [128, 16, 128], F32)
    s2 = S.sbuf("s2", [128, 16, 128], F32)
    top = S.sbuf("top", [128, 16, 16], F32)
    tix = S.sbuf("tix", [128, 16, 16], U32)
    tixf = S.sbuf("tixf", [128, 16, 16], F32)
    cand = S.sbuf("cand", [128, 8, 256], F32)
    cand2 = S.sbuf("cand2", [128, 8, 256], F32)
    pos = S.sbuf("pos", [128, 8, 16], U32)
    posf = S.sbuf("posf", [128, 8, 16], F32)
    ai = S.sbuf("ai", [128, 8, 16], I32)
    af = S.sbuf("af", [128, 8, 16], F32)
    bf_ = S.sbuf("bf_", [128, 8, 16], F32)
    ia = S.sbuf("ia", [128, 8, 16], F32)
    ja = S.sbuf("ja", [128, 8, 16], F32)
    oh = S.sbuf("oh", [128, 2048], F32)
    g = S.sbuf("g", [128, 8, 16], F32)
    ge = S.sbuf("ge", [128, 8, 16], F32)
    gsum = S.sbuf("gsum", [128, 8], F32)
    ef = S.sbuf("ef", [128, 128], F32)
    eidx = [S.sbuf(f"eidx{i}", [128, 128], I32) for i in range(NB)]
    gs = [S.sbuf(f"gs{i}", [128, 128], F32) for i in range(NB)]
    act = S.sbuf("act", [128, 128], F32)
    wv = S.sbuf("wv", [128, 128], F32)
    NG = 16
    ug = [S.sbuf(f"ug{i}", [128, 2 * D], BF16) for i in range(NG)]
    actg = [S.sbuf(f"actg{i}", [128, 4], F32) for i in range(4)]
    wvg = [S.sbuf(f"wvg{i}", [128, 4], F32) for i in range(4)]
    ND = 8
    dg = [S.sbuf(f"dg{i}", [128, 128], BF16) for i in range(ND)]
    yo = [S.sbuf(f"yo{i}", [128, D], F32) for i in range(NB)]
    gi = 0
    di = 0

    def stage_I(t):
        b = t // tiles_per_batch
        i2 = t % NB
        if t % tiles_per_batch == 0:
            load_bcast(S, "sp", shift, mod_d[b, mod_base + 0:mod_base + 1, :])
            load_bcast(S, "sp", gmod, mod_d[b, mod_base + 1:mod_base + 2, :])
            S.op("dve", lambda e: e.scalar_tensor_tensor(out=gmod[:], in0=gmod[:], scalar=1.0, in1=grow[:],
                                                         op0=ALU.add, op1=ALU.mult), reads=[gmod, grow], writes=[gmod])
        X, HB, HT, ST = xs[i2], hb[i2], hT[i2], st[i2]
        S.dma("sp", lambda e: e.dma_start(out=X[:], in_=x_in[t * 128:(t + 1) * 128, :]), writes=[X])
        yield
        emit_norm_mod(S, X, gmod, shift, HB, ST, junk)
        yield
        emit_transpose8(S, HB, pT, HT, identb)
        yield
        for gq in range(4):
            P = pq[gq % 2]
            for j in range(4):
                hp = gq * 4 + j
                for kc in range(8):
                    S.op("pe", lambda e: e.matmul(P[:, j, :], lhsT=wq[:, kc, hp * 128:(hp + 1) * 128], rhs=HT[:, kc, :],
                                                  start=(kc == 0), stop=(kc == 7)), reads=[wq, HT], writes=[P])
            S.op("act", lambda e: e.copy(out=qT[:, gq * 4:(gq + 1) * 4, :], in_=P[:]), reads=[P], writes=[qT])
            yield
        for gq in range(4):
            P = ps[gq % 2]
            for j in range(4):
                hp = gq * 4 + j
                S.op("pe", lambda e: e.matmul(P[:, j, :], lhsT=qT[:, hp, :], rhs=skT[:, hp % 2, :], start=True, stop=True),
                     reads=[qT, skT], writes=[P])
            S.op("act", lambda e: e.copy(out=s_sb[:, gq * 4:(gq + 1) * 4, :], in_=P[:]), reads=[P], writes=[s_sb])
            yield
        for hp in range(16):
            S.op("dve", lambda e: e.max(out=top[:, hp, 0:8], in_=s_sb[:, hp, :]), reads=[s_sb], writes=[top])
            S.op("dve", lambda e: e.max_index(out=tix[:, hp, 0:8], in_max=top[:, hp, 0:8], in_values=s_sb[:, hp, :]),
                 reads=[s_sb, top], writes=[tix])
            S.op("dve", lambda e: e.match_replace(out=s2[:, hp, :], in_to_replace=top[:, hp, 0:8], in_values=s_sb[:, hp, :],
                                                  imm_value=-1e30), reads=[s_sb, top], writes=[s2])
            S.op("dve", lambda e: e.max(out=top[:, hp, 8:16], in_=s2[:, hp, :]), reads=[s2], writes=[top])
            S.op("dve", lambda e: e.max_index(out=tix[:, hp, 8:16], in_max=top[:, hp, 8:16], in_values=s2[:, hp, :]),
                 reads=[s2, top], writes=[tix])
            yield
        S.op("dve", lambda e: e.tensor_copy(out=tixf[:], in_=tix[:]), reads=[tix], writes=[tixf])
        top4 = top[:].rearrange("q (h p) a -> q h p a", p=2)
        tix4 = tixf[:].rearrange("q (h p) a -> q h p a", p=2)
        c4 = cand[:].rearrange("q h (a b) -> q h a b", b=16)
        S.op("dve", lambda e: e.tensor_tensor(out=c4, in0=top4[:, :, 0, :].unsqueeze(3).to_broadcast([128, 8, 16, 16]),
                                              in1=top4[:, :, 1, :].unsqueeze(2).to_broadcast([128, 8, 16, 16]), op=ALU.add),
             reads=[top], writes=[cand])
        for h in range(8):
            S.op("dve", lambda e: e.max(out=g[:, h, 0:8], in_=cand[:, h, :]), reads=[cand], writes=[g])
            S.op("dve", lambda e: e.match_replace(out=cand2[:, h, :], in_to_replace=g[:, h, 0:8], in_values=cand[:, h, :],
                                                  imm_value=-1e30), reads=[cand, g], writes=[cand2])
            S.op("dve", lambda e: e.max(out=g[:, h, 8:16], in_=cand2[:, h, :]), reads=[cand2], writes=[g])
            yield
        for h in range(8):
            S.op("dve", lambda e: e.max_index(out=pos[:, h, 0:8], in_max=g[:, h, 0:8], in_values=cand[:, h, :]),
                 reads=[cand, g], writes=[pos])
            S.op("dve", lambda e: e.max_index(out=pos[:, h, 8:16], in_max=g[:, h, 8:16], in_values=cand2[:, h, :]),
                 reads=[cand2, g], writes=[pos])
            yield
        S.op("dve", lambda e: e.tensor_copy(out=posf[:], in_=pos[:]), reads=[pos], writes=[posf])
        S.op("dve", lambda e: e.tensor_scalar(out=ai[:], in0=posf[:], scalar1=0.0625, scalar2=-0.46875, op0=ALU.mult, op1=ALU.add),
             reads=[posf], writes=[ai])
        S.op("dve", lambda e: e.tensor_copy(out=af[:], in_=ai[:]), reads=[ai], writes=[af])
        S.op("dve", lambda e: e.scalar_tensor_tensor(out=bf_[:], in0=af[:], scalar=-16.0, in1=posf[:], op0=ALU.mult, op1=ALU.add),
             reads=[af, posf], writes=[bf_])
        yield
        oh4 = oh[:].rearrange("q (h k a) -> q h k a", k=16, a=16)
        io4 = iota16[:].unsqueeze(1).unsqueeze(1).to_broadcast([128, 8, 16, 16])
        for (src, pidx, dst) in ((af, 0, ia), (bf_, 1, ja)):
            S.op("dve", lambda e: e.tensor_tensor(out=oh4, in0=src[:].unsqueeze(3).to_broadcast([128, 8, 16, 16]), in1=io4, op=ALU.is_equal),
                 reads=[src, iota16], writes=[oh])
            S.op("dve", lambda e: e.tensor_tensor(out=oh4, in0=oh4, in1=tix4[:, :, pidx, :].unsqueeze(2).to_broadcast([128, 8, 16, 16]), op=ALU.mult),
                 reads=[oh, tixf], writes=[oh])
            S.op("dve", lambda e: e.reduce_sum(out=dst[:], in_=oh4, axis=AX.X), reads=[oh], writes=[dst])
            yield
        S.op("dve", lambda e: e.scalar_tensor_tensor(out=ef[:].rearrange("q (h k) -> q h k", k=16), in0=ia[:], scalar=128.0, in1=ja[:],
                                                     op0=ALU.mult, op1=ALU.add), reads=[ia, ja], writes=[ef])
        EI, GS = eidx[i2], gs[i2]
        S.op("dve", lambda e: e.tensor_scalar(out=ef[:], in0=ef[:], scalar1=16383.0, scalar2=0.0, op0=ALU.min, op1=ALU.max),
             reads=[ef], writes=[ef])
        if idx_base:
            S.op("dve", lambda e: e.tensor_scalar_add(out=ef[:], in0=ef[:], scalar1=float(idx_base)), reads=[ef], writes=[ef])
        S.op("dve", lambda e: e.tensor_copy(out=EI[:], in_=ef[:]), reads=[ef], writes=[EI])
        S.op("dve", lambda e: e.tensor_tensor(out=ge[:], in0=g[:], in1=g[:, :, 0:1].to_broadcast([128, 8, 16]), op=ALU.subtract),
             reads=[g], writes=[ge])
        S.op("act", lambda e: e.activation(out=ge[:], in_=ge[:], func=AF.Exp), reads=[ge], writes=[ge])
        S.op("dve", lambda e: e.reduce_sum(out=gsum[:], in_=ge[:], axis=AX.X), reads=[ge], writes=[gsum])
        S.op("dve", lambda e: e.reciprocal(out=gsum[:], in_=gsum[:]), reads=[gsum], writes=[gsum])
        S.op("dve", lambda e: e.tensor_tensor(out=GS[:].rearrange("q (h k) -> q h k", k=16), in0=ge[:],
                                              in1=gsum[:].unsqueeze(2).to_broadcast([128, 8, 16]), op=ALU.mult),
             reads=[ge, gsum], writes=[GS])
    def stage_UV(t, nxt):
        nonlocal gi, di
        i2 = t % NB
        X, HB, EI, GS = xs[i2], hb[i2], eidx[i2], gs[i2]
        gate = gates[t // tiles_per_batch]
        for grp in range(32):
            AG, WG = actg[grp % 4], wvg[grp % 4]
            UVs = []
            for j in range(4):
                s = grp * 4 + j
                UV = ug[gi % NG]
                gi += 1
                UVs.append(UV)
                S.dma("pool", lambda e: e.indirect_dma_start(out=UV[:], out_offset=None, in_=puv_d,
                                                             in_offset=bass.IndirectOffsetOnAxis(ap=EI[:, s:s + 1], axis=0)),
                      reads=[EI], writes=[UV])
                S.op("dve", lambda e: e.scalar_tensor_tensor(out=junkb[:], in0=UV[:, 0:D], scalar=1.0, in1=HB[:], op0=ALU.mult,
                                                             op1=ALU.mult, accum_out=AG[:, j:j + 1]),
                     reads=[UV, HB], writes=[junkb, AG])
            S.op("act", lambda e: e.activation(out=WG[:], in_=AG[:], func=AF.Gelu), reads=[AG], writes=[WG])
            for j in range(4):
                S.op("act", lambda e: e.mul(out=WG[:, j:j + 1], in_=WG[:, j:j + 1], mul=GS[:, grp * 4 + j:grp * 4 + j + 1]),
                     reads=[WG, GS], writes=[WG])
            for j in range(4):
                s = grp * 4 + j
                DG = dg[di % ND]
                di += 1
                S.op("act", lambda e: e.mul(out=DG[:], in_=identb[:], mul=WG[:, j:j + 1]), reads=[identb, WG], writes=[DG])
                for hh in range(2):
                    S.op("pe", lambda e: e.matmul(pout[hh][:], lhsT=DG[:], rhs=UVs[j][:, D + hh * 512:D + (hh + 1) * 512],
                                                  start=(s == 0), stop=(s == 127)), reads=[DG, UVs[j]], writes=[pout[hh]])
            for _ in range(6):
                next(nxt, None)
        Y = yo[i2]
        for hh in range(2):
            S.op("dve", lambda e: e.tensor_mul(out=Y[:, hh * 512:(hh + 1) * 512], in0=pout[hh][:],
                                               in1=gate[:, hh * 512:(hh + 1) * 512]), reads=[pout[hh], gate], writes=[Y])
        S.op("dve", lambda e: e.tensor_add(out=Y[:], in0=Y[:], in1=X[:]), reads=[Y, X], writes=[Y])
        S.dma("sp", lambda e: e.dma_start(out=x_out[t * 128:(t + 1) * 128, :], in_=Y[:]), reads=[Y], writes=[])
        for _ in nxt:
            pass

    g0 = stage_I(0)
    for _ in g0:
        pass
    for t in range(ntiles):
        nxt = stage_I(t + 1) if t + 1 < ntiles else iter(())
        stage_UV(t, nxt)


def emit_convert(S, nc, pairs, rows):
    NBUF = 4
    bufs = [S.sbuf(f"cv{i}", [128, 4, D], BF16) for i in range(NBUF)]
    i = 0
    for src, dst in pairs:
        sv = src.rearrange("(c p r) d -> c p r d", p=128, r=4)
        dv = dst.rearrange("(c p r) d -> c p r d", p=128, r=4)
        for c in range(rows // 512):
            B = bufs[i % NBUF]
            S.dma("pool", lambda e: e.dma_start(out=B[:], in_=sv[c]), writes=[B])
            S.dma(("sp", "act")[i % 2], lambda e: e.dma_start(out=dv[c], in_=B[:]), reads=[B], writes=[])
            i += 1


def emit_convert_inplace(S, nc, tabs, rows):
    NBUF = 4
    bufs = [S.sbuf(f"cv{i}", [128, 4, D], BF16) for i in range(NBUF)]
    views = []
    i = 0
    for tab in tabs:
        sv = tab.rearrange("(c p r) d -> c p r d", p=128, r=4)
        v16 = tab.bitcast(BF16).rearrange("n (two d) -> (n two) d", two=2)
        dv = v16[0:rows, :].rearrange("(c p r) d -> c p r d", p=128, r=4)
        views.append(v16)
        for c in range(rows // 512):
            B = bufs[i % NBUF]
            S.dma("pool", lambda e: e.dma_start(out=B[:], in_=sv[c]), writes=[B])
            S.dma(("sp", "act")[i % 2], lambda e: e.dma_start(out=dv[c], in_=B[:]), reads=[B], writes=[])
            i += 1
    return views


def emit_convert_uv(S, nc, tab_u, tab_v, rows):
    NBUF = 3
    bufs = [S.sbuf(f"cuv{i}", [128, 4, 2 * D], BF16) for i in range(NBUF)]
    su = tab_u.rearrange("(c p r) d -> c p r d", p=128, r=4)
    sv = tab_v.rearrange("(c p r) d -> c p r d", p=128, r=4)
    uv = tab_u.bitcast(BF16)
    dv = uv.rearrange("(c p r) d -> c p r d", p=128, r=4)
    for c in range(rows // 512):
        B = bufs[c % NBUF]
        S.dma("pool", lambda e: e.dma_start(out=B[:, :, 0:D], in_=su[c]), writes=[B])
        S.dma("pool", lambda e: e.dma_start(out=B[:, :, D:2 * D], in_=sv[c]), writes=[B])
        S.dma(("sp", "act")[c % 2], lambda e: e.dma_start(out=dv[c], in_=B[:]), reads=[B], writes=[])
    return uv


def gen_convert_uv(S, nc, tab_u, tab_v, rows):
    NBUF = 3
    bufs = [S.sbuf(f"cuv{i}", [128, 4, 2 * D], BF16) for i in range(NBUF)]
    su = tab_u.rearrange("(c p r) d -> c p r d", p=128, r=4)
    sv = tab_v.rearrange("(c p r) d -> c p r d", p=128, r=4)
    uv = tab_u.bitcast(BF16)
    dv = uv.rearrange("(c p r) d -> c p r d", p=128, r=4)
    for c in range(rows // 512):
        B = bufs[c % NBUF]
        S.dma("pool", lambda e: e.dma_start(out=B[:, :, 0:D], in_=su[c]), writes=[B])
        S.dma("pool", lambda e: e.dma_start(out=B[:, :, D:2 * D], in_=sv[c]), writes=[B])
        S.dma("sp", lambda e: e.dma_start(out=dv[c], in_=B[:]), reads=[B], writes=[])
        yield


import math


def emit_ada(S, nc, cT_d, ada_w_d, ada_b_d, kvw_d, kvb_d, mod_d):
    cT = S.sbuf("cT", [128, 8, 2], F32)
    S.dma("sp", lambda e: e.dma_start(out=cT[:], in_=cT_d), writes=[cT])
    cs = S.sbuf("cs", [128, 8, 2], F32)
    S.op("act", lambda e: e.activation(out=cs[:], in_=cT[:], func=AF.Silu), reads=[cT], writes=[cs])
    wch = [S.sbuf(f"wch{i}", [128, 8, 512], F32) for i in range(2)]
    bch = [S.sbuf(f"bch{i}", [2, 512], F32) for i in range(2)]
    och = [S.sbuf(f"och{i}", [2, 512], F32) for i in range(2)]
    pa = [S.psum(f"pada{i}", [128, 512], F32) for i in range(2)]
    jobs = []
    for l in range(2):
        for ch in range(12):
            jobs.append((ada_w_d[l], ada_b_d[l:l + 1, :], ch, 6 * l + ch // 2, ch % 2))
    for ch in range(4):
        jobs.append((kvw_d, kvb_d, ch, 12 + ch // 2, ch % 2))
    for i, (w_d, b_d, ch, row, half) in enumerate(jobs):
        W, B, O, P = wch[i % 2], bch[i % 2], och[i % 2], pa[i % 2]
        wv = w_d.rearrange("(k p) n -> p k n", p=128)
        q = ("sp", "act")[i % 2]
        S.dma(q, lambda e: e.dma_start(out=W[:], in_=wv[:, :, ch * 512:(ch + 1) * 512]), writes=[W])
        S.dma(q, lambda e: e.dma_start(out=B[:], in_=b_d[:, ch * 512:(ch + 1) * 512].partition_broadcast(2)), writes=[B])
        for kc in range(8):
            S.op("pe", lambda e: e.matmul(P[0:2, :], lhsT=cs[:, kc, :], rhs=W[:, kc, :], start=(kc == 0), stop=(kc == 7)),
                 reads=[cs, W], writes=[P])
        S.op("dve", lambda e: e.tensor_add(out=O[:], in0=P[0:2, :], in1=B[:]), reads=[P, B], writes=[O])
        S.dma(q, lambda e: e.dma_start(out=mod_d[:, row, half * 512:(half + 1) * 512], in_=O[:]), reads=[O], writes=[])


def emit_mlstm(S, nc, x_in, x_out, win_d, cwT_d, bif_d, hg_d, wout_d, gmix_row, mod_d, ntiles, tiles_per_seq,
               identb, utri, ones, side=None):
    win = S.sbuf("win", [128, 8, 3088], BF16)
    win_v = win_d.rearrange("(k p) n -> p k n", p=128)
    for kc in range(8):
        S.dma("pool", lambda e: e.dma_start(out=win[:, kc, 0:2048], in_=win_v[:, kc, 0:2048]), writes=[win])
        S.dma("pool", lambda e: e.dma_start(out=win[:, kc, 2048:3088], in_=win_v[:, kc, 2048:3088]), writes=[win])
    wout = S.sbuf("wout", [128, 8, 1024], BF16)
    wout_v = wout_d.rearrange("(k p) n -> p k n", p=128)
    for kc in range(8):
        S.dma("pool", lambda e: e.dma_start(out=wout[:, kc, :], in_=wout_v[:, kc, :]), writes=[wout])
    cwT = S.sbuf("cwT", [128, 8, 4], F32)
    S.dma("sp", lambda e: e.dma_start(out=cwT[:], in_=cwT_d), writes=[cwT])
    bif = S.sbuf("bif", [128, 16], F32)
    load_bcast(S, "sp", bif, bif_d)
    hg = S.sbuf("hg", [128, D], F32)
    load_bcast(S, "sp", hg, hg_d)
    grow = S.sbuf("grow", [128, D], F32)
    load_bcast(S, "sp", grow, gmix_row)
    gmod = S.sbuf("gmod", [128, D], F32)
    shift = S.sbuf("shift", [128, D], F32)
    gate = S.sbuf("gate", [128, D], F32)

    NB = 2
    xs = [S.sbuf(f"xs{i}", [128, D], F32) for i in range(NB)]
    hb = S.sbuf("hb", [128, D], BF16)
    hT = S.sbuf("hT", [128, 8, 128], BF16)
    junk = S.sbuf("junk", [128, D], F32)
    st = S.sbuf("st", [128, 4], F32)
    cb = S.sbuf("cb", [128, 8, 131], F32)
    cacc = S.sbuf("cacc", [128, 8, 128], F32)
    ctmp = S.sbuf("ctmp", [128, 8, 128], F32)
    qkT = S.sbuf("qkT", [128, 8, 128], BF16)
    ktok = S.sbuf("ktok", [128, 4, 128], BF16)
    qm = S.sbuf("qm", [128, 8, 128], BF16)
    S.op("pool", lambda e: e.memset(qm[:], 0.0), writes=[qm])
    gts = S.sbuf("gts", [128, 16], F32)
    spf = S.sbuf("spf", [128, 8], F32)
    wexp = S.sbuf("wexp", [128, 8], F32)
    eb = S.sbuf("eb", [128, 8], F32)
    ebL = S.sbuf("ebL", [128, 8], F32)
    vaug = S.sbuf("vaug", [128, 8, 129], BF16)
    sig = S.sbuf("sig", [128, D], F32)
    ATs = S.sbuf("ATs", [128, 8, 128], BF16)
    Cst = S.sbuf("Cst", [128, 8, 129], F32)
    Cbf = S.sbuf("Cbf", [128, 8, 129], BF16)
    den = S.sbuf("den", [128, 8], F32)
    hh = S.sbuf("hh", [128, 8, 128], F32)
    sq = S.sbuf("sq", [128, 8, 128], F32)
    hss = S.sbuf("hss", [128, 8], F32)
    hob = S.sbuf("hob", [128, D], BF16)
    hoT = S.sbuf("hoT", [128, 8, 128], BF16)
    yo = [S.sbuf(f"yo{i}", [128, D], F32) for i in range(NB)]

    pT = S.psum("pT", [128, 8, 128], BF16)
    pg = [S.psum(f"pg{i}", [128, 512], F32) for i in range(2)]
    pAT = [S.psum(f"pAT{i}", [128, 4, 128], F32) for i in range(2)]
    pOS = [S.psum(f"pOS{i}", [128, 3, 160], F32) for i in range(3)]
    pgi = 0

    def PO(h):
        return pOS[h // 3], h % 3

    LN8 = math.log(0.125)
    for t in range(ntiles):
        sidx = t // tiles_per_seq
        first = (t % tiles_per_seq == 0)
        X = xs[t % NB]
        if first:
            load_bcast(S, "sp", shift, mod_d[sidx, 0:1, :])
            load_bcast(S, "sp", gmod, mod_d[sidx, 1:2, :])
            load_bcast(S, "sp", gate, mod_d[sidx, 2:3, :])
            S.op("dve", lambda e: e.scalar_tensor_tensor(out=gmod[:], in0=gmod[:], scalar=1.0, in1=grow[:],
                                                         op0=ALU.add, op1=ALU.mult), reads=[gmod, grow], writes=[gmod])
            S.op("pool", lambda e: e.memset(cb[:, :, 0:3], 0.0), writes=[cb])
            S.op("pool", lambda e: e.memset(Cst[:], 0.0), writes=[Cst])
            S.op("pool", lambda e: e.memset(Cbf[:], 0.0), writes=[Cbf])
        S.dma("sp", lambda e: e.dma_start(out=X[:], in_=x_in[t * 128:(t + 1) * 128, :]), writes=[X])
        if side is not None:
            next(side, None)
            next(side, None)
        emit_norm_mod(S, X, gmod, shift, hb, st, junk)
        emit_transpose8(S, hb, pT, hT, identb)
        for half in range(2):
            P = pg[pgi % 2]
            pgi += 1
            for j in range(4):
                fc = half * 4 + j
                for kc in range(8):
                    S.op("pe", lambda e: e.matmul(P[:, j * 128:(j + 1) * 128], lhsT=win[:, kc, fc * 128:(fc + 1) * 128],
                                                  rhs=hT[:, kc, :], start=(kc == 0), stop=(kc == 7)),
                         reads=[win, hT], writes=[P])
            S.op("act", lambda e: e.copy(out=cb[:, half * 4:(half + 1) * 4, 3:131],
                                         in_=P[:].rearrange("p (j n) -> p j n", n=128)), reads=[P], writes=[cb])
        def cw(w):
            return cwT[:, :, w:w + 1].to_broadcast([128, 8, 128])
        S.op("dve", lambda e: e.tensor_tensor(out=cacc[:], in0=cb[:, :, 3:131], in1=cw(3), op=ALU.mult), reads=[cb, cwT], writes=[cacc])
        for w in range(3):
            S.op("pool", lambda e: e.tensor_tensor(out=ctmp[:], in0=cb[:, :, w:w + 128], in1=cw(w), op=ALU.mult),
                 reads=[cb, cwT], writes=[ctmp])
            S.op("dve", lambda e: e.tensor_add(out=cacc[:], in0=cacc[:], in1=ctmp[:]), reads=[cacc, ctmp], writes=[cacc])
        S.op("act", lambda e: e.activation(out=qkT[:, 4:8, :], in_=cacc[:, 4:8, :], func=AF.Silu), reads=[cacc], writes=[qkT])
        qm4 = qm[:].rearrange("p (f two) n -> p f two n", two=2)
        S.op("act", lambda e: e.activation(out=qm4[0:64, :, 0, :], in_=cacc[0:64, 0:4, :], func=AF.Silu), reads=[cacc], writes=[qm])
        S.op("act", lambda e: e.activation(out=qm4[64:128, :, 1, :], in_=cacc[64:128, 0:4, :], func=AF.Silu), reads=[cacc], writes=[qm])
        S.op("pool", lambda e: e.tensor_copy(out=cb[:, :, 0:3], in_=cb[:, :, 128:131]), reads=[cb], writes=[cb])
        for j in range(4):
            S.op("pe", lambda e: e.transpose(out=pT[:, j, :], in_=qkT[:, 4 + j, :], identity=identb[:]),
                 reads=[qkT, identb], writes=[pT])
        S.op("act", lambda e: e.copy(out=ktok[:], in_=pT[:, 0:4, :]), reads=[pT], writes=[ktok])
        P = pg[pgi % 2]
        pgi += 1
        for kc in range(8):
            S.op("pe", lambda e: e.matmul(P[:, 0:16], lhsT=hT[:, kc, :], rhs=win[:, kc, 3072:3088], start=(kc == 0), stop=(kc == 7)),
                 reads=[hT, win], writes=[P])
        S.op("dve", lambda e: e.tensor_add(out=gts[:], in0=P[:, 0:16], in1=bif[:]), reads=[P, bif], writes=[gts])
        S.op("act", lambda e: e.activation(out=spf[:], in_=gts[:, 8:16], func=AF.Exp, scale=-1.0), reads=[gts], writes=[spf])
        S.op("act", lambda e: e.activation(out=spf[:], in_=spf[:], func=AF.Ln, bias=1.0), reads=[spf], writes=[spf])
        S.op("pe", lambda e: e.matmul(P[:, 16:24], lhsT=utri[:], rhs=spf[:], start=True, stop=True), reads=[utri, spf], writes=[P])
        S.op("pe", lambda e: e.matmul(P[:, 32:40], lhsT=ones[:], rhs=spf[:], start=True, stop=True), reads=[ones, spf], writes=[P])
        S.op("dve", lambda e: e.tensor_add(out=wexp[:], in0=P[:, 16:24], in1=gts[:, 0:8]), reads=[P, gts], writes=[wexp])
        S.op("act", lambda e: e.activation(out=wexp[:], in_=wexp[:], func=AF.Exp), reads=[wexp], writes=[wexp])
        S.op("act", lambda e: e.activation(out=eb[:], in_=P[:, 16:24], func=AF.Exp, scale=-1.0, bias=LN8), reads=[P], writes=[eb])
        S.op("act", lambda e: e.activation(out=ebL[:], in_=P[:, 32:40], func=AF.Exp, scale=-1.0), reads=[P], writes=[ebL])
        for half in range(2):
            P = pg[pgi % 2]
            pgi += 1
            for kc in range(8):
                S.op("pe", lambda e: e.matmul(P[:], lhsT=hT[:, kc, :], rhs=win[:, kc, 1024 + half * 512:1024 + (half + 1) * 512],
                                              start=(kc == 0), stop=(kc == 7)), reads=[hT, win], writes=[P])
            S.op("dve", lambda e: e.tensor_tensor(out=vaug[:, half * 4:(half + 1) * 4, 0:128],
                                                  in0=P[:].rearrange("p (h n) -> p h n", n=128),
                                                  in1=wexp[:, half * 4:(half + 1) * 4].unsqueeze(2).to_broadcast([128, 4, 128]),
                                                  op=ALU.mult), reads=[P, wexp], writes=[vaug])
        S.op("pool", lambda e: e.tensor_copy(out=vaug[:, :, 128:129], in_=wexp[:].unsqueeze(2)), reads=[wexp], writes=[vaug])
        for half in range(2):
            P = pg[pgi % 2]
            pgi += 1
            for kc in range(8):
                S.op("pe", lambda e: e.matmul(P[:], lhsT=hT[:, kc, :], rhs=win[:, kc, 2048 + half * 512:2048 + (half + 1) * 512],
                                              start=(kc == 0), stop=(kc == 7)), reads=[hT, win], writes=[P])
            S.op("act", lambda e: e.activation(out=sig[:, half * 512:(half + 1) * 512], in_=P[:], func=AF.Sigmoid),
                 reads=[P], writes=[sig])
        for h in range(8):
            po, fc = (h % 2) * 64, h // 2
            S.op("pe", lambda e: e.matmul(pAT[h // 4][:, h % 4, :], lhsT=qkT[:, 4 + fc, :], rhs=qm[:, h, :],
                                          start=True, stop=True), reads=[qkT, qm], writes=[pAT[h // 4]])
        for half in range(2):
            S.op(("dve", "pool")[0], lambda e: e.tensor_tensor(out=ATs[:, half * 4:(half + 1) * 4, :], in0=pAT[half][:],
                                                  in1=utri[:].unsqueeze(1).to_broadcast([128, 4, 128]), op=ALU.mult),
                 reads=[pAT[half], utri], writes=[ATs])
        for h in range(8):
            po, fc = (h % 2) * 64, h // 2
            PB, sl = PO(h)
            S.op("pe", lambda e: e.matmul(PB[:, sl, 0:129], lhsT=ATs[:, h, :], rhs=vaug[:, h, :], start=True, stop=False),
                 reads=[ATs, vaug], writes=[PB])
            S.op("pe", lambda e: e.matmul(PB[:, sl, 0:129], lhsT=qm[:, h, :], rhs=Cbf[:, h, :],
                                          start=False, stop=True), reads=[qm, Cbf], writes=[PB])
        for bk in range(3):
            nh = 3 if bk < 2 else 2
            S.op("dve", lambda e: e.tensor_tensor(out=den[:, bk * 3:bk * 3 + nh].unsqueeze(2), in0=pOS[bk][:, 0:nh, 128:129],
                                                  in1=eb[:, bk * 3:bk * 3 + nh].unsqueeze(2), op=ALU.mult),
                 reads=[pOS[bk], eb], writes=[den])
        S.op("act", lambda e: e.activation(out=den[:], in_=den[:], func=AF.Abs), reads=[den], writes=[den])
        S.op("dve", lambda e: e.tensor_scalar_max(out=den[:], in0=den[:], scalar1=1.0), reads=[den], writes=[den])
        S.op("dve", lambda e: e.reciprocal(out=den[:], in_=den[:]), reads=[den], writes=[den])
        S.op("dve", lambda e: e.tensor_mul(out=den[:], in0=den[:], in1=eb[:]), reads=[den, eb], writes=[den])
        for bk in range(3):
            nh = 3 if bk < 2 else 2
            S.op("dve", lambda e: e.tensor_tensor(out=hh[:, bk * 3:bk * 3 + nh, :], in0=pOS[bk][:, 0:nh, 0:128],
                                                  in1=den[:, bk * 3:bk * 3 + nh].unsqueeze(2).to_broadcast([128, nh, 128]),
                                                  op=ALU.mult), reads=[pOS[bk], den], writes=[hh])
        for h in range(8):
            PB, sl = PO(h)
            fc = h // 2
            S.op("pe", lambda e: e.matmul(PB[:, sl, 0:129], lhsT=ktok[:, fc, :], rhs=vaug[:, h, :], start=True, stop=True),
                 reads=[ktok, vaug], writes=[PB])
        for bk in range(3):
            nh = 3 if bk < 2 else 2
            S.op("dve", lambda e: e.tensor_tensor(out=Cst[:, bk * 3:bk * 3 + nh, :], in0=pOS[bk][:, 0:nh, 0:129],
                                                  in1=Cst[:, bk * 3:bk * 3 + nh, :], op=ALU.add), reads=[pOS[bk], Cst], writes=[Cst])
        S.op("pool", lambda e: e.tensor_tensor(out=Cst[:], in0=Cst[:], in1=ebL[:].unsqueeze(2).to_broadcast([128, 8, 129]),
                                               op=ALU.mult), reads=[Cst, ebL], writes=[Cst])
        S.op("act", lambda e: e.copy(out=Cbf[:], in_=Cst[:]), reads=[Cst], writes=[Cbf])
        S.op("pool", lambda e: e.tensor_tensor(out=sq[:], in0=hh[:], in1=hh[:], op=ALU.mult), reads=[hh], writes=[sq])
        S.op("dve", lambda e: e.reduce_sum(out=hss[:], in_=sq[:], axis=AX.X), reads=[sq], writes=[hss])
        S.op("act", lambda e: e.activation(out=hss[:], in_=hss[:], func=AF.Sqrt, scale=1.0 / 128, bias=EPS), reads=[hss], writes=[hss])
        S.op("dve", lambda e: e.reciprocal(out=hss[:], in_=hss[:]), reads=[hss], writes=[hss])
        S.op("dve", lambda e: e.tensor_tensor(out=hh[:], in0=hh[:], in1=hss[:].unsqueeze(2).to_broadcast([128, 8, 128]), op=ALU.mult),
             reads=[hh, hss], writes=[hh])
        hh2 = hh[:].rearrange("p h n -> p (h n)")
        S.op("pool", lambda e: e.tensor_tensor(out=hh2, in0=hh2, in1=hg[:], op=ALU.mult), reads=[hh, hg], writes=[hh])
        S.op("dve", lambda e: e.tensor_tensor(out=hob[:], in0=hh2, in1=sig[:], op=ALU.mult), reads=[hh, sig], writes=[hob])
        emit_transpose8(S, hob, pT, hoT, identb)
        Y = yo[t % NB]
        for half in range(2):
            P = pg[pgi % 2]
            pgi += 1
            for kc in range(8):
                S.op("pe", lambda e: e.matmul(P[:], lhsT=hoT[:, kc, :], rhs=wout[:, kc, half * 512:(half + 1) * 512],
                                              start=(kc == 0), stop=(kc == 7)), reads=[hoT, wout], writes=[P])
            S.op("dve", lambda e: e.tensor_mul(out=Y[:, half * 512:(half + 1) * 512], in0=P[:], in1=gate[:, half * 512:(half + 1) * 512]),
                 reads=[P, gate], writes=[Y])
        S.op("pool", lambda e: e.tensor_add(out=Y[:], in0=Y[:], in1=X[:]), reads=[Y, X], writes=[Y])
        S.dma("sp", lambda e: e.dma_start(out=x_out[t * 128:(t + 1) * 128, :], in_=Y[:]), reads=[Y], writes=[])


def drain(gen):
    if gen is not None:
        for _ in gen:
            pass


def emit_headnorm(S, src, gsm, dst, sq, hss, nh, hd):
    s3 = src[:].rearrange("p (h d) -> p h d", d=hd)
    q3 = sq[:].rearrange("p (h d) -> p h d", d=hd)
    d3 = dst[:].rearrange("p (h d) -> p h d", d=hd)
    S.op("pool", lambda e: e.tensor_tensor(out=sq[:], in0=src[:], in1=src[:], op=ALU.mult), reads=[src], writes=[sq])
    S.op("dve", lambda e: e.reduce_sum(out=hss[:], in_=q3, axis=AX.X), reads=[sq], writes=[hss])
    S.op("act", lambda e: e.activation(out=hss[:], in_=hss[:], func=AF.Sqrt, scale=1.0 / hd, bias=EPS), reads=[hss], writes=[hss])
    S.op("dve", lambda e: e.reciprocal(out=hss[:], in_=hss[:]), reads=[hss], writes=[hss])
    S.op("dve", lambda e: e.tensor_tensor(out=q3, in0=s3, in1=hss[:].unsqueeze(2).to_broadcast([128, nh, hd]), op=ALU.mult),
         reads=[src, hss], writes=[sq])
    S.op("pool", lambda e: e.tensor_tensor(out=d3, in0=q3, in1=gsm[:].unsqueeze(1).to_broadcast([128, nh, hd]), op=ALU.mult),
         reads=[sq, gsm], writes=[dst])


def emit_sb(S, nc, x_in, x_out, kvg_row, kvw_d, kng_row, gmix_row, wq_d, qng_row, wout_d, mod_d, nseq, TPS,
            identb, sutri, sltri, ones):
    NH, HD = 16, 64
    kT_all = S.sbuf("kT_all", [128, 8, TPS * 128], BF16)
    v_all = S.sbuf("v_all", [128, TPS, D], BF16)
    xs = [S.sbuf(f"xs{i}", [128, D], F32) for i in range(2)]
    hb = S.sbuf("hb", [128, D], BF16)
    hT = S.sbuf("hT", [128, 8, 128], BF16)
    junk = S.sbuf("junk", [128, D], F32)
    st = S.sbuf("st", [128, 4], F32)
    grow = S.sbuf("grow", [128, D], F32)
    gmod = S.sbuf("gmod", [128, D], F32)
    shift = S.sbuf("shift", [128, D], F32)
    gsm = S.sbuf("gsm", [128, HD], F32)
    pf = S.sbuf("pf", [128, D], F32)
    sq = S.sbuf("sq", [128, D], F32)
    hss = S.sbuf("hss", [128, NH], F32)
    pb = S.sbuf("pb", [128, D], BF16)
    pT = S.psum("pT", [128, 8, 128], BF16)
    pg = S.psum("pg", [128, 512], F32)

    for sidx in range(nseq):
        t0 = sidx * TPS
        with S.scope():
            kvw = S.sbuf("kvw", [128, 8, 2048], BF16)
            kvw_v = kvw_d.rearrange("(k p) n -> p k n", p=128)
            for kc in range(8):
                S.dma("pool", lambda e: e.dma_start(out=kvw[:, kc, :], in_=kvw_v[:, kc, :]), writes=[kvw])
            load_bcast(S, "sp", grow, kvg_row)
            load_bcast(S, "sp", shift, mod_d[sidx, 12:13, :])
            load_bcast(S, "sp", gmod, mod_d[sidx, 13:14, :])
            load_bcast(S, "sp", gsm, kng_row)
            S.op("dve", lambda e: e.scalar_tensor_tensor(out=gmod[:], in0=gmod[:], scalar=1.0, in1=grow[:],
                                                         op0=ALU.add, op1=ALU.mult), reads=[gmod, grow], writes=[gmod])
            for t in range(TPS):
                X = xs[t % 2]
                S.dma("sp", lambda e: e.dma_start(out=X[:], in_=x_in[(t0 + t) * 128:(t0 + t + 1) * 128, :]), writes=[X])
                emit_norm_mod(S, X, gmod, shift, hb, st, junk)
                emit_transpose8(S, hb, pT, hT, identb)
                for ch in range(4):
                    for kc in range(8):
                        S.op("pe", lambda e: e.matmul(pg[:], lhsT=hT[:, kc, :], rhs=kvw[:, kc, ch * 512:(ch + 1) * 512],
                                                      start=(kc == 0), stop=(kc == 7)), reads=[hT, kvw], writes=[pg])
                    if ch < 2:
                        S.op("act", lambda e: e.copy(out=pf[:, ch * 512:(ch + 1) * 512], in_=pg[:]), reads=[pg], writes=[pf])
                    else:
                        S.op("act", lambda e: e.copy(out=v_all[:, t, (ch - 2) * 512:(ch - 1) * 512], in_=pg[:]), reads=[pg], writes=[v_all])
                emit_headnorm(S, pf, gsm, pb, sq, hss, NH, HD)
                for kc in range(8):
                    S.op("pe", lambda e: e.transpose(out=pT[:, kc, :], in_=pb[:, kc * 128:(kc + 1) * 128], identity=identb[:]),
                         reads=[pb, identb], writes=[pT])
                S.op("act", lambda e: e.copy(out=kT_all[:, :, t * 128:(t + 1) * 128], in_=pT[:]), reads=[pT], writes=[kT_all])
        with S.scope():
            wq = S.sbuf("wq", [128, 8, D], BF16)
            wout = S.sbuf("wout", [128, 8, D], BF16)
            wq_v = wq_d.rearrange("(k p) n -> p k n", p=128)
            wout_v = wout_d.rearrange("(k p) n -> p k n", p=128)
            for kc in range(8):
                S.dma("pool", lambda e: e.dma_start(out=wq[:, kc, :], in_=wq_v[:, kc, :]), writes=[wq])
                S.dma("pool", lambda e: e.dma_start(out=wout[:, kc, :], in_=wout_v[:, kc, :]), writes=[wout])
            gate = S.sbuf("gate", [128, D], F32)
            load_bcast(S, "sp", grow, gmix_row)
            load_bcast(S, "sp", shift, mod_d[sidx, 6:7, :])
            load_bcast(S, "sp", gmod, mod_d[sidx, 7:8, :])
            load_bcast(S, "sp", gate, mod_d[sidx, 8:9, :])
            load_bcast(S, "sp", gsm, qng_row)
            S.op("dve", lambda e: e.scalar_tensor_tensor(out=gmod[:], in0=gmod[:], scalar=1.0, in1=grow[:],
                                                         op0=ALU.add, op1=ALU.mult), reads=[gmod, grow], writes=[gmod])
            S.op("dve", lambda e: e.tensor_scalar_mul(out=gsm[:], in0=gsm[:], scalar1=HD ** -0.5), reads=[gsm], writes=[gsm])
            qm = S.sbuf("qm", [128, NH, 128], BF16)
            S.op("pool", lambda e: e.memset(qm[:], 0.0), writes=[qm])
            qm4 = qm[:].rearrange("p (f two) n -> p f two n", two=2)
            E = [S.sbuf(f"E{i}", [128, 512], F32) for i in range(4)]
            SP = [S.sbuf(f"SP{i}", [128, 512], F32) for i in range(4)]
            ARG = [S.sbuf(f"ARG{i}", [128, 512], F32) for i in range(2)]
            XB = [S.sbuf(f"XB{i}", [128, 512], F32) for i in range(2)]
            A = [S.sbuf(f"A{i}", [128, 512], BF16) for i in range(3)]
            SPcum = [S.sbuf(f"SPcum{i}", [128, 512], F32) for i in range(4)]
            yo = [S.sbuf(f"yo{i}", [128, D], F32) for i in range(2)]
            oT = S.sbuf("oT", [128, 8, 128], BF16)
            pz = [S.psum(f"pz{i}", [128, 4, 128], F32) for i in range(2)]
            pnb = [S.psum(f"pnb{i}", [128, 512], F32) for i in range(2)]
            po = [S.psum(f"po{i}", [128, 512], F32) for i in range(2)]
            m3 = sutri[:].unsqueeze(1).to_broadcast([128, 4, 128])
            for qt in range(TPS):
                X = xs[qt % 2]
                S.dma("sp", lambda e: e.dma_start(out=X[:], in_=x_in[(t0 + qt) * 128:(t0 + qt + 1) * 128, :]), writes=[X])
                emit_norm_mod(S, X, gmod, shift, hb, st, junk)
                emit_transpose8(S, hb, pT, hT, identb)
                for ch in range(2):
                    for kc in range(8):
                        S.op("pe", lambda e: e.matmul(pg[:], lhsT=hT[:, kc, :], rhs=wq[:, kc, ch * 512:(ch + 1) * 512],
                                                      start=(kc == 0), stop=(kc == 7)), reads=[hT, wq], writes=[pg])
                    S.op("act", lambda e: e.copy(out=pf[:, ch * 512:(ch + 1) * 512], in_=pg[:]), reads=[pg], writes=[pf])
                emit_headnorm(S, pf, gsm, pb, sq, hss, NH, HD)
                for kc in range(8):
                    S.op("pe", lambda e: e.transpose(out=pT[:, kc, :], in_=pb[:, kc * 128:(kc + 1) * 128], identity=identb[:]),
                         reads=[pb, identb], writes=[pT])
                S.op("act", lambda e: e.copy(out=qm4[0:64, :, 0, :], in_=pT[0:64, :, :]), reads=[pT], writes=[qm])
                S.op("act", lambda e: e.copy(out=qm4[64:128, :, 1, :], in_=pT[64:128, :, :]), reads=[pT], writes=[qm])
                items = [(kt, g) for kt in range(qt, -1, -1) for g in range(4)]
                NI = len(items)

                def S1(i):
                    kt, g = items[i]
                    Z = pz[i % 2]
                    for j in range(4):
                        h = 4 * g + j
                        S.op("pe", lambda e: e.matmul(Z[:, j, :], lhsT=kT_all[:, h // 2, kt * 128:(kt + 1) * 128], rhs=qm[:, h, :],
                                                      start=True, stop=True), reads=[kT_all, qm], writes=[Z])

                def S2(i):
                    kt, g = items[i]
                    Z, Eb, SPb = pz[i % 2], E[i % 4], SP[i % 4]
                    Z2 = Z[:].rearrange("p j n -> p (j n)")
                    S.op("act", lambda e: e.activation(out=Eb[:], in_=Z2, func=AF.Exp), reads=[Z], writes=[Eb])
                    S.op("act", lambda e: e.activation(out=SPb[:], in_=Eb[:], func=AF.Ln, bias=1.0), reads=[Eb], writes=[SPb])
                    if kt == qt:
                        S.op("dve", lambda e: e.tensor_tensor(out=SPb[:].rearrange("p (j n) -> p j n", n=128),
                                                              in0=SPb[:].rearrange("p (j n) -> p j n", n=128), in1=m3, op=ALU.mult),
                             reads=[SPb, sutri], writes=[SPb])

                def S3(i):
                    kt, g = items[i]
                    diag = (kt == qt)
                    NBp, SPb = pnb[i % 2], SP[i % 4]
                    S.op("pe", lambda e: e.matmul(NBp[:], lhsT=sltri[:], rhs=SPb[:], start=True, stop=diag), reads=[sltri, SPb], writes=[NBp])
                    if not diag:
                        S.op("pe", lambda e: e.matmul(NBp[:], lhsT=ones[:], rhs=SPcum[g][:], start=False, stop=True),
                             reads=[ones, SPcum[g]], writes=[NBp])
                    if kt > 0:
                        if diag:
                            S.op("pool", lambda e: e.tensor_copy(out=SPcum[g][:], in_=SPb[:]), reads=[SPb], writes=[SPcum[g]])
                        else:
                            S.op("pool", lambda e: e.tensor_add(out=SPcum[g][:], in0=SPcum[g][:], in1=SPb[:]), reads=[SPb, SPcum[g]], writes=[SPcum[g]])

                def S4(i):
                    kt, g = items[i]
                    diag = (kt == qt)
                    NBp, Eb, SPb, ARGb, Xb, Ab = pnb[i % 2], E[i % 4], SP[i % 4], ARG[i % 2], XB[i % 2], A[i % 3]
                    S.op("dve", lambda e: e.tensor_tensor(out=ARGb[:], in0=SPb[:], in1=NBp[:], op=ALU.add), reads=[SPb, NBp], writes=[ARGb])
                    S.op("act", lambda e: e.activation(out=Xb[:], in_=ARGb[:], func=AF.Exp, scale=-1.0), reads=[ARGb], writes=[Xb])
                    if diag:
                        S.op("dve", lambda e: e.tensor_tensor(out=ARGb[:], in0=Xb[:], in1=Eb[:], op=ALU.mult), reads=[Xb, Eb], writes=[ARGb])
                        S.op("pool", lambda e: e.tensor_tensor(out=Ab[:].rearrange("p (j n) -> p j n", n=128),
                                                               in0=ARGb[:].rearrange("p (j n) -> p j n", n=128), in1=m3, op=ALU.mult),
                             reads=[ARGb, sutri], writes=[Ab])
                    else:
                        S.op("dve", lambda e: e.tensor_tensor(out=Ab[:], in0=Xb[:], in1=Eb[:], op=ALU.mult), reads=[Xb, Eb], writes=[Ab])

                def S5(i):
                    kt, g = items[i]
                    Ab = A[i % 3]
                    for j in range(4):
                        h = 4 * g + j
                        PO = po[h // 8]
                        S.op("pe", lambda e: e.matmul(PO[:, (h % 8) * 64:(h % 8 + 1) * 64], lhsT=Ab[:, j * 128:(j + 1) * 128],
                                                      rhs=v_all[:, kt, h * 64:(h + 1) * 64], start=(kt == qt and h % 8 == 0), stop=(kt == 0)),
                             reads=[Ab, v_all], writes=[PO])

                for n in range(NI + 4):
                    if n < NI:
                        S1(n)
                    if 0 <= n - 1 < NI:
                        S2(n - 1)
                    if 0 <= n - 2 < NI:
                        S3(n - 2)
                    if 0 <= n - 3 < NI:
                        S4(n - 3)
                    if 0 <= n - 4 < NI:
                        S5(n - 4)
                for half in range(2):
                    S.op("act", lambda e: e.copy(out=pb[:, half * 512:(half + 1) * 512], in_=po[half][:]), reads=[po[half]], writes=[pb])
                emit_transpose8(S, pb, pT, oT, identb)
                Y = yo[qt % 2]
                for half in range(2):
                    for kc in range(8):
                        S.op("pe", lambda e: e.matmul(pg[:], lhsT=oT[:, kc, :], rhs=wout[:, kc, half * 512:(half + 1) * 512],
                                                      start=(kc == 0), stop=(kc == 7)), reads=[oT, wout], writes=[pg])
                    S.op("dve", lambda e: e.tensor_mul(out=Y[:, half * 512:(half + 1) * 512], in0=pg[:], in1=gate[:, half * 512:(half + 1) * 512]),
                         reads=[pg, gate], writes=[Y])
                S.op("pool", lambda e: e.tensor_add(out=Y[:], in0=Y[:], in1=X[:]), reads=[Y, X], writes=[Y])
                S.dma("sp", lambda e: e.dma_start(out=x_out[(t0 + qt) * 128:(t0 + qt + 1) * 128, :], in_=Y[:]), reads=[Y], writes=[])


NCORES = 8
SEQ = 2048
TPS = SEQ // 128
NSEQ = 2
NT = NSEQ * TPS
_NC_CACHE = {}


def build_program():
    nc = bass.Bass("TRN2", target_bir_lowering=False)

    def din(name, shape):
        return nc.dram_tensor(name, list(shape), F32, kind="ExternalInput").ap()

    x = din("x", [NT * 128, D])
    cT = din("cT", [128, 8, 2])
    ident = din("ident", [128, 128]); utri_d = din("utri", [128, 128]); sutri_d = din("sutri", [128, 128])
    sltri_d = din("sltri", [128, 128]); ones_d = din("ones", [128, 128]); iota_d = din("iota16", [128, 16])
    ada_w = din("ada_w", [2, D, 6 * D]); ada_b = din("ada_b", [2, 6 * D])
    norm_mix_g = din("norm_mix_g", [2, D]); norm_ffn_g = din("norm_ffn_g", [2, D])
    ma_w_in = din("ma_w_in", [D, 3088]); cwT = din("cwT", [128, 8, 4]); ma_b_if = din("ma_b_if", [1, 16])
    ma_hnorm_g = din("ma_hnorm_g", [1, D]); ma_w_out = din("ma_w_out", [D, D])
    kv_ada_w = din("kv_ada_w", [D, 2 * D]); kv_ada_b = din("kv_ada_b", [1, 2 * D]); kv_norm_g = din("kv_norm_g", [1, D])
    kv_w = din("kv_w", [D, 2 * D]); k_norm_g = din("k_norm_g", [1, 64])
    sb_w_q = din("sb_w_q", [D, D]); sb_q_norm_g = din("sb_q_norm_g", [1, 64]); sb_w_out = din("sb_w_out", [D, D])
    peer_w_q = din("peer_w_q", [2, D, 2 * D]); peer_sub_keys = din("peer_sub_keys", [2, 2, 128, 128])
    peer_u = din("peer_u", [2, 16384, D]); peer_v = din("peer_v", [2, 16384, D])
    pu_flat = peer_u.rearrange("l e d -> (l e) d")
    pv_flat = peer_v.rearrange("l e d -> (l e) d")
    out = nc.dram_tensor("out", [NT * 128, D], F32, kind="ExternalOutput").ap()
    mod = nc.dram_tensor("mod_scr", [2, 14, D], F32, kind="Internal").ap()
    xa = nc.dram_tensor("xa_scr", [NT * 128, D], F32, kind="Internal").ap()
    xb = nc.dram_tensor("xb_scr", [NT * 128, D], F32, kind="Internal").ap()
    xc = nc.dram_tensor("xc_scr", [NT * 128, D], F32, kind="Internal").ap()

    with ExitStack() as es:
        S = Sched(nc, es)
        identb = S.sbuf("identb", [128, 128], BF16)
        S.dma("pool", lambda e: e.dma_start(out=identb[:], in_=ident), writes=[identb])
        cs = {}
        for nm, d in (("utri", utri_d), ("sutri", sutri_d), ("sltri", sltri_d), ("ones", ones_d)):
            cs[nm] = S.sbuf(nm, [128, 128], F32)
            S.dma("sp", lambda e: e.dma_start(out=cs[nm][:], in_=d), writes=[cs[nm]])
        iota16 = S.sbuf("iota16", [128, 16], F32)
        S.dma("sp", lambda e: e.dma_start(out=iota16[:], in_=iota_d), writes=[iota16])
        puv = pu_flat.bitcast(BF16)
        with S.scope():
            emit_ada(S, nc, cT, ada_w, ada_b, kv_ada_w, kv_ada_b, mod)
        with S.scope():
            side = gen_convert_uv(S, nc, pu_flat, pv_flat, 32768)
            emit_mlstm(S, nc, x, xa, ma_w_in, cwT, ma_b_if, ma_hnorm_g, ma_w_out, norm_mix_g[0:1, :], mod, NT, TPS,
                       identb, cs["utri"], cs["ones"], side)
            drain(side)
        with S.scope():
            emit_peer(S, nc, xa, xb, peer_w_q[0], peer_sub_keys[0], puv, norm_ffn_g[0:1, :], mod, 3, NT, TPS, identb, iota16, 0)
        with S.scope():
            emit_sb(S, nc, xb, xc, kv_norm_g, kv_w, k_norm_g, norm_mix_g[1:2, :], sb_w_q, sb_q_norm_g, sb_w_out, mod, NSEQ, TPS,
                    identb, cs["sutri"], cs["sltri"], cs["ones"])
        with S.scope():
            emit_peer(S, nc, xc, out, peer_w_q[1], peer_sub_keys[1], puv, norm_ffn_g[1:2, :], mod, 9, NT, TPS, identb, iota16, 16384)
        S.barrier()
    return nc


def kernel(x, c, ada_w, ada_b, norm_mix_g, norm_ffn_g, ma_w_in, ma_conv_w, ma_b_if, ma_hnorm_g, ma_w_out,
           kv_ada_w, kv_ada_b, kv_norm_g, kv_w, k_norm_g, sb_w_q, sb_q_norm_g, sb_w_out,
           peer_w_q, peer_sub_keys, peer_u, peer_v):
    f = lambda a: np.ascontiguousarray(np.asarray(a, dtype=np.float32))
    x = f(x); c = f(c)
    one = np.ones((128, 128), np.float32)
    shared = {
        "ident": np.eye(128, dtype=np.float32), "utri": np.triu(one), "sutri": np.triu(one, 1), "sltri": np.tril(one, -1), "ones": one,
        "iota16": np.tile(np.arange(16, dtype=np.float32), (128, 1)),
        "ada_w": f(ada_w), "ada_b": f(ada_b), "norm_mix_g": f(norm_mix_g), "norm_ffn_g": f(norm_ffn_g),
        "ma_w_in": f(ma_w_in)[0], "cwT": np.ascontiguousarray(f(ma_conv_w)[0].reshape(4, 8, 128).transpose(2, 1, 0)),
        "ma_b_if": f(ma_b_if).reshape(1, 16), "ma_hnorm_g": f(ma_hnorm_g).reshape(1, D), "ma_w_out": f(ma_w_out)[0],
        "kv_ada_w": f(kv_ada_w), "kv_ada_b": f(kv_ada_b).reshape(1, 2 * D), "kv_norm_g": f(kv_norm_g).reshape(1, D),
        "kv_w": f(kv_w), "k_norm_g": f(k_norm_g).reshape(1, 64),
        "sb_w_q": f(sb_w_q)[0], "sb_q_norm_g": f(sb_q_norm_g).reshape(1, 64), "sb_w_out": f(sb_w_out)[0],
        "peer_w_q": f(peer_w_q), "peer_sub_keys": f(peer_sub_keys), "peer_u": f(peer_u), "peer_v": f(peer_v),
    }
    in_maps = []
    for i in range(NCORES):
        m = dict(shared)
        m["x"] = x[NSEQ * i:NSEQ * (i + 1)].reshape(NT * 128, D)
        m["cT"] = np.ascontiguousarray(c[NSEQ * i:NSEQ * (i + 1)].T.reshape(8, 128, NSEQ).transpose(1, 0, 2))
        in_maps.append(m)
    if "nc" not in _NC_CACHE:
        _NC_CACHE["nc"] = build_program()
    res = run_bass_kernel_spmd(_NC_CACHE["nc"], in_maps, core_ids=list(range(NCORES)))
    return np.concatenate([r["out"].reshape(NSEQ, SEQ, D) for r in res.results], axis=0)
```

```python
import numpy as np
from contextlib import ExitStack
import concourse.bass as bass
import concourse.mybir as mybir
from concourse.bass_utils import run_bass_kernel_spmd

F32 = mybir.dt.float32
BF16 = mybir.dt.bfloat16
U32 = mybir.dt.uint32
I32 = mybir.dt.int32
AF = mybir.ActivationFunctionType
ALU = mybir.AluOpType
AX = mybir.AxisListType


class Buf:
    __slots__ = ("name", "w", "r", "t")

    def __init__(self, name, t=None):
        self.name = name
        self.w = None
        self.r = {}
        self.t = t

    def __getitem__(self, idx):
        return self.t[idx]


class Sched:
    NDMA = 12

    def __init__(self, nc, es):
        self.nc = nc
        self.es = es
        self.engs = {"pe": nc.tensor, "act": nc.scalar, "dve": nc.vector, "pool": nc.gpsimd, "sp": nc.sync}
        self.sem = {}
        self.cnt = {}
        for k in ("pe", "act", "dve", "pool"):
            self.sem[k] = es.enter_context(nc.semaphore("s_" + k))
            self.cnt[k] = 0
        self.dq = {}
        for q in ("sp", "act", "pool"):
            sems = [es.enter_context(nc.semaphore(f"d_{q}{i}")) for i in range(self.NDMA)]
            self.dq[q] = {"sems": sems, "n": 0}
            for i in range(self.NDMA):
                self.sem[("dma", q, i)] = sems[i]
                self.cnt[("dma", q, i)] = 0
        self.waited = {e: {} for e in self.engs}
        self.nins = 0

    def sbuf(self, name, shape, dt):
        self.nins += 0
        self._uid = getattr(self, "_uid", 0) + 1
        name = f"sb{self._uid}_{name}"
        t = self.es.enter_context(self.nc.sbuf_tensor(name, list(shape), dt))
        return Buf(name, t)

    def psum(self, name, shape, dt=F32):
        self._uid = getattr(self, "_uid", 0) + 1
        name = f"ps{self._uid}_{name}"
        t = self.es.enter_context(self.nc.psum_tensor(name, list(shape), dt))
        return Buf(name, t)

    def view(self, name):
        return Buf(name)

    def scope(self):
        return _Scope(self)

    def _wait(self, engine, key, c):
        if c <= 0:
            return
        w = self.waited[engine]
        if w.get(key, 0) >= c:
            return
        self.engs[engine].wait_ge(self.sem[key], c)
        w[key] = c

    def _deps(self, engine, reads, writes):
        deps = {}
        for b in reads:
            if b.w is not None:
                k, c = b.w
                deps[k] = max(deps.get(k, 0), c)
        for b in writes:
            if b.w is not None:
                k, c = b.w
                deps[k] = max(deps.get(k, 0), c)
            for k, c in b.r.items():
                deps[k] = max(deps.get(k, 0), c)
        return deps

    def op(self, engine, fn, reads=(), writes=()):
        deps = self._deps(engine, reads, writes)
        for k, c in deps.items():
            if k == engine and engine == "pe":
                continue
            self._wait(engine, k, c)
        ins = fn(self.engs[engine])
        self.cnt[engine] += 1
        ins.then_inc(self.sem[engine], 1)
        me = (engine, self.cnt[engine])
        for b in reads:
            b.r[engine] = self.cnt[engine]
        for b in writes:
            b.w = me
            b.r = {}
        self.nins += 1
        return ins

    def dma(self, q, fn, reads=(), writes=()):
        d = self.dq[q]
        i = d["n"] % self.NDMA
        d["n"] += 1
        key = ("dma", q, i)
        deps = self._deps(q, reads, writes)
        deps[key] = max(deps.get(key, 0), self.cnt[key])
        for k, c in deps.items():
            self._wait(q, k, c)
        ins = fn(self.engs[q])
        self.cnt[key] += 16
        ins.then_inc(self.sem[key], 16)
        me = (key, self.cnt[key])
        for b in reads:
            b.r[key] = self.cnt[key]
        for b in writes:
            b.w = me
            b.r = {}
        self.nins += 1
        return ins

    def finish(self, bufs):
        for b in bufs:
            if b.w is not None:
                k, c = b.w
                for e in ("sp", "act", "pool", "dve", "pe"):
                    self._wait(e, k, c)

    def barrier(self):
        for e in self.engs:
            for k, c in self.cnt.items():
                if c > 0 and k != e:
                    self._wait(e, k, c)


class _Scope:
    def __init__(self, S):
        self.S = S

    def __enter__(self):
        self.old = self.S.es
        self.es2 = ExitStack()
        self.es2.__enter__()
        self.S.es = self.es2
        return self

    def __exit__(self, *a):
        self.S.barrier()
        self.S.es = self.old
        return self.es2.__exit__(*a)


EPS = 1e-6
D = 1024


def load_bcast(S, q, dst, row_ap):
    S.dma(q, lambda e: e.dma_start(out=dst[:], in_=row_ap.partition_broadcast(128)), writes=[dst])


def emit_norm_mod(S, xs, gmod, shift, hb, st, junk):
    S.op("act", lambda e: e.activation(out=junk[:], in_=xs[:], func=AF.Square, accum_out=st[:, 0:1]),
         reads=[xs], writes=[junk, st])
    S.op("act", lambda e: e.activation(out=st[:, 1:2], in_=st[:, 0:1], func=AF.Sqrt, scale=1.0 / D, bias=EPS),
         reads=[st], writes=[st])
    S.op("dve", lambda e: e.reciprocal(out=st[:, 2:3], in_=st[:, 1:2]), reads=[st], writes=[st])
    S.op("dve", lambda e: e.scalar_tensor_tensor(out=junk[:], in0=xs[:], scalar=st[:, 2:3], in1=gmod[:],
                                                 op0=ALU.mult, op1=ALU.mult), reads=[xs, st, gmod], writes=[junk])
    S.op("dve", lambda e: e.tensor_add(out=hb[:], in0=junk[:], in1=shift[:]), reads=[junk, shift], writes=[hb])


def emit_transpose8(S, hb, pT, hT, identb):
    for kc in range(8):
        S.op("pe", lambda e: e.transpose(out=pT[:, kc, :], in_=hb[:, kc * 128:(kc + 1) * 128], identity=identb[:]),
             reads=[hb, identb], writes=[pT])
    S.op("act", lambda e: e.copy(out=hT[:], in_=pT[:]), reads=[pT], writes=[hT])


def emit_peer(S, nc, x_in, x_out, wq_d, sk_d, puv_d, gffn_row, mod_d, mod_base, ntiles, tiles_per_batch,
              identb, iota16, idx_base=0):
    wq = S.sbuf("wq", [128, 8, 2048], BF16)
    wq_v = wq_d.rearrange("(k p) n -> p k n", p=128)
    for kc in range(8):
        S.dma("pool", lambda e: e.dma_start(out=wq[:, kc, :], in_=wq_v[:, kc, :]), writes=[wq])
    skn = S.sbuf("skn", [128, 2, 128], BF16)
    S.dma("pool", lambda e: e.dma_start(out=skn[:], in_=sk_d.rearrange("p n k -> n p k")), writes=[skn])
    skT = S.sbuf("skT", [128, 2, 128], BF16)
    pT = S.psum("pT", [128, 8, 128], BF16)
    for p in range(2):
        S.op("pe", lambda e: e.transpose(out=pT[:, p, :], in_=skn[:, p, :], identity=identb[:]),
             reads=[skn, identb], writes=[pT])
    S.op("act", lambda e: e.copy(out=skT[:], in_=pT[:, 0:2, :]), reads=[pT], writes=[skT])

    grow = S.sbuf("grow", [128, D], F32)
    load_bcast(S, "sp", grow, gffn_row)
    gmod = S.sbuf("gmod", [128, D], F32)
    shift = S.sbuf("shift", [128, D], F32)
    nbatch = (ntiles + tiles_per_batch - 1) // tiles_per_batch
    gates = [S.sbuf(f"gate{i}", [128, D], F32) for i in range(nbatch)]
    for i in range(nbatch):
        load_bcast(S, "sp", gates[i], mod_d[i, mod_base + 2:mod_base + 3, :])

    NB = 2
    xs = [S.sbuf(f"xs{i}", [128, D], F32) for i in range(NB)]
    hb = [S.sbuf(f"hb{i}", [128, D], BF16) for i in range(NB)]
    hT = [S.sbuf(f"hT{i}", [128, 8, 128], BF16) for i in range(NB)]
    junk = S.sbuf("junk", [128, D], F32)
    junkb = S.sbuf("junkb", [128, D], BF16)
    st = [S.sbuf(f"st{i}", [128, 4], F32) for i in range(NB)]
    qT = S.sbuf("qT", [128, 16, 128], BF16)
    pq = [S.psum(f"pq{i}", [128, 4, 128], F32) for i in range(2)]
    ps = [S.psum(f"ps{i}", [128, 4, 128], F32) for i in range(2)]
    pout = [S.psum(f"po{i}", [128, 512], F32) for i in range(2)]
    s_sb = S.sbuf("s_sb", [128, 16, 128], F32)
    s2 = S.sbuf("s2", [128, 16, 128], F32)
    top = S.sbuf("top", [128, 16, 16], F32)
    tix = S.sbuf("tix", [128, 16, 16], U32)
    tixf = S.sbuf("tixf", [128, 16, 16], F32)
    cand = S.sbuf("cand", [128, 8, 256], F32)
    cand2 = S.sbuf("cand2", [128, 8, 256], F32)
    pos = S.sbuf("pos", [128, 8, 16], U32)
    posf = S.sbuf("posf", [128, 8, 16], F32)
    ai = S.sbuf("ai", [128, 8, 16], I32)
    af = S.sbuf("af", [128, 8, 16], F32)
    bf_ = S.sbuf("bf_", [128, 8, 16], F32)
    ia = S.sbuf("ia", [128, 8, 16], F32)
    ja = S.sbuf("ja", [128, 8, 16], F32)
    oh = S.sbuf("oh", [128, 2048], F32)
    g = S.sbuf("g", [128, 8, 16], F32)
    ge = S.sbuf("ge", [128, 8, 16], F32)
    gsum = S.sbuf("gsum", [128, 8], F32)
    ef = S.sbuf("ef", [128, 128], F32)
    eidx = [S.sbuf(f"eidx{i}", [128, 128], I32) for i in range(NB)]
    gs = [S.sbuf(f"gs{i}", [128, 128], F32) for i in range(NB)]
    NG = 16
    ug = [S.sbuf(f"ug{i}", [128, 2 * D], BF16) for i in range(NG)]
    actg = [S.sbuf(f"actg{i}", [128, 4], F32) for i in range(4)]
    wvg = [S.sbuf(f"wvg{i}", [128, 4], F32) for i in range(4)]
    ND = 12
    dg = [S.sbuf(f"dg{i}", [128, 128], BF16) for i in range(ND)]
    yo = [S.sbuf(f"yo{i}", [128, D], F32) for i in range(NB)]
    gi = 0
    di = 0

    def stage_I(t):
        b = t // tiles_per_batch
        i2 = t % NB
        if t % tiles_per_batch == 0:
            load_bcast(S, "sp", shift, mod_d[b, mod_base + 0:mod_base + 1, :])
            load_bcast(S, "sp", gmod, mod_d[b, mod_base + 1:mod_base + 2, :])
            S.op("dve", lambda e: e.scalar_tensor_tensor(out=gmod[:], in0=gmod[:], scalar=1.0, in1=grow[:],
                                                         op0=ALU.add, op1=ALU.mult), reads=[gmod, grow], writes=[gmod])
        X, HB, HT, ST = xs[i2], hb[i2], hT[i2], st[i2]
        S.dma("sp", lambda e: e.dma_start(out=X[:], in_=x_in[t * 128:(t + 1) * 128, :]), writes=[X])
        yield
        emit_norm_mod(S, X, gmod, shift, HB, ST, junk)
        yield
        emit_transpose8(S, HB, pT, HT, identb)
        yield
        for gq in range(4):
            P = pq[gq % 2]
            for j in range(4):
                hp = gq * 4 + j
                for kc in range(8):
                    S.op("pe", lambda e: e.matmul(P[:, j, :], lhsT=wq[:, kc, hp * 128:(hp + 1) * 128], rhs=HT[:, kc, :],
                                                  start=(kc == 0), stop=(kc == 7)), reads=[wq, HT], writes=[P])
            S.op("act", lambda e: e.copy(out=qT[:, gq * 4:(gq + 1) * 4, :], in_=P[:]), reads=[P], writes=[qT])
            yield
        for gq in range(4):
            P = ps[gq % 2]
            for j in range(4):
                hp = gq * 4 + j
                S.op("pe", lambda e: e.matmul(P[:, j, :], lhsT=qT[:, hp, :], rhs=skT[:, hp % 2, :], start=True, stop=True),
                     reads=[qT, skT], writes=[P])
            S.op("act", lambda e: e.copy(out=s_sb[:, gq * 4:(gq + 1) * 4, :], in_=P[:]), reads=[P], writes=[s_sb])
            yield
        for hp in range(16):
            S.op("dve", lambda e: e.max(out=top[:, hp, 0:8], in_=s_sb[:, hp, :]), reads=[s_sb], writes=[top])
            S.op("dve", lambda e: e.max_index(out=tix[:, hp, 0:8], in_max=top[:, hp, 0:8], in_values=s_sb[:, hp, :]),
                 reads=[s_sb, top], writes=[tix])
            S.op("dve", lambda e: e.match_replace(out=s2[:, hp, :], in_to_replace=top[:, hp, 0:8], in_values=s_sb[:, hp, :],
                                                  imm_value=-1e30), reads=[s_sb, top], writes=[s2])
            S.op("dve", lambda e: e.max(out=top[:, hp, 8:16], in_=s2[:, hp, :]), reads=[s2], writes=[top])
            S.op("dve", lambda e: e.max_index(out=tix[:, hp, 8:16], in_max=top[:, hp, 8:16], in_values=s2[:, hp, :]),
                 reads=[s2, top], writes=[tix])
            yield
        S.op("dve", lambda e: e.tensor_copy(out=tixf[:], in_=tix[:]), reads=[tix], writes=[tixf])
        top4 = top[:].rearrange("q (h p) a -> q h p a", p=2)
        tix4 = tixf[:].rearrange("q (h p) a -> q h p a", p=2)
        c4 = cand[:].rearrange("q h (a b) -> q h a b", b=16)
        S.op("dve", lambda e: e.tensor_tensor(out=c4, in0=top4[:, :, 0, :].unsqueeze(3).to_broadcast([128, 8, 16, 16]),
                                              in1=top4[:, :, 1, :].unsqueeze(2).to_broadcast([128, 8, 16, 16]), op=ALU.add),
             reads=[top], writes=[cand])
        for h in range(8):
            S.op("dve", lambda e: e.max(out=g[:, h, 0:8], in_=cand[:, h, :]), reads=[cand], writes=[g])
            S.op("dve", lambda e: e.match_replace(out=cand2[:, h, :], in_to_replace=g[:, h, 0:8], in_values=cand[:, h, :],
                                                  imm_value=-1e30), reads=[cand, g], writes=[cand2])
            S.op("dve", lambda e: e.max(out=g[:, h, 8:16], in_=cand2[:, h, :]), reads=[cand2], writes=[g])
            yield
        for h in range(8):
            S.op("dve", lambda e: e.max_index(out=pos[:, h, 0:8], in_max=g[:, h, 0:8], in_values=cand[:, h, :]),
                 reads=[cand, g], writes=[pos])
            S.op("dve", lambda e: e.max_index(out=pos[:, h, 8:16], in_max=g[:, h, 8:16], in_values=cand2[:, h, :]),
                 reads=[cand2, g], writes=[pos])
            yield
        S.op("dve", lambda e: e.tensor_copy(out=posf[:], in_=pos[:]), reads=[pos], writes=[posf])
        S.op("dve", lambda e: e.tensor_scalar(out=ai[:], in0=posf[:], scalar1=0.0625, scalar2=-0.46875, op0=ALU.mult, op1=ALU.add),
             reads=[posf], writes=[ai])
        S.op("dve", lambda e: e.tensor_copy(out=af[:], in_=ai[:]), reads=[ai], writes=[af])
        S.op("dve", lambda e: e.scalar_tensor_tensor(out=bf_[:], in0=af[:], scalar=-16.0, in1=posf[:], op0=ALU.mult, op1=ALU.add),
             reads=[af, posf], writes=[bf_])
        yield
        oh4 = oh[:].rearrange("q (h k a) -> q h k a", k=16, a=16)
        io4 = iota16[:].unsqueeze(1).unsqueeze(1).to_broadcast([128, 8, 16, 16])
        for (src, pidx, dst) in ((af, 0, ia), (bf_, 1, ja)):
            S.op("dve", lambda e: e.tensor_tensor(out=oh4, in0=src[:].unsqueeze(3).to_broadcast([128, 8, 16, 16]), in1=io4, op=ALU.is_equal),
                 reads=[src, iota16], writes=[oh])
            S.op("dve", lambda e: e.tensor_tensor(out=oh4, in0=oh4, in1=tix4[:, :, pidx, :].unsqueeze(2).to_broadcast([128, 8, 16, 16]), op=ALU.mult),
                 reads=[oh, tixf], writes=[oh])
            S.op("dve", lambda e: e.reduce_sum(out=dst[:], in_=oh4, axis=AX.X), reads=[oh], writes=[dst])
            yield
        S.op("dve", lambda e: e.scalar_tensor_tensor(out=ef[:].rearrange("q (h k) -> q h k", k=16), in0=ia[:], scalar=128.0, in1=ja[:],
                                                     op0=ALU.mult, op1=ALU.add), reads=[ia, ja], writes=[ef])
        EI, GS = eidx[i2], gs[i2]
        S.op("dve", lambda e: e.tensor_scalar(out=ef[:], in0=ef[:], scalar1=16383.0, scalar2=0.0, op0=ALU.min, op1=ALU.max),
             reads=[ef], writes=[ef])
        if idx_base:
            S.op("dve", lambda e: e.tensor_scalar_add(out=ef[:], in0=ef[:], scalar1=float(idx_base)), reads=[ef], writes=[ef])
        S.op("dve", lambda e: e.tensor_copy(out=EI[:], in_=ef[:]), reads=[ef], writes=[EI])
        S.op("dve", lambda e: e.tensor_tensor(out=ge[:], in0=g[:], in1=g[:, :, 0:1].to_broadcast([128, 8, 16]), op=ALU.subtract),
             reads=[g], writes=[ge])
        S.op("act", lambda e: e.activation(out=ge[:], in_=ge[:], func=AF.Exp), reads=[ge], writes=[ge])
        S.op("dve", lambda e: e.reduce_sum(out=gsum[:], in_=ge[:], axis=AX.X), reads=[ge], writes=[gsum])
        S.op("dve", lambda e: e.reciprocal(out=gsum[:], in_=gsum[:]), reads=[gsum], writes=[gsum])
        S.op("dve", lambda e: e.tensor_tensor(out=GS[:].rearrange("q (h k) -> q h k", k=16), in0=ge[:],
                                              in1=gsum[:].unsqueeze(2).to_broadcast([128, 8, 16]), op=ALU.mult),
             reads=[ge, gsum], writes=[GS])
    def stage_UV(t, nxt):
        nonlocal gi, di
        i2 = t % NB
        X, HB, EI, GS = xs[i2], hb[i2], eidx[i2], gs[i2]
        gate = gates[t // tiles_per_batch]
        for grp in range(32):
            AG, WG = actg[grp % 4], wvg[grp % 4]
            UVs = []
            for j in range(4):
                s = grp * 4 + j
                UV = ug[gi % NG]
                gi += 1
                UVs.append(UV)
                S.dma("pool", lambda e: e.indirect_dma_start(out=UV[:], out_offset=None, in_=puv_d,
                                                             in_offset=bass.IndirectOffsetOnAxis(ap=EI[:, s:s + 1], axis=0)),
                      reads=[EI], writes=[UV])
                S.op("dve", lambda e: e.scalar_tensor_tensor(out=junkb[:], in0=UV[:, 0:D], scalar=1.0, in1=HB[:], op0=ALU.mult,
                                                             op1=ALU.mult, accum_out=AG[:, j:j + 1]),
                     reads=[UV, HB], writes=[junkb, AG])
            S.op("act", lambda e: e.activation(out=WG[:], in_=AG[:], func=AF.Gelu), reads=[AG], writes=[WG])
            for j in range(4):
                S.op("act", lambda e: e.mul(out=WG[:, j:j + 1], in_=WG[:, j:j + 1], mul=GS[:, grp * 4 + j:grp * 4 + j + 1]),
                     reads=[WG, GS], writes=[WG])
            for j in range(4):
                s = grp * 4 + j
                DG = dg[di % ND]
                di += 1
                S.op("act", lambda e: e.mul(out=DG[:], in_=identb[:], mul=WG[:, j:j + 1]), reads=[identb, WG], writes=[DG])
                for hh in range(2):
                    S.op("pe", lambda e: e.matmul(pout[hh][:], lhsT=DG[:], rhs=UVs[j][:, D + hh * 512:D + (hh + 1) * 512],
                                                  start=(s == 0), stop=(s == 127)), reads=[DG, UVs[j]], writes=[pout[hh]])
            for _ in range(6):
                next(nxt, None)
        Y = yo[i2]
        for hh in range(2):
            S.op("dve", lambda e: e.tensor_mul(out=Y[:, hh * 512:(hh + 1) * 512], in0=pout[hh][:],
                                               in1=gate[:, hh * 512:(hh + 1) * 512]), reads=[pout[hh], gate], writes=[Y])
        S.op("dve", lambda e: e.tensor_add(out=Y[:], in0=Y[:], in1=X[:]), reads=[Y, X], writes=[Y])
        S.dma("sp", lambda e: e.dma_start(out=x_out[t * 128:(t + 1) * 128, :], in_=Y[:]), reads=[Y], writes=[])
        for _ in nxt:
            pass

    g0 = stage_I(0)
    for _ in g0:
        pass
    for t in range(ntiles):
        nxt = stage_I(t + 1) if t + 1 < ntiles else iter(())
        stage_UV(t, nxt)


def emit_convert(S, nc, pairs, rows):
    NBUF = 4
    bufs = [S.sbuf(f"cv{i}", [128, 4, D], BF16) for i in range(NBUF)]
    i = 0
    for src, dst in pairs:
        sv = src.rearrange("(c p r) d -> c p r d", p=128, r=4)
        dv = dst.rearrange("(c p r) d -> c p r d", p=128, r=4)
        for c in range(rows // 512):
            B = bufs[i % NBUF]
            S.dma("pool", lambda e: e.dma_start(out=B[:], in_=sv[c]), writes=[B])
            S.dma(("sp", "act")[i % 2], lambda e: e.dma_start(out=dv[c], in_=B[:]), reads=[B], writes=[])
            i += 1


def emit_convert_inplace(S, nc, tabs, rows):
    NBUF = 4
    bufs = [S.sbuf(f"cv{i}", [128, 4, D], BF16) for i in range(NBUF)]
    views = []
    i = 0
    for tab in tabs:
        sv = tab.rearrange("(c p r) d -> c p r d", p=128, r=4)
        v16 = tab.bitcast(BF16).rearrange("n (two d) -> (n two) d", two=2)
        dv = v16[0:rows, :].rearrange("(c p r) d -> c p r d", p=128, r=4)
        views.append(v16)
        for c in range(rows // 512):
            B = bufs[i % NBUF]
            S.dma("pool", lambda e: e.dma_start(out=B[:], in_=sv[c]), writes=[B])
            S.dma(("sp", "act")[i % 2], lambda e: e.dma_start(out=dv[c], in_=B[:]), reads=[B], writes=[])
            i += 1
    return views


def emit_convert_uv(S, nc, tab_u, tab_v, rows):
    NBUF = 3
    bufs = [S.sbuf(f"cuv{i}", [128, 4, 2 * D], BF16) for i in range(NBUF)]
    su = tab_u.rearrange("(c p r) d -> c p r d", p=128, r=4)
    sv = tab_v.rearrange("(c p r) d -> c p r d", p=128, r=4)
    uv = tab_u.bitcast(BF16)
    dv = uv.rearrange("(c p r) d -> c p r d", p=128, r=4)
    for c in range(rows // 512):
        B = bufs[c % NBUF]
        S.dma("pool", lambda e: e.dma_start(out=B[:, :, 0:D], in_=su[c]), writes=[B])
        S.dma("pool", lambda e: e.dma_start(out=B[:, :, D:2 * D], in_=sv[c]), writes=[B])
        S.dma(("sp", "act")[c % 2], lambda e: e.dma_start(out=dv[c], in_=B[:]), reads=[B], writes=[])
    return uv


def gen_convert_uv(S, nc, tab_u, tab_v, rows):
    NBUF = 3
    bufs = [S.sbuf(f"cuv{i}", [128, 4, 2 * D], BF16) for i in range(NBUF)]
    su = tab_u.rearrange("(c p r) d -> c p r d", p=128, r=4)
    sv = tab_v.rearrange("(c p r) d -> c p r d", p=128, r=4)
    uv = tab_u.bitcast(BF16)
    dv = uv.rearrange("(c p r) d -> c p r d", p=128, r=4)
    for c in range(rows // 512):
        B = bufs[c % NBUF]
        S.dma("pool", lambda e: e.dma_start(out=B[:, :, 0:D], in_=su[c]), writes=[B])
        S.dma("pool", lambda e: e.dma_start(out=B[:, :, D:2 * D], in_=sv[c]), writes=[B])
        S.dma("sp", lambda e: e.dma_start(out=dv[c], in_=B[:]), reads=[B], writes=[])
        yield


import math


def emit_ada(S, nc, cT_d, ada_w_d, ada_b_d, kvw_d, kvb_d, mod_d):
    cT = S.sbuf("cT", [128, 8, 2], F32)
    S.dma("sp", lambda e: e.dma_start(out=cT[:], in_=cT_d), writes=[cT])
    cs = S.sbuf("cs", [128, 8, 2], F32)
    S.op("act", lambda e: e.activation(out=cs[:], in_=cT[:], func=AF.Silu), reads=[cT], writes=[cs])
    wch = [S.sbuf(f"wch{i}", [128, 8, 512], F32) for i in range(2)]
    bch = [S.sbuf(f"bch{i}", [2, 512], F32) for i in range(2)]
    och = [S.sbuf(f"och{i}", [2, 512], F32) for i in range(2)]
    pa = [S.psum(f"pada{i}", [128, 512], F32) for i in range(2)]
    jobs = []
    for l in range(2):
        for ch in range(12):
            jobs.append((ada_w_d[l], ada_b_d[l:l + 1, :], ch, 6 * l + ch // 2, ch % 2))
    for ch in range(4):
        jobs.append((kvw_d, kvb_d, ch, 12 + ch // 2, ch % 2))
    for i, (w_d, b_d, ch, row, half) in enumerate(jobs):
        W, B, O, P = wch[i % 2], bch[i % 2], och[i % 2], pa[i % 2]
        wv = w_d.rearrange("(k p) n -> p k n", p=128)
        q = ("sp", "act")[i % 2]
        S.dma(q, lambda e: e.dma_start(out=W[:], in_=wv[:, :, ch * 512:(ch + 1) * 512]), writes=[W])
        S.dma(q, lambda e: e.dma_start(out=B[:], in_=b_d[:, ch * 512:(ch + 1) * 512].partition_broadcast(2)), writes=[B])
        for kc in range(8):
            S.op("pe", lambda e: e.matmul(P[0:2, :], lhsT=cs[:, kc, :], rhs=W[:, kc, :], start=(kc == 0), stop=(kc == 7)),
                 reads=[cs, W], writes=[P])
        S.op("dve", lambda e: e.tensor_add(out=O[:], in0=P[0:2, :], in1=B[:]), reads=[P, B], writes=[O])
        S.dma(q, lambda e: e.dma_start(out=mod_d[:, row, half * 512:(half + 1) * 512], in_=O[:]), reads=[O], writes=[])


def emit_mlstm(S, nc, x_in, x_out, win_d, cwT_d, bif_d, hg_d, wout_d, gmix_row, mod_d, ntiles, tiles_per_seq,
               identb, utri, ones, side=None):
    win = S.sbuf("win", [128, 8, 3088], BF16)
    win_v = win_d.rearrange("(k p) n -> p k n", p=128)
    for kc in range(8):
        S.dma("pool", lambda e: e.dma_start(out=win[:, kc, 0:2048], in_=win_v[:, kc, 0:2048]), writes=[win])
        S.dma("pool", lambda e: e.dma_start(out=win[:, kc, 2048:3088], in_=win_v[:, kc, 2048:3088]), writes=[win])
    wout = S.sbuf("wout", [128, 8, 1024], BF16)
    wout_v = wout_d.rearrange("(k p) n -> p k n", p=128)
    for kc in range(8):
        S.dma("pool", lambda e: e.dma_start(out=wout[:, kc, :], in_=wout_v[:, kc, :]), writes=[wout])
    cwT = S.sbuf("cwT", [128, 8, 4], F32)
    S.dma("sp", lambda e: e.dma_start(out=cwT[:], in_=cwT_d), writes=[cwT])
    bif = S.sbuf("bif", [128, 16], F32)
    load_bcast(S, "sp", bif, bif_d)
    hg = S.sbuf("hg", [128, D], F32)
    load_bcast(S, "sp", hg, hg_d)
    grow = S.sbuf("grow", [128, D], F32)
    load_bcast(S, "sp", grow, gmix_row)
    gmod = S.sbuf("gmod", [128, D], F32)
    shift = S.sbuf("shift", [128, D], F32)
    gate = S.sbuf("gate", [128, D], F32)

    NB = 2
    xs = [S.sbuf(f"xs{i}", [128, D], F32) for i in range(NB)]
    hb = S.sbuf("hb", [128, D], BF16)
    hT = S.sbuf("hT", [128, 8, 128], BF16)
    junk = S.sbuf("junk", [128, D], F32)
    st = S.sbuf("st", [128, 4], F32)
    cb = S.sbuf("cb", [128, 8, 131], F32)
    cacc = S.sbuf("cacc", [128, 8, 128], F32)
    ctmp = S.sbuf("ctmp", [128, 8, 128], F32)
    qkT = S.sbuf("qkT", [128, 8, 128], BF16)
    ktok = S.sbuf("ktok", [128, 4, 128], BF16)
    qm = S.sbuf("qm", [128, 8, 128], BF16)
    S.op("pool", lambda e: e.memset(qm[:], 0.0), writes=[qm])
    gts = S.sbuf("gts", [128, 16], F32)
    spf = S.sbuf("spf", [128, 8], F32)
    wexp = S.sbuf("wexp", [128, 8], F32)
    eb = S.sbuf("eb", [128, 8], F32)
    ebL = S.sbuf("ebL", [128, 8], F32)
    vaug = S.sbuf("vaug", [128, 8, 129], BF16)
    sig = S.sbuf("sig", [128, D], F32)
    ATs = S.sbuf("ATs", [128, 8, 128], BF16)
    Cst = S.sbuf("Cst", [128, 8, 129], F32)
    Cbf = S.sbuf("Cbf", [128, 8, 129], BF16)
    den = S.sbuf("den", [128, 8], F32)
    hh = S.sbuf("hh", [128, 8, 128], F32)
    sq = S.sbuf("sq", [128, 8, 128], F32)
    hss = S.sbuf("hss", [128, 8], F32)
    hob = S.sbuf("hob", [128, D], BF16)
    hoT = S.sbuf("hoT", [128, 8, 128], BF16)
    yo = [S.sbuf(f"yo{i}", [128, D], F32) for i in range(NB)]

    pT = S.psum("pT", [128, 8, 128], BF16)
    pg = [S.psum(f"pg{i}", [128, 512], F32) for i in range(2)]
    pAT = [S.psum(f"pAT{i}", [128, 4, 128], F32) for i in range(2)]
    pOS = [S.psum(f"pOS{i}", [128, 3, 160], F32) for i in range(3)]
    pgi = 0

    def PO(h):
        return pOS[h // 3], h % 3

    LN8 = math.log(0.125)
    for t in range(ntiles):
        sidx = t // tiles_per_seq
        first = (t % tiles_per_seq == 0)
        X = xs[t % NB]
        if first:
            load_bcast(S, "sp", shift, mod_d[sidx, 0:1, :])
            load_bcast(S, "sp", gmod, mod_d[sidx, 1:2, :])
            load_bcast(S, "sp", gate, mod_d[sidx, 2:3, :])
            S.op("dve", lambda e: e.scalar_tensor_tensor(out=gmod[:], in0=gmod[:], scalar=1.0, in1=grow[:],
                                                         op0=ALU.add, op1=ALU.mult), reads=[gmod, grow], writes=[gmod])
            S.op("pool", lambda e: e.memset(cb[:, :, 0:3], 0.0), writes=[cb])
            S.op("pool", lambda e: e.memset(Cst[:], 0.0), writes=[Cst])
            S.op("pool", lambda e: e.memset(Cbf[:], 0.0), writes=[Cbf])
        S.dma("sp", lambda e: e.dma_start(out=X[:], in_=x_in[t * 128:(t + 1) * 128, :]), writes=[X])
        if side is not None:
            next(side, None)
            next(side, None)
        emit_norm_mod(S, X, gmod, shift, hb, st, junk)
        emit_transpose8(S, hb, pT, hT, identb)
        for half in range(2):
            P = pg[pgi % 2]
            pgi += 1
            for j in range(4):
                fc = half * 4 + j
                for kc in range(8):
                    S.op("pe", lambda e: e.matmul(P[:, j * 128:(j + 1) * 128], lhsT=win[:, kc, fc * 128:(fc + 1) * 128],
                                                  rhs=hT[:, kc, :], start=(kc == 0), stop=(kc == 7)),
                         reads=[win, hT], writes=[P])
            S.op("act", lambda e: e.copy(out=cb[:, half * 4:(half + 1) * 4, 3:131],
                                         in_=P[:].rearrange("p (j n) -> p j n", n=128)), reads=[P], writes=[cb])
        def cw(w):
            return cwT[:, :, w:w + 1].to_broadcast([128, 8, 128])
        S.op("dve", lambda e: e.tensor_tensor(out=cacc[:], in0=cb[:, :, 3:131], in1=cw(3), op=ALU.mult), reads=[cb, cwT], writes=[cacc])
        for w in range(3):
            S.op("pool", lambda e: e.tensor_tensor(out=ctmp[:], in0=cb[:, :, w:w + 128], in1=cw(w), op=ALU.mult),
                 reads=[cb, cwT], writes=[ctmp])
            S.op("dve", lambda e: e.tensor_add(out=cacc[:], in0=cacc[:], in1=ctmp[:]), reads=[cacc, ctmp], writes=[cacc])
        S.op("act", lambda e: e.activation(out=qkT[:, 4:8, :], in_=cacc[:, 4:8, :], func=AF.Silu), reads=[cacc], writes=[qkT])
        qm4 = qm[:].rearrange("p (f two) n -> p f two n", two=2)
        S.op("act", lambda e: e.activation(out=qm4[0:64, :, 0, :], in_=cacc[0:64, 0:4, :], func=AF.Silu), reads=[cacc], writes=[qm])
        S.op("act", lambda e: e.activation(out=qm4[64:128, :, 1, :], in_=cacc[64:128, 0:4, :], func=AF.Silu), reads=[cacc], writes=[qm])
        S.op("pool", lambda e: e.tensor_copy(out=cb[:, :, 0:3], in_=cb[:, :, 128:131]), reads=[cb], writes=[cb])
        for j in range(4):
            S.op("pe", lambda e: e.transpose(out=pT[:, j, :], in_=qkT[:, 4 + j, :], identity=identb[:]),
                 reads=[qkT, identb], writes=[pT])
        S.op("act", lambda e: e.copy(out=ktok[:], in_=pT[:, 0:4, :]), reads=[pT], writes=[ktok])
        P = pg[pgi % 2]
        pgi += 1
        for kc in range(8):
            S.op("pe", lambda e: e.matmul(P[:, 0:16], lhsT=hT[:, kc, :], rhs=win[:, kc, 3072:3088], start=(kc == 0), stop=(kc == 7)),
                 reads=[hT, win], writes=[P])
        S.op("dve", lambda e: e.tensor_add(out=gts[:], in0=P[:, 0:16], in1=bif[:]), reads=[P, bif], writes=[gts])
        S.op("act", lambda e: e.activation(out=spf[:], in_=gts[:, 8:16], func=AF.Exp, scale=-1.0), reads=[gts], writes=[spf])
        S.op("act", lambda e: e.activation(out=spf[:], in_=spf[:], func=AF.Ln, bias=1.0), reads=[spf], writes=[spf])
        S.op("pe", lambda e: e.matmul(P[:, 16:24], lhsT=utri[:], rhs=spf[:], start=True, stop=True), reads=[utri, spf], writes=[P])
        S.op("pe", lambda e: e.matmul(P[:, 32:40], lhsT=ones[:], rhs=spf[:], start=True, stop=True), reads=[ones, spf], writes=[P])
        S.op("dve", lambda e: e.tensor_add(out=wexp[:], in0=P[:, 16:24], in1=gts[:, 0:8]), reads=[P, gts], writes=[wexp])
        S.op("act", lambda e: e.activation(out=wexp[:], in_=wexp[:], func=AF.Exp), reads=[wexp], writes=[wexp])
        S.op("act", lambda e: e.activation(out=eb[:], in_=P[:, 16:24], func=AF.Exp, scale=-1.0, bias=LN8), reads=[P], writes=[eb])
        S.op("act", lambda e: e.activation(out=ebL[:], in_=P[:, 32:40], func=AF.Exp, scale=-1.0), reads=[P], writes=[ebL])
        for half in range(2):
            P = pg[pgi % 2]
            pgi += 1
            for kc in range(8):
                S.op("pe", lambda e: e.matmul(P[:], lhsT=hT[:, kc, :], rhs=win[:, kc, 1024 + half * 512:1024 + (half + 1) * 512],
                                              start=(kc == 0), stop=(kc == 7)), reads=[hT, win], writes=[P])
            S.op("dve", lambda e: e.tensor_tensor(out=vaug[:, half * 4:(half + 1) * 4, 0:128],
                                                  in0=P[:].rearrange("p (h n) -> p h n", n=128),
                                                  in1=wexp[:, half * 4:(half + 1) * 4].unsqueeze(2).to_broadcast([128, 4, 128]),
                                                  op=ALU.mult), reads=[P, wexp], writes=[vaug])
        S.op("pool", lambda e: e.tensor_copy(out=vaug[:, :, 128:129], in_=wexp[:].unsqueeze(2)), reads=[wexp], writes=[vaug])
        for half in range(2):
            P = pg[pgi % 2]
            pgi += 1
            for kc in range(8):
                S.op("pe", lambda e: e.matmul(P[:], lhsT=hT[:, kc, :], rhs=win[:, kc, 2048 + half * 512:2048 + (half + 1) * 512],
                                              start=(kc == 0), stop=(kc == 7)), reads=[hT, win], writes=[P])
            S.op("act", lambda e: e.activation(out=sig[:, half * 512:(half + 1) * 512], in_=P[:], func=AF.Sigmoid),
                 reads=[P], writes=[sig])
        for h in range(8):
            po, fc = (h % 2) * 64, h // 2
            S.op("pe", lambda e: e.matmul(pAT[h // 4][:, h % 4, :], lhsT=qkT[:, 4 + fc, :], rhs=qm[:, h, :],
                                          start=True, stop=True), reads=[qkT, qm], writes=[pAT[h // 4]])
        for half in range(2):
            S.op(("dve", "pool")[0], lambda e: e.tensor_tensor(out=ATs[:, half * 4:(half + 1) * 4, :], in0=pAT[half][:],
                                                  in1=utri[:].unsqueeze(1).to_broadcast([128, 4, 128]), op=ALU.mult),
                 reads=[pAT[half], utri], writes=[ATs])
        for h in range(8):
            po, fc = (h % 2) * 64, h // 2
            PB, sl = PO(h)
            S.op("pe", lambda e: e.matmul(PB[:, sl, 0:129], lhsT=ATs[:, h, :], rhs=vaug[:, h, :], start=True, stop=False),
                 reads=[ATs, vaug], writes=[PB])
            S.op("pe", lambda e: e.matmul(PB[:, sl, 0:129], lhsT=qm[:, h, :], rhs=Cbf[:, h, :],
                                          start=False, stop=True), reads=[qm, Cbf], writes=[PB])
        for bk in range(3):
            nh = 3 if bk < 2 else 2
            S.op("dve", lambda e: e.tensor_tensor(out=den[:, bk * 3:bk * 3 + nh].unsqueeze(2), in0=pOS[bk][:, 0:nh, 128:129],
                                                  in1=eb[:, bk * 3:bk * 3 + nh].unsqueeze(2), op=ALU.mult),
                 reads=[pOS[bk], eb], writes=[den])
        S.op("act", lambda e: e.activation(out=den[:], in_=den[:], func=AF.Abs), reads=[den], writes=[den])
        S.op("dve", lambda e: e.tensor_scalar_max(out=den[:], in0=den[:], scalar1=1.0), reads=[den], writes=[den])
        S.op("dve", lambda e: e.reciprocal(out=den[:], in_=den[:]), reads=[den], writes=[den])
        S.op("dve", lambda e: e.tensor_mul(out=den[:], in0=den[:], in1=eb[:]), reads=[den, eb], writes=[den])
        for bk in range(3):
            nh = 3 if bk < 2 else 2
            S.op("dve", lambda e: e.tensor_tensor(out=hh[:, bk * 3:bk * 3 + nh, :], in0=pOS[bk][:, 0:nh, 0:128],
                                                  in1=den[:, bk * 3:bk * 3 + nh].unsqueeze(2).to_broadcast([128, nh, 128]),
                                                  op=ALU.mult), reads=[pOS[bk], den], writes=[hh])
        for h in range(8):
            PB, sl = PO(h)
            fc = h // 2
            S.op("pe", lambda e: e.matmul(PB[:, sl, 0:129], lhsT=ktok[:, fc, :], rhs=vaug[:, h, :], start=True, stop=True),
                 reads=[ktok, vaug], writes=[PB])
        for bk in range(3):
            nh = 3 if bk < 2 else 2
            S.op("dve", lambda e: e.tensor_tensor(out=Cst[:, bk * 3:bk * 3 + nh, :], in0=pOS[bk][:, 0:nh, 0:129],
                                                  in1=Cst[:, bk * 3:bk * 3 + nh, :], op=ALU.add), reads=[pOS[bk], Cst], writes=[Cst])
        S.op("pool", lambda e: e.tensor_tensor(out=Cst[:], in0=Cst[:], in1=ebL[:].unsqueeze(2).to_broadcast([128, 8, 129]),
                                               op=ALU.mult), reads=[Cst, ebL], writes=[Cst])
        S.op("act", lambda e: e.copy(out=Cbf[:], in_=Cst[:]), reads=[Cst], writes=[Cbf])
        S.op("pool", lambda e: e.tensor_tensor(out=sq[:], in0=hh[:], in1=hh[:], op=ALU.mult), reads=[hh], writes=[sq])
        S.op("dve", lambda e: e.reduce_sum(out=hss[:], in_=sq[:], axis=AX.X), reads=[sq], writes=[hss])
        S.op("act", lambda e: e.activation(out=hss[:], in_=hss[:], func=AF.Sqrt, scale=1.0 / 128, bias=EPS), reads=[hss], writes=[hss])
        S.op("dve", lambda e: e.reciprocal(out=hss[:], in_=hss[:]), reads=[hss], writes=[hss])
        S.op("dve", lambda e: e.tensor_tensor(out=hh[:], in0=hh[:], in1=hss[:].unsqueeze(2).to_broadcast([128, 8, 128]), op=ALU.mult),
             reads=[hh, hss], writes=[hh])
        hh2 = hh[:].rearrange("p h n -> p (h n)")
        S.op("pool", lambda e: e.tensor_tensor(out=hh2, in0=hh2, in1=hg[:], op=ALU.mult), reads=[hh, hg], writes=[hh])
        S.op("dve", lambda e: e.tensor_tensor(out=hob[:], in0=hh2, in1=sig[:], op=ALU.mult), reads=[hh, sig], writes=[hob])
        emit_transpose8(S, hob, pT, hoT, identb)
        Y = yo[t % NB]
        for half in range(2):
            P = pg[pgi % 2]
            pgi += 1
            for kc in range(8):
                S.op("pe", lambda e: e.matmul(P[:], lhsT=hoT[:, kc, :], rhs=wout[:, kc, half * 512:(half + 1) * 512],
                                              start=(kc == 0), stop=(kc == 7)), reads=[hoT, wout], writes=[P])
            S.op("dve", lambda e: e.tensor_mul(out=Y[:, half * 512:(half + 1) * 512], in0=P[:], in1=gate[:, half * 512:(half + 1) * 512]),
                 reads=[P, gate], writes=[Y])
        S.op("pool", lambda e: e.tensor_add(out=Y[:], in0=Y[:], in1=X[:]), reads=[Y, X], writes=[Y])
        S.dma("sp", lambda e: e.dma_start(out=x_out[t * 128:(t + 1) * 128, :], in_=Y[:]), reads=[Y], writes=[])


def drain(gen):
    if gen is not None:
        for _ in gen:
            pass


def emit_headnorm(S, src, gsm, dst, sq, hss, nh, hd):
    s3 = src[:].rearrange("p (h d) -> p h d", d=hd)
    q3 = sq[:].rearrange("p (h d) -> p h d", d=hd)
    d3 = dst[:].rearrange("p (h d) -> p h d", d=hd)
    S.op("pool", lambda e: e.tensor_tensor(out=sq[:], in0=src[:], in1=src[:], op=ALU.mult), reads=[src], writes=[sq])
    S.op("dve", lambda e: e.reduce_sum(out=hss[:], in_=q3, axis=AX.X), reads=[sq], writes=[hss])
    S.op("act", lambda e: e.activation(out=hss[:], in_=hss[:], func=AF.Sqrt, scale=1.0 / hd, bias=EPS), reads=[hss], writes=[hss])
    S.op("dve", lambda e: e.reciprocal(out=hss[:], in_=hss[:]), reads=[hss], writes=[hss])
    S.op("dve", lambda e: e.tensor_tensor(out=q3, in0=s3, in1=hss[:].unsqueeze(2).to_broadcast([128, nh, hd]), op=ALU.mult),
         reads=[src, hss], writes=[sq])
    S.op("pool", lambda e: e.tensor_tensor(out=d3, in0=q3, in1=gsm[:].unsqueeze(1).to_broadcast([128, nh, hd]), op=ALU.mult),
         reads=[sq, gsm], writes=[dst])


def emit_sb(S, nc, x_in, x_out, kvg_row, kvw_d, kng_row, gmix_row, wq_d, qng_row, wout_d, mod_d, nseq, TPS,
            identb, sutri, sltri, ones):
    NH, HD = 16, 64
    kT_all = S.sbuf("kT_all", [128, 8, TPS * 128], BF16)
    v_all = S.sbuf("v_all", [128, TPS, D], BF16)
    xs = [S.sbuf(f"xs{i}", [128, D], F32) for i in range(2)]
    hb = S.sbuf("hb", [128, D], BF16)
    hT = S.sbuf("hT", [128, 8, 128], BF16)
    junk = S.sbuf("junk", [128, D], F32)
    st = S.sbuf("st", [128, 4], F32)
    grow = S.sbuf("grow", [128, D], F32)
    gmod = S.sbuf("gmod", [128, D], F32)
    shift = S.sbuf("shift", [128, D], F32)
    gsm = S.sbuf("gsm", [128, HD], F32)
    pf = S.sbuf("pf", [128, D], F32)
    sq = S.sbuf("sq", [128, D], F32)
    hss = S.sbuf("hss", [128, NH], F32)
    pb = S.sbuf("pb", [128, D], BF16)
    pT = S.psum("pT", [128, 8, 128], BF16)
    pg = S.psum("pg", [128, 512], F32)

    for sidx in range(nseq):
        t0 = sidx * TPS
        with S.scope():
            kvw = S.sbuf("kvw", [128, 8, 2048], BF16)
            kvw_v = kvw_d.rearrange("(k p) n -> p k n", p=128)
            for kc in range(8):
                S.dma("pool", lambda e: e.dma_start(out=kvw[:, kc, :], in_=kvw_v[:, kc, :]), writes=[kvw])
            load_bcast(S, "sp", grow, kvg_row)
            load_bcast(S, "sp", shift, mod_d[sidx, 12:13, :])
            load_bcast(S, "sp", gmod, mod_d[sidx, 13:14, :])
            load_bcast(S, "sp", gsm, kng_row)
            S.op("dve", lambda e: e.scalar_tensor_tensor(out=gmod[:], in0=gmod[:], scalar=1.0, in1=grow[:],
                                                         op0=ALU.add, op1=ALU.mult), reads=[gmod, grow], writes=[gmod])
            for t in range(TPS):
                X = xs[t % 2]
                S.dma("sp", lambda e: e.dma_start(out=X[:], in_=x_in[(t0 + t) * 128:(t0 + t + 1) * 128, :]), writes=[X])
                emit_norm_mod(S, X, gmod, shift, hb, st, junk)
                emit_transpose8(S, hb, pT, hT, identb)
                for ch in range(4):
                    for kc in range(8):
                        S.op("pe", lambda e: e.matmul(pg[:], lhsT=hT[:, kc, :], rhs=kvw[:, kc, ch * 512:(ch + 1) * 512],
                                                      start=(kc == 0), stop=(kc == 7)), reads=[hT, kvw], writes=[pg])
                    if ch < 2:
                        S.op("act", lambda e: e.copy(out=pf[:, ch * 512:(ch + 1) * 512], in_=pg[:]), reads=[pg], writes=[pf])
                    else:
                        S.op("act", lambda e: e.copy(out=v_all[:, t, (ch - 2) * 512:(ch - 1) * 512], in_=pg[:]), reads=[pg], writes=[v_all])
                emit_headnorm(S, pf, gsm, pb, sq, hss, NH, HD)
                for kc in range(8):
                    S.op("pe", lambda e: e.transpose(out=pT[:, kc, :], in_=pb[:, kc * 128:(kc + 1) * 128], identity=identb[:]),
                         reads=[pb, identb], writes=[pT])
                S.op("act", lambda e: e.copy(out=kT_all[:, :, t * 128:(t + 1) * 128], in_=pT[:]), reads=[pT], writes=[kT_all])
        with S.scope():
            wq = S.sbuf("wq", [128, 8, D], BF16)
            wout = S.sbuf("wout", [128, 8, D], BF16)
            wq_v = wq_d.rearrange("(k p) n -> p k n", p=128)
            wout_v = wout_d.rearrange("(k p) n -> p k n", p=128)
            for kc in range(8):
                S.dma("pool", lambda e: e.dma_start(out=wq[:, kc, :], in_=wq_v[:, kc, :]), writes=[wq])
                S.dma("pool", lambda e: e.dma_start(out=wout[:, kc, :], in_=wout_v[:, kc, :]), writes=[wout])
            gate = S.sbuf("gate", [128, D], F32)
            load_bcast(S, "sp", grow, gmix_row)
            load_bcast(S, "sp", shift, mod_d[sidx, 6:7, :])
            load_bcast(S, "sp", gmod, mod_d[sidx, 7:8, :])
            load_bcast(S, "sp", gate, mod_d[sidx, 8:9, :])
            load_bcast(S, "sp", gsm, qng_row)
            S.op("dve", lambda e: e.scalar_tensor_tensor(out=gmod[:], in0=gmod[:], scalar=1.0, in1=grow[:],
                                                         op0=ALU.add, op1=ALU.mult), reads=[gmod, grow], writes=[gmod])
            S.op("dve", lambda e: e.tensor_scalar_mul(out=gsm[:], in0=gsm[:], scalar1=HD ** -0.5), reads=[gsm], writes=[gsm])
            qm = S.sbuf("qm", [128, NH, 128], BF16)
            S.op("pool", lambda e: e.memset(qm[:], 0.0), writes=[qm])
            qm4 = qm[:].rearrange("p (f two) n -> p f two n", two=2)
            E = [S.sbuf(f"E{i}", [128, 512], F32) for i in range(4)]
            SP = [S.sbuf(f"SP{i}", [128, 512], F32) for i in range(4)]
            ARG = [S.sbuf(f"ARG{i}", [128, 512], F32) for i in range(2)]
            XB = [S.sbuf(f"XB{i}", [128, 512], F32) for i in range(2)]
            A = [S.sbuf(f"A{i}", [128, 512], BF16) for i in range(3)]
            SPcum = [S.sbuf(f"SPcum{i}", [128, 512], F32) for i in range(4)]
            yo = [S.sbuf(f"yo{i}", [128, D], F32) for i in range(2)]
            oT = S.sbuf("oT", [128, 8, 128], BF16)
            pz = [S.psum(f"pz{i}", [128, 4, 128], F32) for i in range(2)]
            pnb = [S.psum(f"pnb{i}", [128, 512], F32) for i in range(2)]
            po = [S.psum(f"po{i}", [128, 512], F32) for i in range(2)]
            m3 = sutri[:].unsqueeze(1).to_broadcast([128, 4, 128])
            for qt in range(TPS):
                X = xs[qt % 2]
                S.dma("sp", lambda e: e.dma_start(out=X[:], in_=x_in[(t0 + qt) * 128:(t0 + qt + 1) * 128, :]), writes=[X])
                emit_norm_mod(S, X, gmod, shift, hb, st, junk)
                emit_transpose8(S, hb, pT, hT, identb)
                for ch in range(2):
                    for kc in range(8):
                        S.op("pe", lambda e: e.matmul(pg[:], lhsT=hT[:, kc, :], rhs=wq[:, kc, ch * 512:(ch + 1) * 512],
                                                      start=(kc == 0), stop=(kc == 7)), reads=[hT, wq], writes=[pg])
                    S.op("act", lambda e: e.copy(out=pf[:, ch * 512:(ch + 1) * 512], in_=pg[:]), reads=[pg], writes=[pf])
                emit_headnorm(S, pf, gsm, pb, sq, hss, NH, HD)
                for kc in range(8):
                    S.op("pe", lambda e: e.transpose(out=pT[:, kc, :], in_=pb[:, kc * 128:(kc + 1) * 128], identity=identb[:]),
                         reads=[pb, identb], writes=[pT])
                S.op("act", lambda e: e.copy(out=qm4[0:64, :, 0, :], in_=pT[0:64, :, :]), reads=[pT], writes=[qm])
                S.op("act", lambda e: e.copy(out=qm4[64:128, :, 1, :], in_=pT[64:128, :, :]), reads=[pT], writes=[qm])
                items = [(kt, g) for kt in range(qt, -1, -1) for g in range(4)]
                NI = len(items)

                def S1(i):
                    kt, g = items[i]
                    Z = pz[i % 2]
                    for j in range(4):
                        h = 4 * g + j
                        S.op("pe", lambda e: e.matmul(Z[:, j, :], lhsT=kT_all[:, h // 2, kt * 128:(kt + 1) * 128], rhs=qm[:, h, :],
                                                      start=True, stop=True), reads=[kT_all, qm], writes=[Z])

                def S2(i):
                    kt, g = items[i]
                    Z, Eb, SPb = pz[i % 2], E[i % 4], SP[i % 4]
                    Z2 = Z[:].rearrange("p j n -> p (j n)")
                    S.op("act", lambda e: e.activation(out=Eb[:], in_=Z2, func=AF.Exp), reads=[Z], writes=[Eb])
                    S.op("act", lambda e: e.activation(out=SPb[:], in_=Eb[:], func=AF.Ln, bias=1.0), reads=[Eb], writes=[SPb])
                    if kt == qt:
                        S.op("dve", lambda e: e.tensor_tensor(out=SPb[:].rearrange("p (j n) -> p j n", n=128),
                                                              in0=SPb[:].rearrange("p (j n) -> p j n", n=128), in1=m3, op=ALU.mult),
                             reads=[SPb, sutri], writes=[SPb])

                def S3(i):
                    kt, g = items[i]
                    diag = (kt == qt)
                    NBp, SPb = pnb[i % 2], SP[i % 4]
                    S.op("pe", lambda e: e.matmul(NBp[:], lhsT=sltri[:], rhs=SPb[:], start=True, stop=diag), reads=[sltri, SPb], writes=[NBp])
                    if not diag:
                        S.op("pe", lambda e: e.matmul(NBp[:], lhsT=ones[:], rhs=SPcum[g][:], start=False, stop=True),
                             reads=[ones, SPcum[g]], writes=[NBp])
                    if kt > 0:
                        if diag:
                            S.op("pool", lambda e: e.tensor_copy(out=SPcum[g][:], in_=SPb[:]), reads=[SPb], writes=[SPcum[g]])
                        else:
                            S.op("pool", lambda e: e.tensor_add(out=SPcum[g][:], in0=SPcum[g][:], in1=SPb[:]), reads=[SPb, SPcum[g]], writes=[SPcum[g]])

                def S4(i):
                    kt, g = items[i]
                    diag = (kt == qt)
                    NBp, Eb, SPb, ARGb, Xb, Ab = pnb[i % 2], E[i % 4], SP[i % 4], ARG[i % 2], XB[i % 2], A[i % 3]
                    S.op("dve", lambda e: e.tensor_tensor(out=ARGb[:], in0=SPb[:], in1=NBp[:], op=ALU.add), reads=[SPb, NBp], writes=[ARGb])
                    S.op("act", lambda e: e.activation(out=Xb[:], in_=ARGb[:], func=AF.Exp, scale=-1.0), reads=[ARGb], writes=[Xb])
                    if diag:
                        S.op("dve", lambda e: e.tensor_tensor(out=ARGb[:], in0=Xb[:], in1=Eb[:], op=ALU.mult), reads=[Xb, Eb], writes=[ARGb])
                        S.op("pool", lambda e: e.tensor_tensor(out=Ab[:].rearrange("p (j n) -> p j n", n=128),
                                                               in0=ARGb[:].rearrange("p (j n) -> p j n", n=128), in1=m3, op=ALU.mult),
                             reads=[ARGb, sutri], writes=[Ab])
                    else:
                        S.op("dve", lambda e: e.tensor_tensor(out=Ab[:], in0=Xb[:], in1=Eb[:], op=ALU.mult), reads=[Xb, Eb], writes=[Ab])

                def S5(i):
                    kt, g = items[i]
                    Ab = A[i % 3]
                    for j in range(4):
                        h = 4 * g + j
                        PO = po[h // 8]
                        S.op("pe", lambda e: e.matmul(PO[:, (h % 8) * 64:(h % 8 + 1) * 64], lhsT=Ab[:, j * 128:(j + 1) * 128],
                                                      rhs=v_all[:, kt, h * 64:(h + 1) * 64], start=(kt == qt and h % 8 == 0), stop=(kt == 0)),
                             reads=[Ab, v_all], writes=[PO])

                for n in range(NI + 4):
                    if n < NI:
                        S1(n)
                    if 0 <= n - 1 < NI:
                        S2(n - 1)
                    if 0 <= n - 2 < NI:
                        S3(n - 2)
                    if 0 <= n - 3 < NI:
                        S4(n - 3)
                    if 0 <= n - 4 < NI:
                        S5(n - 4)
                for half in range(2):
                    S.op("act", lambda e: e.copy(out=pb[:, half * 512:(half + 1) * 512], in_=po[half][:]), reads=[po[half]], writes=[pb])
                emit_transpose8(S, pb, pT, oT, identb)
                Y = yo[qt % 2]
                for half in range(2):
                    for kc in range(8):
                        S.op("pe", lambda e: e.matmul(pg[:], lhsT=oT[:, kc, :], rhs=wout[:, kc, half * 512:(half + 1) * 512],
                                                      start=(kc == 0), stop=(kc == 7)), reads=[oT, wout], writes=[pg])
                    S.op("dve", lambda e: e.tensor_mul(out=Y[:, half * 512:(half + 1) * 512], in0=pg[:], in1=gate[:, half * 512:(half + 1) * 512]),
                         reads=[pg, gate], writes=[Y])
                S.op("pool", lambda e: e.tensor_add(out=Y[:], in0=Y[:], in1=X[:]), reads=[Y, X], writes=[Y])
                S.dma("sp", lambda e: e.dma_start(out=x_out[(t0 + qt) * 128:(t0 + qt + 1) * 128, :], in_=Y[:]), reads=[Y], writes=[])


NCORES = 8
SEQ = 2048
TPS = SEQ // 128
NSEQ = 2
NT = NSEQ * TPS
_NC_CACHE = {}


def build_program():
    nc = bass.Bass("TRN2", target_bir_lowering=False)

    def din(name, shape):
        return nc.dram_tensor(name, list(shape), F32, kind="ExternalInput").ap()

    x = din("x", [NT * 128, D])
    cT = din("cT", [128, 8, 2])
    ident = din("ident", [128, 128]); utri_d = din("utri", [128, 128]); sutri_d = din("sutri", [128, 128])
    sltri_d = din("sltri", [128, 128]); ones_d = din("ones", [128, 128]); iota_d = din("iota16", [128, 16])
    ada_w = din("ada_w", [2, D, 6 * D]); ada_b = din("ada_b", [2, 6 * D])
    norm_mix_g = din("norm_mix_g", [2, D]); norm_ffn_g = din("norm_ffn_g", [2, D])
    ma_w_in = din("ma_w_in", [D, 3088]); cwT = din("cwT", [128, 8, 4]); ma_b_if = din("ma_b_if", [1, 16])
    ma_hnorm_g = din("ma_hnorm_g", [1, D]); ma_w_out = din("ma_w_out", [D, D])
    kv_ada_w = din("kv_ada_w", [D, 2 * D]); kv_ada_b = din("kv_ada_b", [1, 2 * D]); kv_norm_g = din("kv_norm_g", [1, D])
    kv_w = din("kv_w", [D, 2 * D]); k_norm_g = din("k_norm_g", [1, 64])
    sb_w_q = din("sb_w_q", [D, D]); sb_q_norm_g = din("sb_q_norm_g", [1, 64]); sb_w_out = din("sb_w_out", [D, D])
    peer_w_q = din("peer_w_q", [2, D, 2 * D]); peer_sub_keys = din("peer_sub_keys", [2, 2, 128, 128])
    peer_u = din("peer_u", [2, 16384, D]); peer_v = din("peer_v", [2, 16384, D])
    pu_flat = peer_u.rearrange("l e d -> (l e) d")
    pv_flat = peer_v.rearrange("l e d -> (l e) d")
    out = nc.dram_tensor("out", [NT * 128, D], F32, kind="ExternalOutput").ap()
    mod = nc.dram_tensor("mod_scr", [2, 14, D], F32, kind="Internal").ap()
    xa = nc.dram_tensor("xa_scr", [NT * 128, D], F32, kind="Internal").ap()
    xb = nc.dram_tensor("xb_scr", [NT * 128, D], F32, kind="Internal").ap()
    xc = nc.dram_tensor("xc_scr", [NT * 128, D], F32, kind="Internal").ap()

    with ExitStack() as es:
        S = Sched(nc, es)
        identb = S.sbuf("identb", [128, 128], BF16)
        S.dma("pool", lambda e: e.dma_start(out=identb[:], in_=ident), writes=[identb])
        cs = {}
        for nm, d in (("utri", utri_d), ("sutri", sutri_d), ("sltri", sltri_d), ("ones", ones_d)):
            cs[nm] = S.sbuf(nm, [128, 128], F32)
            S.dma("sp", lambda e: e.dma_start(out=cs[nm][:], in_=d), writes=[cs[nm]])
        iota16 = S.sbuf("iota16", [128, 16], F32)
        S.dma("sp", lambda e: e.dma_start(out=iota16[:], in_=iota_d), writes=[iota16])
        puv = pu_flat.bitcast(BF16)
        with S.scope():
            emit_ada(S, nc, cT, ada_w, ada_b, kv_ada_w, kv_ada_b, mod)
        with S.scope():
            side = gen_convert_uv(S, nc, pu_flat, pv_flat, 32768)
            emit_mlstm(S, nc, x, xa, ma_w_in, cwT, ma_b_if, ma_hnorm_g, ma_w_out, norm_mix_g[0:1, :], mod, NT, TPS,
                       identb, cs["utri"], cs["ones"], side)
            drain(side)
        with S.scope():
            emit_peer(S, nc, xa, xb, peer_w_q[0], peer_sub_keys[0], puv, norm_ffn_g[0:1, :], mod, 3, NT, TPS, identb, iota16, 0)
        with S.scope():
            emit_sb(S, nc, xb, xc, kv_norm_g, kv_w, k_norm_g, norm_mix_g[1:2, :], sb_w_q, sb_q_norm_g, sb_w_out, mod, NSEQ, TPS,
                    identb, cs["sutri"], cs["sltri"], cs["ones"])
        with S.scope():
            emit_peer(S, nc, xc, out, peer_w_q[1], peer_sub_keys[1], puv, norm_ffn_g[1:2, :], mod, 9, NT, TPS, identb, iota16, 16384)
        S.barrier()
    return nc


def kernel(x, c, ada_w, ada_b, norm_mix_g, norm_ffn_g, ma_w_in, ma_conv_w, ma_b_if, ma_hnorm_g, ma_w_out,
           kv_ada_w, kv_ada_b, kv_norm_g, kv_w, k_norm_g, sb_w_q, sb_q_norm_g, sb_w_out,
           peer_w_q, peer_sub_keys, peer_u, peer_v):
    f = lambda a: np.ascontiguousarray(np.asarray(a, dtype=np.float32))
    x = f(x); c = f(c)
    one = np.ones((128, 128), np.float32)
    shared = {
        "ident": np.eye(128, dtype=np.float32), "utri": np.triu(one), "sutri": np.triu(one, 1), "sltri": np.tril(one, -1), "ones": one,
        "iota16": np.tile(np.arange(16, dtype=np.float32), (128, 1)),
        "ada_w": f(ada_w), "ada_b": f(ada_b), "norm_mix_g": f(norm_mix_g), "norm_ffn_g": f(norm_ffn_g),
        "ma_w_in": f(ma_w_in)[0], "cwT": np.ascontiguousarray(f(ma_conv_w)[0].reshape(4, 8, 128).transpose(2, 1, 0)),
        "ma_b_if": f(ma_b_if).reshape(1, 16), "ma_hnorm_g": f(ma_hnorm_g).reshape(1, D), "ma_w_out": f(ma_w_out)[0],
        "kv_ada_w": f(kv_ada_w), "kv_ada_b": f(kv_ada_b).reshape(1, 2 * D), "kv_norm_g": f(kv_norm_g).reshape(1, D),
        "kv_w": f(kv_w), "k_norm_g": f(k_norm_g).reshape(1, 64),
        "sb_w_q": f(sb_w_q)[0], "sb_q_norm_g": f(sb_q_norm_g).reshape(1, 64), "sb_w_out": f(sb_w_out)[0],
        "peer_w_q": f(peer_w_q), "peer_sub_keys": f(peer_sub_keys), "peer_u": f(peer_u), "peer_v": f(peer_v),
    }
    in_maps = []
    for i in range(NCORES):
        m = dict(shared)
        m["x"] = x[NSEQ * i:NSEQ * (i + 1)].reshape(NT * 128, D)
        m["cT"] = np.ascontiguousarray(c[NSEQ * i:NSEQ * (i + 1)].T.reshape(8, 128, NSEQ).transpose(1, 0, 2))
        in_maps.append(m)
    if "nc" not in _NC_CACHE:
        _NC_CACHE["nc"] = build_program()
    res = run_bass_kernel_spmd(_NC_CACHE["nc"], in_maps, core_ids=list(range(NCORES)))
    return np.concatenate([r["out"].reshape(NSEQ, SEQ, D) for r in res.results], axis=0)
```
